# Optimizing a Trainium2 kernel written in Bass

```python
import math
import jax, jax.numpy as jnp
from jax import lax
import numpy as np

D_MODEL = 2048
BATCH = 8
SEQ = 2048
DEPTH = 1

MLA_HEADS = 16
MLA_Q_RANK = 768
MLA_KV_RANK = 512
MLA_NOPE_DIM = 128
MLA_ROPE_DIM = 64
MLA_V_DIM = 128
MLA_QK_DIM = MLA_NOPE_DIM + MLA_ROPE_DIM
ROPE_THETA = 10000.0
Q_BLOCK = 128
DIL_GROUPS = ((128, 1), (512, 4), (2048, 16))
DIL_HEADS_PER_GROUP = 4
DIL_HEADS = DIL_HEADS_PER_GROUP * len(DIL_GROUPS)
DIL_HEAD_DIM = 128
DIL_WIDTH = DIL_HEADS * DIL_HEAD_DIM
IN_SIZES = (MLA_Q_RANK, MLA_KV_RANK, MLA_ROPE_DIM, DIL_WIDTH, DIL_WIDTH, DIL_WIDTH, D_MODEL, D_MODEL)
IN_COLS = sum(IN_SIZES)
IN_OFFSETS = tuple(int(v) for v in np.cumsum(IN_SIZES)[:-1])
N_EXPERTS = 32
TOP_K = 4
D_FF = 2048
SWIGLU_LIMIT = 7.0
SWIGLU_ALPHA = 1.702
MOE_BLOCK = 128
DN_ALPHA = (2.0 * DEPTH) ** 0.25
DN_BETA = (8.0 * DEPTH) ** -0.25
LN_EPS = 1e-5
RMS_EPS = 1e-6

kernel_name = "hybrid_mla_dilated_moe_deepnorm"


def layer_norm(x, g, b):
    xf = x.astype(jnp.float32)
    mu = jnp.mean(xf, axis=-1, keepdims=True)
    var = jnp.mean(jnp.square(xf - mu), axis=-1, keepdims=True)
    return ((xf - mu) * lax.rsqrt(var + LN_EPS) * g.astype(jnp.float32) + b.astype(jnp.float32)).astype(x.dtype)


def rms_norm(x, g):
    xf = x.astype(jnp.float32)
    ms = jnp.mean(jnp.square(xf), axis=-1, keepdims=True)
    return (xf * lax.rsqrt(ms + RMS_EPS) * g.astype(jnp.float32)).astype(x.dtype)


def rope(x, pos):
    half = x.shape[-1] // 2
    inv = ROPE_THETA ** (-jnp.arange(half, dtype=jnp.float32) / half)
    ang = pos.astype(jnp.float32)[:, None] * inv[None, :]
    cos = jnp.cos(ang)[:, None, :]
    sin = jnp.sin(ang)[:, None, :]
    xf = x.astype(jnp.float32)
    x1, x2 = xf[..., :half], xf[..., half:]
    return jnp.concatenate([x1 * cos - x2 * sin, x2 * cos + x1 * sin], axis=-1).astype(x.dtype)


def mla_attention(c_q, c_kv, k_pe, q_norm_g, kv_norm_g, w_uq, w_ukv):
    B, S, _ = c_q.shape
    pos = jnp.arange(S)
    q = (rms_norm(c_q, q_norm_g) @ w_uq).reshape(B, S, MLA_HEADS, MLA_QK_DIM)
    q = jnp.concatenate([q[..., :MLA_NOPE_DIM], rope(q[..., MLA_NOPE_DIM:], pos)], axis=-1)
    kv = (rms_norm(c_kv, kv_norm_g) @ w_ukv).reshape(B, S, MLA_HEADS, MLA_NOPE_DIM + MLA_V_DIM)
    k_nope, v = kv[..., :MLA_NOPE_DIM], kv[..., MLA_NOPE_DIM:]
    k_rot = rope(k_pe[:, :, None, :], pos)
    k = jnp.concatenate([k_nope, jnp.broadcast_to(k_rot, (B, S, MLA_HEADS, MLA_ROPE_DIM))], axis=-1)
    scale = MLA_QK_DIM ** -0.5
    nb = S // Q_BLOCK
    qb = q.reshape(B, nb, Q_BLOCK, MLA_HEADS, MLA_QK_DIM).transpose(1, 0, 2, 3, 4)

    def block(args):
        qi, bi = args
        s = jnp.einsum('bqhd,bkhd->bhqk', qi, k).astype(jnp.float32) * scale
        qpos = bi * Q_BLOCK + jnp.arange(Q_BLOCK)
        causal = pos[None, :] <= qpos[:, None]
        s = jnp.where(causal[None, None], s, -jnp.inf)
        p = jax.nn.softmax(s, axis=-1).astype(v.dtype)
        return jnp.einsum('bhqk,bkhd->bqhd', p, v)

    o = lax.map(block, (qb, jnp.arange(nb)))
    return o.transpose(1, 0, 2, 3, 4).reshape(B, S, MLA_HEADS * MLA_V_DIM)


def dilated_group(q, k, v, slopes, window, dilation):
    B, S, Hg, Dh = q.shape
    n_win = window // dilation
    blk = n_win
    n = S // dilation
    nb = -(-n // blk)
    n_pad = nb * blk

    def to_sub(t):
        return t.reshape(B, n, dilation, Hg, Dh).transpose(0, 2, 1, 3, 4)

    qs = jnp.pad(to_sub(q), ((0, 0), (0, 0), (0, n_pad - n), (0, 0), (0, 0)))
    qs = qs.reshape(B, dilation, nb, blk, Hg, Dh)

    def key_blocks(t):
        tp = jnp.pad(to_sub(t), ((0, 0), (0, 0), (blk, n_pad - n), (0, 0), (0, 0)))
        prev = tp[:, :, :n_pad].reshape(B, dilation, nb, blk, Hg, Dh)
        cur = tp[:, :, blk:].reshape(B, dilation, nb, blk, Hg, Dh)
        return jnp.concatenate([prev, cur], axis=3)

    kb, vb = key_blocks(k), key_blocks(v)
    s = jnp.einsum('brnqhd,brnkhd->brnhqk', qs, kb).astype(jnp.float32) * (Dh ** -0.5)
    i = jnp.arange(blk)[:, None]
    j = jnp.arange(2 * blk)[None, :] - blk
    steps = i - j
    kpos = jnp.arange(nb)[:, None, None] * blk + j[None]
    valid = ((steps >= 0) & (steps <= n_win))[None] & (kpos >= 0)
    alibi = -slopes.astype(jnp.float32)[:, None, None] * (steps * dilation).astype(jnp.float32)[None]
    s = jnp.where(valid[None, None, :, None], s + alibi, -jnp.inf)
    lse = jax.nn.logsumexp(s, axis=-1)
    p = jnp.exp(s - lse[..., None]).astype(v.dtype)
    o = jnp.einsum('brnhqk,brnkhd->brnqhd', p, vb)
    o = o.reshape(B, dilation, n_pad, Hg, Dh)[:, :, :n].transpose(0, 2, 1, 3, 4).reshape(B, S, Hg, Dh)
    lse = lse.transpose(0, 1, 2, 4, 3).reshape(B, dilation, n_pad, Hg)[:, :, :n]
    lse = lse.transpose(0, 2, 1, 3).reshape(B, S, Hg)
    return o, lse


def dilated_attention(q, k, v):
    B, S = q.shape[:2]
    slopes = 2.0 ** (-8.0 * jnp.arange(1, DIL_HEADS + 1, dtype=jnp.float32) / DIL_HEADS)
    outs, lses = [], []
    for g, (window, dilation) in enumerate(DIL_GROUPS):
        sl = slice(g * DIL_HEADS_PER_GROUP, (g + 1) * DIL_HEADS_PER_GROUP)
        o, l = dilated_group(q[:, :, sl], k[:, :, sl], v[:, :, sl], slopes[sl], window, dilation)
        outs.append(o)
        lses.append(l)
    wts = jax.nn.softmax(jnp.stack(lses, axis=0), axis=0)
    o = jnp.concatenate([o_g * w_g[..., None].astype(o_g.dtype) for o_g, w_g in zip(outs, wts)], axis=2)
    return o.reshape(B, S, DIL_WIDTH)


def token_mixer(x, w_in, q_norm_g, kv_norm_g, w_uq, w_ukv, w_o_mla, w_o_dil, w_out):
    B, S, _ = x.shape
    proj = x @ w_in
    c_q, c_kv, k_pe, q_d, k_d, v_d, gate_mla, gate_dil = jnp.split(proj, IN_OFFSETS, axis=-1)
    o_mla = mla_attention(c_q, c_kv, k_pe, q_norm_g, kv_norm_g, w_uq, w_ukv)
    hs = (B, S, DIL_HEADS, DIL_HEAD_DIM)
    o_dil = dilated_attention(q_d.reshape(hs), k_d.reshape(hs), v_d.reshape(hs))
    merged = jax.nn.sigmoid(gate_mla) * (o_mla @ w_o_mla) + jax.nn.sigmoid(gate_dil) * (o_dil @ w_o_dil)
    return merged @ w_out


def moe_ffn(x, w_router, b_router, w1, b1, w2, b2):
    B, S, D = x.shape
    xt = x.reshape(-1, D)
    N = xt.shape[0]
    logits = (xt @ w_router + b_router).astype(jnp.float32)
    top_val, top_idx = lax.top_k(logits, TOP_K)
    gates = jax.nn.softmax(top_val, axis=-1)
    NK = N * TOP_K
    e_flat = top_idx.reshape(-1).astype(jnp.int32)
    g_flat = gates.reshape(-1)
    tok_flat = jnp.arange(NK, dtype=jnp.int32) // TOP_K
    order = jnp.argsort(e_flat)
    e_sorted, g_sorted, tok_sorted = e_flat[order], g_flat[order], tok_flat[order]
    counts = jnp.bincount(e_flat, length=N_EXPERTS)
    padded = (counts + MOE_BLOCK - 1) // MOE_BLOCK * MOE_BLOCK
    pad_end = jnp.cumsum(padded)
    pad_start = pad_end - padded
    start = jnp.cumsum(counts) - counts
    dest = pad_start[e_sorted] + jnp.arange(NK, dtype=jnp.int32) - start[e_sorted]
    cap = (-(-NK // MOE_BLOCK) + N_EXPERTS) * MOE_BLOCK
    n_blocks = cap // MOE_BLOCK
    tok_buf = jnp.zeros((cap,), jnp.int32).at[dest].set(tok_sorted)
    gate_buf = jnp.zeros((cap,), jnp.float32).at[dest].set(g_sorted)
    blk_expert = jnp.minimum(
        jnp.searchsorted(pad_end, jnp.arange(n_blocks) * MOE_BLOCK, side='right'), N_EXPERTS - 1)

    def run(args):
        toks, g, e = args
        h = xt[toks] @ w1[e] + b1[e]
        x_glu = jnp.minimum(h[:, ::2], SWIGLU_LIMIT)
        x_lin = jnp.clip(h[:, 1::2], -SWIGLU_LIMIT, SWIGLU_LIMIT)
        a = x_glu * jax.nn.sigmoid(SWIGLU_ALPHA * x_glu) * (x_lin + 1.0)
        return ((a @ w2[e] + b2[e]) * g[:, None]).astype(x.dtype)

    out = lax.map(run, (tok_buf.reshape(n_blocks, MOE_BLOCK), gate_buf.reshape(n_blocks, MOE_BLOCK), blk_expert))
    y = jax.ops.segment_sum(out.reshape(cap, D), tok_buf, num_segments=N)
    return y.reshape(B, S, D).astype(x.dtype)


def setup_inputs(seed: int = 0) -> dict:
    key = jax.random.key(seed)
    ks = jax.random.split(key, 24)
    f32 = jnp.float32
    L = DEPTH

    def nrm(k, shape, scale):
        return jax.random.normal(k, shape, f32) * scale

    return {
        "x": jax.random.normal(ks[0], (BATCH, SEQ, D_MODEL), f32),
        "w_in": nrm(ks[1], (L, D_MODEL, IN_COLS), D_MODEL ** -0.5),
        "q_norm_g": 1.0 + nrm(ks[2], (L, MLA_Q_RANK), 0.02),
        "kv_norm_g": 1.0 + nrm(ks[3], (L, MLA_KV_RANK), 0.02),
        "w_uq": nrm(ks[4], (L, MLA_Q_RANK, MLA_HEADS * MLA_QK_DIM), MLA_Q_RANK ** -0.5),
        "w_ukv": nrm(ks[5], (L, MLA_KV_RANK, MLA_HEADS * (MLA_NOPE_DIM + MLA_V_DIM)), MLA_KV_RANK ** -0.5),
        "w_o_mla": nrm(ks[6], (L, MLA_HEADS * MLA_V_DIM, D_MODEL), DN_BETA * (MLA_HEADS * MLA_V_DIM) ** -0.5),
        "w_o_dil": nrm(ks[7], (L, DIL_WIDTH, D_MODEL), DN_BETA * DIL_WIDTH ** -0.5),
        "w_out": nrm(ks[8], (L, D_MODEL, D_MODEL), DN_BETA * D_MODEL ** -0.5),
        "ln1_g": 1.0 + nrm(ks[9], (L, D_MODEL), 0.02),
        "ln1_b": nrm(ks[10], (L, D_MODEL), 0.02),
        "w_router": nrm(ks[11], (L, D_MODEL, N_EXPERTS), D_MODEL ** -0.5),
        "b_router": nrm(ks[12], (L, N_EXPERTS), 0.01),
        "w1": nrm(ks[13], (L, N_EXPERTS, D_MODEL, 2 * D_FF), D_MODEL ** -0.5),
        "b1": nrm(ks[14], (L, N_EXPERTS, 2 * D_FF), 0.02),
        "w2": nrm(ks[15], (L, N_EXPERTS, D_FF, D_MODEL), DN_BETA * D_FF ** -0.5),
        "b2": nrm(ks[16], (L, N_EXPERTS, D_MODEL), 0.02),
        "ln2_g": 1.0 + nrm(ks[17], (L, D_MODEL), 0.02),
        "ln2_b": nrm(ks[18], (L, D_MODEL), 0.02),
    }


def reference(x, w_in, q_norm_g, kv_norm_g, w_uq, w_ukv, w_o_mla, w_o_dil, w_out, ln1_g, ln1_b,
              w_router, b_router, w1, b1, w2, b2, ln2_g, ln2_b):
    for l in range(DEPTH):
        mix = token_mixer(x, w_in[l], q_norm_g[l], kv_norm_g[l], w_uq[l], w_ukv[l],
                          w_o_mla[l], w_o_dil[l], w_out[l])
        x = layer_norm(DN_ALPHA * x + mix, ln1_g[l], ln1_b[l])
        ffn = moe_ffn(x, w_router[l], b_router[l], w1[l], b1[l], w2[l], b2[l])
        x = layer_norm(DN_ALPHA * x + ffn, ln2_g[l], ln2_b[l])
    return x
```

```python
import math
from contextlib import ExitStack

import numpy as np
import ml_dtypes
import concourse.bass as bass
import concourse.mybir as mybir
from concourse.bass_utils import run_bass_kernel_spmd

F32 = mybir.dt.float32
BF16 = mybir.dt.bfloat16
I32 = mybir.dt.int32
U32 = mybir.dt.uint32
AF = mybir.ActivationFunctionType
ALU = mybir.AluOpType
AX = mybir.AxisListType

S = 2048
D = 2048
NCORES = 8
QR, KVR, ROPE = 768, 512, 64
MH = 16
DH = 12
NE = 32
CAP = 384
NST = CAP // 128
DFF = 2048
DN_ALPHA = 2.0 ** 0.25
LN_EPS = 1e-5
RMS_EPS = 1e-6
DIL = ((2048, 1), (512, 4), (128, 16))


class Buf:
    __slots__ = ("name", "ws", "rs")

    def __init__(self, name=""):
        self.name = name
        self.ws = []
        self.rs = []


class DSem:
    __slots__ = ("sem", "count")

    def __init__(self, sem):
        self.sem = sem
        self.count = 0


class Op:
    __slots__ = ("eng", "fn", "deps", "sig", "idx", "dsem", "dval", "waits", "ph")


class Phase:
    ENGS = ("pe", "act", "dve", "pool", "sp")

    POOL = None

    def __init__(self, nc, name):
        self.nc = nc
        self.name = name
        self.ops = []
        pool = Phase.POOL
        if pool is None or pool["nc"] is not nc:
            st = ExitStack()
            pool = Phase.POOL = {"nc": nc, "stack": st, "esem": {}, "ebase": {}, "dsems": []}
            for e in ("pe", "act", "dve", "pool"):
                pool["esem"][e] = st.enter_context(nc.semaphore(f"sem_{e}"))
                pool["ebase"][e] = 0
        self.pool = pool
        self.esem = pool["esem"]
        self.dsems = []
        self.nd = 0

    def dsem(self):
        pool = self.pool
        if self.nd == len(pool["dsems"]):
            pool["dsems"].append(DSem(pool["stack"].enter_context(self.nc.semaphore(f"sem_d{self.nd}"))))
        d = pool["dsems"][self.nd]
        self.nd += 1
        self.dsems.append(d)
        return d

    def op(self, eng, fn, r=(), w=(), dsem=None):
        o = Op()
        o.eng, o.fn, o.sig, o.idx, o.dsem, o.dval, o.waits = eng, fn, False, 0, dsem, 0, None
        o.ph = self
        isdma = dsem is not None
        if isdma:
            dsem.count += 16
            o.dval = dsem.count
        deps = {}

        def add(p, raw):
            if p is o or p.ph is not self:
                return
            if (not isdma) and p.dsem is None and p.eng == eng:
                if not raw or eng == "pe":
                    return
            deps[id(p)] = p

        for b in r:
            for p in b.ws:
                add(p, True)
        for b in w:
            for p in b.ws:
                add(p, False)
            for p in b.rs:
                add(p, False)
        o.deps = list(deps.values())
        for p in o.deps:
            if p.dsem is None:
                p.sig = True
        for b in w:
            if b.rs:
                b.ws = [o]
                b.rs = []
            else:
                b.ws = [p for p in b.ws if not (p.dsem is None and p.eng == eng and not isdma)] + [o]
        for b in r:
            b.rs.append(o)
        self.ops.append(o)
        return o

    def dma(self, out, in_, r=(), w=(), dsem=None, eng="sp", **kw):
        return self.op(eng, lambda e: e.dma_start(out=out, in_=in_, **kw), r, w, dsem=dsem)

    def mm(self, out, lhsT, rhs, start, stop, r=(), w=()):
        return self.op("pe", lambda e: e.matmul(out, lhsT, rhs, start=start, stop=stop), r, w)

    def finalize(self):
        nc = self.nc
        cnt = {e: self.pool["ebase"].get(e, 0) for e in self.ENGS}
        for o in self.ops:
            if o.dsem is None and o.sig:
                cnt[o.eng] += 1
                o.idx = cnt[o.eng]
        for e in self.pool["ebase"]:
            self.pool["ebase"][e] = cnt[e]
        seen = {}
        for o in self.ops:
            need = {}
            for p in o.deps:
                if p.dsem is not None:
                    key, sem, val = ("d", id(p.dsem)), p.dsem.sem, p.dval
                else:
                    key, sem, val = ("e", p.eng), self.esem[p.eng], p.idx
                if val > need.get(key, (None, 0))[1]:
                    need[key] = (sem, val)
            o.waits = []
            for key, (sem, val) in need.items():
                if val > seen.get((o.eng, key), 0):
                    seen[(o.eng, key)] = val
                    o.waits.append((sem, val))
        by = {e: [o for o in self.ops if o.eng == e] for e in self.ENGS}
        esem = self.esem
        dsems = self.dsems

        def mk(en):
            def body(e):
                for o in by[en]:
                    for sem, val in o.waits:
                        e.wait_ge(sem, val)
                    ins = o.fn(e)
                    if o.dsem is not None:
                        ins.then_inc(o.dsem.sem, 16)
                    elif o.sig:
                        ins.then_inc(esem[en], 1)
                if en == "sp":
                    for d in dsems:
                        if d.count:
                            e.wait_ge(d.sem, d.count)
            return body

        with nc.Block() as blk:
            blk.tensor(mk("pe"))
            blk.scalar(mk("act"))
            blk.vector(mk("dve"))
            blk.gpsimd(mk("pool"))
            blk.sync(mk("sp"))


class Ring:
    def __init__(self, P, tiles, with_dsem=False):
        self.tiles = tiles
        self.bufs = [Buf() for _ in tiles]
        self.ds = [P.dsem() for _ in tiles] if with_dsem else None
        self.i = -1

    def next(self):
        self.i += 1
        k = self.i % len(self.tiles)
        return self.tiles[k], self.bufs[k], (self.ds[k] if self.ds else None)


def build(stop_after=None, debug=(), nblkA=67, doV=True, nhB2=16):
    nc = bass.Bass("TRN2", target_bir_lowering=False)

    def din(name, shape, dt=F32):
        return nc.dram_tensor(name, list(shape), dt, kind="ExternalInput").ap()

    def dscr(name, shape, dt):
        kind = "ExternalOutput" if name in debug else "Internal"
        return nc.dram_tensor(name, list(shape), dt, kind=kind).ap()

    xT = din("xT", [D, S])
    x_tm = din("x", [S, D])
    wA = din("wA", [67, 128, 16, 128])
    wV = din("wV", [12, 128, 4, 512])
    out = nc.dram_tensor("out", [S, D], F32, kind="ExternalOutput").ap()

    lat = dscr("lat", [1408, S], BF16)
    qd = dscr("qd", [1536, S], BF16)
    kd = dscr("kd", [1536, S], BF16)
    vd = dscr("vd", [3, S, 512], BF16)
    sg = dscr("sg", [4096, S], F32)

    es = ExitStack()

    def sb(name, shape, dt):
        return es.enter_context(nc.sbuf_tensor(name, list(shape), dt))

    def ps(name, shape, dt=F32):
        return es.enter_context(nc.psum_tensor(name, list(shape), dt))

    with es:
        P = Phase(nc, "A")
        XT = sb("XT", [128, 16, S], BF16)
        XTb = [Buf() for _ in range(16)]
        xst = Ring(P, [sb(f"xst{i}", [128, S], F32) for i in range(2)], True)
        wst = Ring(P, [sb(f"wst{i}", [128, 2048], F32) for i in range(3)], True)
        wbf = Ring(P, [sb(f"wbf{i}", [128, 2048], BF16) for i in range(6)])
        obf = Ring(P, [sb(f"obf{i}", [128, S], BF16) for i in range(2)], True)
        of32 = Ring(P, [sb(f"of{i}", [128, S], F32) for i in range(2)], True)
        ovd = Ring(P, [sb(f"ovd{i}", [128, 512], BF16) for i in range(4)], True)
        pbanks = Ring(P, [ps(f"pa{i}", [128, 512]) for i in range(8)])

        for kc in range(16):
            t, b, ds = xst.next()
            P.dma(t[:], xT[kc * 128:(kc + 1) * 128, :], w=[b], dsem=ds)
            eng = "dve" if kc % 2 == 0 else "pool"
            P.op(eng, lambda e, t=t, kc=kc: e.tensor_copy(out=XT[:, kc, :], in_=t[:]), r=[b], w=[XTb[kc]])

        cast_i = [0]

        def load_w(src_ap):
            t, b, ds = wst.next()
            P.dma(t[:], src_ap, w=[b], dsem=ds)
            t2, b2, _ = wbf.next()
            eng = ("dve", "pool")[cast_i[0] % 2]
            cast_i[0] += 1
            P.op(eng, lambda e, t=t, t2=t2: e.tensor_copy(out=t2[:], in_=t[:]), r=[b], w=[b2])
            return t2, b2

        evac_i = [0]
        for blk_i in (nblkA if isinstance(nblkA, (list, tuple)) else range(nblkA)):
            wt, wb = load_w(wA[blk_i].rearrange("p k c -> p (k c)"))
            wv = wt[:].rearrange("p (k c) -> p k c", k=16)
            banks = [pbanks.next() for _ in range(4)]
            for kc in range(16):
                for tb in range(4):
                    pt, pb, _ = banks[tb]
                    P.mm(pt[:], wv[:, kc, :], XT[:, kc, tb * 512:(tb + 1) * 512], kc == 0, kc == 15,
                         r=[wb, XTb[kc]], w=[pb])
            if blk_i < 11:
                kind, dst = "lat", lat[blk_i * 128:(blk_i + 1) * 128, :]
            elif blk_i < 23:
                kind, hd, dst = "qk", blk_i - 11, qd[(blk_i - 11) * 128:(blk_i - 10) * 128, :]
            elif blk_i < 35:
                kind, hd, dst = "qk", blk_i - 23, kd[(blk_i - 23) * 128:(blk_i - 22) * 128, :]
            else:
                kind, dst = "gate", sg[(blk_i - 35) * 128:(blk_i - 34) * 128, :]
            if kind == "gate":
                ot, ob, ods = of32.next()
            else:
                ot, ob, ods = obf.next()
            for tb in range(4):
                pt, pb, _ = banks[tb]
                if kind == "gate":
                    P.op("act", lambda e, pt=pt, ot=ot, tb=tb: e.activation(
                        out=ot[:, tb * 512:(tb + 1) * 512], in_=pt[:], func=AF.Sigmoid), r=[pb], w=[ob])
                    continue
                if kind == "qk" and hd >= 4:
                    dil = 4 if hd < 8 else 16
                    ni = 512 // dil
                    o_ap = ot[:].rearrange("p (r i) -> p r i", r=dil)[:, :, tb * ni:(tb + 1) * ni]
                    i_ap = pt[:].rearrange("p (i r) -> p r i", r=dil)
                else:
                    o_ap = ot[:, tb * 512:(tb + 1) * 512]
                    i_ap = pt[:]
                if evac_i[0] % 2 == 0:
                    P.op("dve", lambda e, o_ap=o_ap, i_ap=i_ap: e.tensor_copy(out=o_ap, in_=i_ap), r=[pb], w=[ob])
                else:
                    P.op("act", lambda e, o_ap=o_ap, i_ap=i_ap: e.activation(out=o_ap, in_=i_ap, func=AF.Copy),
                         r=[pb], w=[ob])
                evac_i[0] += 1
            P.dma(dst, ot[:], r=[ob], dsem=ods)

        for g in (range(3) if doV else ()):
            n, dil = DIL[g]
            wts = [load_w(wV[g * 4 + kq].rearrange("p k c -> p (k c)")) for kq in range(4)]
            for T in range(16):
                r_, blk_ = divmod(T, n // 128)
                pt, pb, _ = pbanks.next()
                for kc in range(16):
                    wt, wb = wts[kc // 4]
                    rhs = wt[:].rearrange("p (k c) -> p k c", k=4)[:, kc % 4, :]
                    lhsT = XT[:, kc, :].rearrange("p (i r) -> p r i", r=dil)[:, r_, blk_ * 128:(blk_ + 1) * 128]
                    P.mm(pt[:], lhsT, rhs, kc == 0, kc == 15, r=[wb, XTb[kc]], w=[pb])
                ot, ob, ods = ovd.next()
                if T % 2 == 0:
                    P.op("dve", lambda e, ot=ot, pt=pt: e.tensor_copy(out=ot[:], in_=pt[:]), r=[pb], w=[ob])
                else:
                    P.op("act", lambda e, ot=ot, pt=pt: e.activation(out=ot[:], in_=pt[:], func=AF.Copy),
                         r=[pb], w=[ob])
                P.dma(vd[g, T * 128:(T + 1) * 128, :], ot[:], r=[ob], dsem=ods)
        P.finalize()
    if stop_after == "A":
        return nc

    wUQ = din("wUQ", [16, 128, 6 * 384])
    gq = din("gq", [128, 6])
    wUK = din("wUK", [16, 128, 4 * 128])
    wUV = din("wUV", [4, 128, 2048])
    gkv = din("gkv", [128, 4])
    cosT = din("cosT", [64, S])
    sinT = din("sinT", [64, S])
    maskc = din("maskc", [128, 128], BF16)
    qm = dscr("qm", [16, 192, S], BF16)
    km = dscr("km", [16, 128, S], BF16)
    krot = dscr("krot", [64, S], BF16)
    vm = dscr("vm", [S, 2048], BF16)
    om = dscr("om", [2048, S], BF16)

    es = ExitStack()
    with es:
        LAT = sb("LAT", [128, 10, S], BF16)
        LATb = [Buf() for _ in range(10)]
        KPE = sb("KPE", [64, S], BF16)
        KPEP = sb("KPEP", [64, S], BF16)
        COS = sb("COS", [64, S], F32)
        SIN = sb("SIN", [64, S], F32)
        GQ = sb("GQ", [128, 6], F32)
        GKV = sb("GKV", [128, 4], F32)
        ONESF = sb("ONESF", [128, 128], F32)
        cb_ = Buf()
        es1 = ExitStack()
        with es1:
            sb1 = lambda n, sh, dt: es1.enter_context(nc.sbuf_tensor(n, list(sh), dt))
            ps1 = lambda n, sh, dt=F32: es1.enter_context(nc.psum_tensor(n, list(sh), dt))
            P = Phase(nc, "B1")
            P.dma(LAT[:], lat[0:1280, :].rearrange("(k p) s -> p k s", p=128), w=LATb, dsem=P.dsem())
            P.dma(KPE[:], lat[1280:1344, :], w=[cb_], dsem=P.dsem())
            P.dma(KPEP[:], lat[1344:1408, :], w=[cb_], dsem=P.dsem())
            P.dma(COS[:], cosT, w=[cb_], dsem=P.dsem())
            P.dma(SIN[:], sinT, w=[cb_], dsem=P.dsem())
            P.dma(GQ[:], gq, w=[cb_], dsem=P.dsem())
            P.dma(GKV[:], gkv, w=[cb_], dsem=P.dsem())
            P.op("pool", lambda e: e.memset(ONESF[:], 1.0), w=[cb_])
            sq = Ring(P, [sb1(f"sq{i}", [128, 512], F32) for i in range(3)])
            rr = Ring(P, [sb1(f"rr{i}", [128, 512], F32) for i in range(2)])
            pss = Ring(P, [ps1(f"pss{i}", [128, 512]) for i in range(2)])
            k_ = 0
            for (c0, ncn, nfeat) in ((0, 6, 768), (6, 4, 512)):
                for tb in range(4):
                    pt, pb, _ = pss.next()
                    for kc in range(ncn):
                        st, sbuf_, _ = sq.next()
                        src = LAT[:, c0 + kc, tb * 512:(tb + 1) * 512]
                        if k_ % 2 == 0:
                            P.op("act", lambda e, st=st, src=src: e.activation(out=st[:], in_=src, func=AF.Square),
                                 r=[LATb[c0 + kc]], w=[sbuf_])
                        else:
                            P.op("dve", lambda e, st=st, src=src: e.tensor_tensor(out=st[:], in0=src, in1=src, op=ALU.mult),
                                 r=[LATb[c0 + kc]], w=[sbuf_])
                        k_ += 1
                        P.mm(pt[:], ONESF[:], st[:], kc == 0, kc == ncn - 1, r=[sbuf_, cb_], w=[pb])
                    rt, rb, _ = rr.next()
                    P.op("act", lambda e, rt=rt, pt=pt, nfeat=nfeat: e.activation(
                        out=rt[:], in_=pt[:], func=AF.Sqrt, bias=RMS_EPS, scale=1.0 / nfeat), r=[pb], w=[rb])
                    P.op("dve", lambda e, rt=rt: e.reciprocal(out=rt[:], in_=rt[:]), r=[rb], w=[rb])
                    for kc in range(ncn):
                        src = LAT[:, c0 + kc, tb * 512:(tb + 1) * 512]
                        eng = "dve" if kc % 2 == 0 else "pool"
                        P.op(eng, lambda e, src=src, rt=rt: e.tensor_tensor(out=src, in0=src, in1=rt[:], op=ALU.mult),
                             r=[rb, LATb[c0 + kc]], w=[LATb[c0 + kc]])
            WV = sb1("WV", [128, 4, 2048], BF16)
            WVb = Buf()
            wvs = Ring(P, [sb1(f"wvs{i}", [128, 2048], F32) for i in range(2)], True)
            for kc in range(4):
                t, b, ds = wvs.next()
                P.dma(t[:], wUV[kc], w=[b], dsem=ds)
                P.op("dve" if kc % 2 == 0 else "pool", lambda e, t=t, kc=kc: e.tensor_scalar(
                    out=WV[:, kc, :], in0=t[:], scalar1=GKV[:, kc:kc + 1], scalar2=None, op0=ALU.mult),
                    r=[b, cb_], w=[WVb])
            pv = Ring(P, [ps1(f"pv{i}", [128, 512]) for i in range(4)])
            vt = Ring(P, [sb1(f"vt{i}", [128, 2048], BF16) for i in range(2)], True)
            ev = 0
            for t in range(16):
                ot, ob, ods = vt.next()
                for hb in range(4):
                    pt, pb, _ = pv.next()
                    for kc in range(4):
                        P.mm(pt[:], LAT[:, 6 + kc, t * 128:(t + 1) * 128], WV[:, kc, hb * 512:(hb + 1) * 512],
                             kc == 0, kc == 3, r=[LATb[6 + kc], WVb], w=[pb])
                    o_ap = ot[:, hb * 512:(hb + 1) * 512]
                    if ev % 2 == 0:
                        P.op("dve", lambda e, o_ap=o_ap, pt=pt: e.tensor_copy(out=o_ap, in_=pt[:]), r=[pb], w=[ob])
                    else:
                        P.op("act", lambda e, o_ap=o_ap, pt=pt: e.activation(out=o_ap, in_=pt[:], func=AF.Copy), r=[pb], w=[ob])
                    ev += 1
                P.dma(vm[t * 128:(t + 1) * 128, :], ot[:], r=[ob], dsem=ods)
            wks = Ring(P, [sb1(f"wks{i}", [128, 512], F32) for i in range(2)], True)
            wkb = Ring(P, [sb1(f"wkb{i}", [128, 512], BF16) for i in range(2)])
            kn = Ring(P, [sb1(f"kn{i}", [128, S], BF16) for i in range(2)], True)
            for h in range(16):
                t, b, ds = wks.next()
                P.dma(t[:], wUK[h], w=[b], dsem=ds)
                t2, b2, _ = wkb.next()
                for kc in range(4):
                    P.op("pool" if kc % 2 == 0 else "dve", lambda e, t=t, t2=t2, kc=kc: e.tensor_scalar(
                        out=t2[:, kc * 128:(kc + 1) * 128], in0=t[:, kc * 128:(kc + 1) * 128],
                        scalar1=GKV[:, kc:kc + 1], scalar2=None, op0=ALU.mult), r=[b, cb_], w=[b2])
                ot, ob, ods = kn.next()
                for tb in range(4):
                    pt, pb, _ = pv.next()
                    for kc in range(4):
                        P.mm(pt[:], t2[:, kc * 128:(kc + 1) * 128], LAT[:, 6 + kc, tb * 512:(tb + 1) * 512],
                             kc == 0, kc == 3, r=[b2, LATb[6 + kc]], w=[pb])
                    o_ap = ot[:, tb * 512:(tb + 1) * 512]
                    if ev % 2 == 0:
                        P.op("dve", lambda e, o_ap=o_ap, pt=pt: e.tensor_copy(out=o_ap, in_=pt[:]), r=[pb], w=[ob])
                    else:
                        P.op("act", lambda e, o_ap=o_ap, pt=pt: e.activation(out=o_ap, in_=pt[:], func=AF.Copy), r=[pb], w=[ob])
                    ev += 1
                P.dma(km[h], ot[:], r=[ob], dsem=ods)
            P.finalize()
        if stop_after == "B1":
            return nc
        es2 = ExitStack()
        with es2:
            sb1 = lambda n, sh, dt: es2.enter_context(nc.sbuf_tensor(n, list(sh), dt))
            ps1 = lambda n, sh, dt=F32: es2.enter_context(nc.psum_tensor(n, list(sh), dt))
            P = Phase(nc, "B2")
            tmp = Ring(P, [sb1(f"tmp{i}", [64, 512], F32) for i in range(4)])
            KR = sb1("KR", [64, S], BF16)
            KRb = Buf()
            for tb in range(4):
                sl = slice(tb * 512, (tb + 1) * 512)
                t1, b1_, _ = tmp.next()
                t2, b2_, _ = tmp.next()
                P.op("dve", lambda e, t1=t1, sl=sl: e.tensor_tensor(out=t1[:], in0=KPE[:, sl], in1=COS[:, sl], op=ALU.mult), w=[b1_])
                P.op("pool", lambda e, t2=t2, sl=sl: e.tensor_tensor(out=t2[:], in0=KPEP[:, sl], in1=SIN[:, sl], op=ALU.mult), w=[b2_])
                P.op("dve", lambda e, t1=t1, t2=t2, sl=sl: e.tensor_tensor(out=KR[:, sl], in0=t1[:], in1=t2[:], op=ALU.add),
                     r=[b1_, b2_], w=[KRb])
            P.dma(krot, KR[:], r=[KRb], dsem=P.dsem())
            wqs = Ring(P, [sb1(f"wqs{i}", [128, 2304], F32) for i in range(2)], True)
            wqb = Ring(P, [sb1(f"wqb{i}", [128, 2304], BF16) for i in range(2)])
            qn = Ring(P, [sb1(f"qn{i}", [128, S], BF16) for i in range(2)], True)
            qr = Ring(P, [sb1(f"qr{i}", [64, S], BF16) for i in range(2)], True)
            pqn = Ring(P, [ps1(f"pqn{i}", [128, 512]) for i in range(2)])
            pqa = Ring(P, [ps1(f"pqa{i}", [128, 512]) for i in range(2)])
            pqb = Ring(P, [ps1(f"pqb{i}", [128, 512]) for i in range(2)])
            ev = 0
            for h in range(nhB2):
                t, b, ds = wqs.next()
                P.dma(t[:], wUQ[h], w=[b], dsem=ds)
                t2, b2, _ = wqb.next()
                for kc in range(6):
                    P.op("pool" if kc % 2 == 0 else "dve", lambda e, t=t, t2=t2, kc=kc: e.tensor_scalar(
                        out=t2[:, kc * 384:(kc + 1) * 384], in0=t[:, kc * 384:(kc + 1) * 384],
                        scalar1=GQ[:, kc:kc + 1], scalar2=None, op0=ALU.mult), r=[b], w=[b2])
                w3 = t2[:].rearrange("p (k c) -> p k c", k=6)
                qnt, qnb, qnd = qn.next()
                qrt, qrb, qrd = qr.next()
                for tb in range(4):
                    sl = slice(tb * 512, (tb + 1) * 512)
                    p1, pb1, _ = pqn.next()
                    p2, pb2, _ = pqa.next()
                    p3, pb3, _ = pqb.next()
                    for kc in range(6):
                        P.mm(p1[:], w3[:, kc, 0:128], LAT[:, kc, sl], kc == 0, kc == 5, r=[b2, LATb[kc]], w=[pb1])
                    for kc in range(6):
                        P.mm(p2[:], w3[:, kc, 128:256], LAT[:, kc, sl], kc == 0, kc == 5, r=[b2, LATb[kc]], w=[pb2])
                    for kc in range(6):
                        P.mm(p3[:], w3[:, kc, 256:384], LAT[:, kc, sl], kc == 0, kc == 5, r=[b2, LATb[kc]], w=[pb3])
                    P.op("act", lambda e, qnt=qnt, p1=p1, sl=sl: e.activation(out=qnt[:, sl], in_=p1[:], func=AF.Copy),
                         r=[pb1], w=[qnb])
                    t1, b1_, _ = tmp.next()
                    t2_, b2_, _ = tmp.next()
                    P.op("dve", lambda e, t1=t1, p2=p2, sl=sl: e.tensor_tensor(out=t1[:], in0=p2[0:64, :], in1=COS[:, sl], op=ALU.mult),
                         r=[pb2], w=[b1_])
                    P.op("dve", lambda e, t2_=t2_, p3=p3, sl=sl: e.tensor_tensor(out=t2_[:], in0=p3[0:64, :], in1=SIN[:, sl], op=ALU.mult),
                         r=[pb3], w=[b2_])
                    P.op("pool", lambda e, t1=t1, t2_=t2_, qrt=qrt, sl=sl: e.tensor_tensor(out=qrt[:, sl], in0=t1[:], in1=t2_[:], op=ALU.add),
                         r=[b1_, b2_], w=[qrb])
                P.dma(qm[h, 0:128, :], qnt[:], r=[qnb], dsem=qnd)
                P.dma(qm[h, 128:192, :], qrt[:], r=[qrb], dsem=qrd)
            P.finalize()
    if stop_after == "B":
        return nc

    es = ExitStack()
    with es:
        P = Phase(nc, "C")
        VALL = sb("VALL", [128, 16, 2048], BF16)
        KROT = sb("KROT", [64, S], BF16)
        MASKC = sb("MASKC", [128, 128], BF16)
        ONESB = sb("ONESB", [128, 128], BF16)
        cb_ = Buf()
        P.dma(VALL[:], vm.rearrange("(t p) c -> p t c", p=128), w=[cb_], dsem=P.dsem())
        P.dma(KROT[:], krot, w=[cb_], dsem=P.dsem())
        P.dma(MASKC[:], maskc, w=[cb_], dsem=P.dsem())
        P.op("pool", lambda e: e.memset(ONESB[:], 1.0), w=[cb_])
        qn = Ring(P, [sb(f"cqn{i}", [128, S], BF16) for i in range(2)], True)
        qr = Ring(P, [sb(f"cqr{i}", [64, S], BF16) for i in range(2)], True)
        kn = Ring(P, [sb(f"ckn{i}", [128, S], BF16) for i in range(2)], True)
        pr = Ring(P, [sb(f"cp{i}", [128, 512], BF16) for i in range(3)])
        oh = Ring(P, [sb(f"coh{i}", [128, S], BF16) for i in range(2)], True)
        rz = Ring(P, [sb(f"crz{i}", [128, 512], F32) for i in range(2)])
        pS = Ring(P, [ps(f"cS{i}", [128, 512]) for i in range(2)])
        pO = Ring(P, [ps(f"cO{i}", [128, 512]) for i in range(2)])
        pZ = Ring(P, [ps(f"cZ{i}", [128, 512]) for i in range(2)])
        sc_mla = 192.0 ** -0.5
        for h in range(16):
            qnt, qnb, d1 = qn.next()
            qrt, qrb, d2 = qr.next()
            knt, knb, d3 = kn.next()
            P.dma(qnt[:], qm[h, 0:128, :], w=[qnb], dsem=d1)
            P.dma(qrt[:], qm[h, 128:192, :], w=[qrb], dsem=d2)
            P.dma(knt[:], km[h], w=[knb], dsem=d3)
            oht, ohb, ohd = oh.next()
            for Q in range(4):
                po, pob, _ = pO.next()
                pz, pzb, _ = pZ.next()
                nj = 4 * Q + 4
                for j in range(nj):
                    q0 = max(512 * Q, 128 * j)
                    wd = 512 * Q + 512 - q0
                    c0 = q0 - 512 * Q
                    pst, psb, _ = pS.next()
                    P.mm(pst[:, 0:wd], knt[:, j * 128:(j + 1) * 128], qnt[:, q0:q0 + wd], True, False, r=[knb, qnb], w=[psb])
                    P.mm(pst[:, 0:wd], KROT[:, j * 128:(j + 1) * 128], qrt[:, q0:q0 + wd], False, True, r=[cb_, qrb], w=[psb])
                    pt, ptb, _ = pr.next()
                    P.op("act", lambda e, pt=pt, pst=pst, wd=wd: e.activation(out=pt[:, 0:wd], in_=pst[:, 0:wd], func=AF.Exp, scale=sc_mla),
                         r=[psb], w=[ptb])
                    if j >= 4 * Q:
                        P.op("pool", lambda e, pt=pt: e.tensor_tensor(out=pt[:, 0:128], in0=pt[:, 0:128], in1=MASKC[:], op=ALU.mult),
                             r=[ptb, cb_], w=[ptb])
                    P.mm(po[:, c0:c0 + wd], VALL[:, j, h * 128:(h + 1) * 128], pt[:, 0:wd], j == 0, j == nj - 1, r=[cb_, ptb], w=[pob])
                    P.mm(pz[:, c0:c0 + wd], ONESB[:], pt[:, 0:wd], j == 0, j == nj - 1, r=[cb_, ptb], w=[pzb])
                rt, rb, _ = rz.next()
                P.op("dve", lambda e, rt=rt, pz=pz: e.reciprocal(out=rt[:], in_=pz[:]), r=[pzb], w=[rb])
                P.op("dve", lambda e, oht=oht, po=po, rt=rt, Q=Q: e.tensor_tensor(
                    out=oht[:, Q * 512:(Q + 1) * 512], in0=po[:], in1=rt[:], op=ALU.mult), r=[pob, rb], w=[ohb])
            P.dma(om[h * 128:(h + 1) * 128, :], oht[:], r=[ohb], dsem=ohd)
        P.finalize()
    if stop_after == "C":
        return nc

    dmask = din("dmask", [128, 20, 128])
    od = dscr("od", [1536, S], BF16)
    es = ExitStack()
    with es:
        P = Phase(nc, "D")
        VD = sb("VD", [128, 3, 16, 512], BF16)
        MASKS = sb("MASKS", [128, 20, 128], F32)
        ONESB = sb("ONESBd", [128, 128], BF16)
        cb_ = Buf()
        for g in range(3):
            P.dma(VD[:, g], vd[g].rearrange("(t p) c -> p t c", p=128), w=[cb_], dsem=P.dsem())
        P.dma(MASKS[:], dmask, w=[cb_], dsem=P.dsem())
        P.op("pool", lambda e: e.memset(ONESB[:], 1.0), w=[cb_])
        UN = [sb(f"UN{g}", [128, S], F32) for g in range(3)]
        ZN = [sb(f"ZN{g}", [128, S], F32) for g in range(3)]
        UNb = [Buf() for _ in range(3)]
        ZNb = [Buf() for _ in range(3)]
        RT = sb("RTd", [128, S], F32)
        RTb = Buf()
        qdr = Ring(P, [sb(f"dq{i}", [128, S], BF16) for i in range(2)], True)
        kdr = Ring(P, [sb(f"dk{i}", [128, S], BF16) for i in range(2)], True)
        ecr = Ring(P, [sb(f"dec{i}", [128, 512], F32) for i in range(2)])
        epr = Ring(P, [sb(f"dep{i}", [128, 512], F32) for i in range(2)])
        pcr = Ring(P, [sb(f"dpc{i}", [128, 512], BF16) for i in range(2)])
        ppr = Ring(P, [sb(f"dpp{i}", [128, 512], BF16) for i in range(2)])
        odr = Ring(P, [sb(f"dod{i}", [128, S], BF16) for i in range(2)], True)
        pSc = Ring(P, [ps(f"dSc{i}", [128, 512]) for i in range(2)])
        pSp = Ring(P, [ps(f"dSp{i}", [128, 512]) for i in range(2)])
        pU = Ring(P, [ps(f"dU{i}", [128, 512]) for i in range(2)])
        pZ = Ring(P, [ps(f"dZ{i}", [128, 512]) for i in range(2)])
        sc_d = 128.0 ** -0.5
        for hs in range(4):
            for g in range(3):
                hd = g * 4 + hs
                n, dil = DIL[g]
                nb = n // 128
                qt, qb, d1 = qdr.next()
                kt, kb, d2 = kdr.next()
                P.dma(qt[:], qd[hd * 128:(hd + 1) * 128, :], w=[qb], dsem=d1)
                P.dma(kt[:], kd[hd * 128:(hd + 1) * 128, :], w=[kb], dsem=d2)
                for c in range(4):
                    us = [4 * c + s_ for s_ in range(4)]
                    hp = [u % nb != 0 for u in us]
                    s0 = hp.index(True) if any(hp) else 4
                    assert all(hp[s0:])
                    sc, scb, _ = pSc.next()
                    for s_, u in enumerate(us):
                        P.mm(sc[:, s_ * 128:(s_ + 1) * 128], kt[:, u * 128:(u + 1) * 128], qt[:, u * 128:(u + 1) * 128],
                             True, True, r=[kb, qb], w=[scb])
                    ec, ecb, _ = ecr.next()
                    P.op("act", lambda e, ec=ec, sc=sc: e.activation(out=ec[:], in_=sc[:], func=AF.Exp, scale=sc_d), r=[scb], w=[ecb])
                    pc, pcb, _ = pcr.next()
                    P.op("dve", lambda e, pc=pc, ec=ec, hd=hd: e.tensor_tensor(
                        out=pc[:].rearrange("p (s i) -> p s i", s=4), in0=ec[:].rearrange("p (s i) -> p s i", s=4),
                        in1=MASKS[:, hd:hd + 1, :].to_broadcast([128, 4, 128]), op=ALU.mult), r=[ecb, cb_], w=[pcb])
                    if s0 < 4:
                        sp, spb, _ = pSp.next()
                        for s_ in range(s0, 4):
                            u = us[s_]
                            P.mm(sp[:, s_ * 128:(s_ + 1) * 128], kt[:, (u - 1) * 128:u * 128], qt[:, u * 128:(u + 1) * 128],
                                 True, True, r=[kb, qb], w=[spb])
                        ep, epb, _ = epr.next()
                        P.op("act", lambda e, ep=ep, sp=sp, s0=s0: e.activation(
                            out=ep[:, s0 * 128:512], in_=sp[:, s0 * 128:512], func=AF.Exp, scale=sc_d), r=[spb], w=[epb])
                        pp, ppb, _ = ppr.next()
                        ns = 4 - s0
                        P.op("dve", lambda e, pp=pp, ep=ep, hd=hd, s0=s0, ns=ns: e.tensor_tensor(
                            out=pp[:, s0 * 128:512].rearrange("p (s i) -> p s i", s=ns),
                            in0=ep[:, s0 * 128:512].rearrange("p (s i) -> p s i", s=ns),
                            in1=MASKS[:, 12 + hd:13 + hd, :].to_broadcast([128, ns, 128]), op=ALU.mult), r=[epb, cb_], w=[ppb])
                    pu, pub, _ = pU.next()
                    pz, pzb, _ = pZ.next()
                    for s_, u in enumerate(us):
                        sl = slice(s_ * 128, (s_ + 1) * 128)
                        P.mm(pu[:, sl], VD[:, g, u, hs * 128:(hs + 1) * 128], pc[:, sl], True, not hp[s_], r=[cb_, pcb], w=[pub])
                        if hp[s_]:
                            P.mm(pu[:, sl], VD[:, g, u - 1, hs * 128:(hs + 1) * 128], pp[:, sl], False, True, r=[cb_, ppb], w=[pub])
                    for s_, u in enumerate(us):
                        sl = slice(s_ * 128, (s_ + 1) * 128)
                        P.mm(pz[:, sl], ONESB[:], pc[:, sl], True, not hp[s_], r=[cb_, pcb], w=[pzb])
                        if hp[s_]:
                            P.mm(pz[:, sl], ONESB[:], pp[:, sl], False, True, r=[cb_, ppb], w=[pzb])

                    def nat(t):
                        if g == 0:
                            return t[:, c * 512:(c + 1) * 512], None
                        if g == 1:
                            return t[:].rearrange("p (i r) -> p r i", r=4)[:, c, :], None
                        return t[:].rearrange("p (i r) -> p r i", r=16)[:, 4 * c:4 * c + 4, :], 4

                    uo, rs_ = nat(UN[g])
                    zo, _ = nat(ZN[g])
                    ui = pu[:] if rs_ is None else pu[:].rearrange("p (r i) -> p r i", r=4)
                    zi = pz[:] if rs_ is None else pz[:].rearrange("p (r i) -> p r i", r=4)
                    P.op("act", lambda e, uo=uo, ui=ui: e.activation(out=uo, in_=ui, func=AF.Copy), r=[pub], w=[UNb[g]])
                    P.op("dve", lambda e, zo=zo, zi=zi: e.tensor_copy(out=zo, in_=zi), r=[pzb], w=[ZNb[g]])
            P.op("pool", lambda e: e.tensor_tensor(out=RT[:], in0=ZN[0][:], in1=ZN[1][:], op=ALU.add), r=[ZNb[0], ZNb[1]], w=[RTb])
            P.op("pool", lambda e: e.tensor_tensor(out=RT[:], in0=RT[:], in1=ZN[2][:], op=ALU.add), r=[RTb, ZNb[2]], w=[RTb])
            P.op("dve", lambda e: e.reciprocal(out=RT[:], in_=RT[:]), r=[RTb], w=[RTb])
            for g in range(3):
                ot, ob, ods = odr.next()
                P.op("pool" if g != 1 else "dve", lambda e, ot=ot, g=g: e.tensor_tensor(out=ot[:], in0=UN[g][:], in1=RT[:], op=ALU.mult),
                     r=[UNb[g], RTb], w=[ob])
                P.dma(od[(g * 4 + hs) * 128:(g * 4 + hs + 1) * 128, :], ot[:], r=[ob], dsem=ods)
        P.finalize()
    if stop_after == "D":
        return nc

    wOM = din("wOM", [16, 128, 2048])
    wOD = din("wOD", [16, 128, 1536])
    p1s = dscr("p1s", [2048, S], F32)
    mgd = dscr("mgd", [2048, S], BF16) if "mgd" in debug else None
    es = ExitStack()
    with es:
        P = Phase(nc, "E1a")
        OM = sb("OM", [128, 16, S], BF16)
        OMb = Buf()
        P.dma(OM[:], om.rearrange("(k p) s -> p k s", p=128), w=[OMb], dsem=P.dsem())
        wst = Ring(P, [sb(f"e1ws{i}", [128, 2048], F32) for i in range(3)], True)
        wbf = Ring(P, [sb(f"e1wb{i}", [128, 2048], BF16) for i in range(3)])
        sgr = Ring(P, [sb(f"e1sg{i}", [128, S], F32) for i in range(2)], True)
        otr = Ring(P, [sb(f"e1o{i}", [128, S], F32) for i in range(2)], True)
        pY = Ring(P, [ps(f"e1p{i}", [128, 512]) for i in range(4)])
        for m in range(16):
            t, b, ds = wst.next()
            P.dma(t[:], wOM[m], w=[b], dsem=ds)
            wt, wb, _ = wbf.next()
            P.op("pool", lambda e, t=t, wt=wt: e.tensor_copy(out=wt[:], in_=t[:]), r=[b], w=[wb])
            st, sbf, sds = sgr.next()
            P.dma(st[:], sg[m * 128:(m + 1) * 128, :], w=[sbf], dsem=sds)
            ot, ob, ods = otr.next()
            for tb in range(4):
                sl = slice(tb * 512, (tb + 1) * 512)
                pt, pb, _ = pY.next()
                for kc in range(16):
                    P.mm(pt[:], wt[:, kc * 128:(kc + 1) * 128], OM[:, kc, sl], kc == 0, kc == 15, r=[wb, OMb], w=[pb])
                P.op("dve", lambda e, ot=ot, pt=pt, st=st, sl=sl: e.tensor_tensor(out=ot[:, sl], in0=pt[:], in1=st[:, sl], op=ALU.mult),
                     r=[pb, sbf], w=[ob])
            P.dma(p1s[m * 128:(m + 1) * 128, :], ot[:], r=[ob], dsem=ods)
        P.finalize()
    esMG = ExitStack()
    MG = esMG.enter_context(nc.sbuf_tensor("MG", [128, 16, S], BF16))
    es = ExitStack()
    with es:
        P = Phase(nc, "E1b")
        ODs = sb("ODs", [128, 12, S], BF16)
        ODb = Buf()
        MGb = Buf()
        P.dma(ODs[:], od.rearrange("(k p) s -> p k s", p=128), w=[ODb], dsem=P.dsem())
        wst = Ring(P, [sb(f"e2ws{i}", [128, 1536], F32) for i in range(3)], True)
        wbf = Ring(P, [sb(f"e2wb{i}", [128, 1536], BF16) for i in range(3)])
        sgr = Ring(P, [sb(f"e2sg{i}", [128, S], F32) for i in range(2)], True)
        p1r = Ring(P, [sb(f"e2p1{i}", [128, S], F32) for i in range(2)], True)
        tmr = Ring(P, [sb(f"e2t{i}", [128, 512], F32) for i in range(3)])
        pY = Ring(P, [ps(f"e2p{i}", [128, 512]) for i in range(4)])
        for m in range(16):
            t, b, ds = wst.next()
            P.dma(t[:], wOD[m], w=[b], dsem=ds)
            wt, wb, _ = wbf.next()
            P.op("pool", lambda e, t=t, wt=wt: e.tensor_copy(out=wt[:], in_=t[:]), r=[b], w=[wb])
            st, sbf, sds = sgr.next()
            P.dma(st[:], sg[2048 + m * 128:2048 + (m + 1) * 128, :], w=[sbf], dsem=sds)
            p1t, p1b, p1d = p1r.next()
            P.dma(p1t[:], p1s[m * 128:(m + 1) * 128, :], w=[p1b], dsem=p1d)
            for tb in range(4):
                sl = slice(tb * 512, (tb + 1) * 512)
                pt, pb, _ = pY.next()
                for kc in range(12):
                    P.mm(pt[:], wt[:, kc * 128:(kc + 1) * 128], ODs[:, kc, sl], kc == 0, kc == 11, r=[wb, ODb], w=[pb])
                tt, tbf, _ = tmr.next()
                P.op("dve", lambda e, tt=tt, pt=pt, st=st, sl=sl: e.tensor_tensor(out=tt[:], in0=pt[:], in1=st[:, sl], op=ALU.mult),
                     r=[pb, sbf], w=[tbf])
                P.op("pool", lambda e, tt=tt, p1t=p1t, sl=sl, m=m: e.tensor_tensor(out=MG[:, m, sl], in0=tt[:], in1=p1t[:, sl], op=ALU.add),
                     r=[tbf, p1b], w=[MGb])
        if mgd is not None:
            P.dma(mgd.rearrange("(k p) s -> p k s", p=128), MG[:], r=[MGb], dsem=P.dsem())
        P.finalize()
    if stop_after == "E1":
        esMG.close()
        return nc

    wOUT = din("wOUT", [16, 128, 2048])
    hs_ = dscr("hs", [S, D], F32)
    es = ExitStack()
    with es:
        P = Phase(nc, "E2a")
        MGb = Buf()
        WOh = sb("WOh", [128, 16, 1024], BF16)
        WOb = [Buf() for _ in range(16)]
        wst = Ring(P, [sb(f"e3ws{i}", [128, 1024], F32) for i in range(3)], True)
        xr = Ring(P, [sb(f"e3x{i}", [128, 1024], F32) for i in range(2)], True)
        hr = Ring(P, [sb(f"e3h{i}", [128, 1024], F32) for i in range(2)], True)
        pH = Ring(P, [ps(f"e3p{i}", [128, 512]) for i in range(4)])
        ci = 0
        for half in range(2):
            hsl = slice(half * 1024, (half + 1) * 1024)
            for kc in range(16):
                t, b, ds = wst.next()
                P.dma(t[:], wOUT[kc][:, hsl], w=[b], dsem=ds)
                eng = ("dve", "pool")[ci % 2]
                ci += 1
                P.op(eng, lambda e, t=t, kc=kc: e.tensor_copy(out=WOh[:, kc, :], in_=t[:]), r=[b], w=[WOb[kc]])
            for t_ in range(16):
                xt, xb, xd = xr.next()
                P.dma(xt[:], x_tm[t_ * 128:(t_ + 1) * 128, hsl], w=[xb], dsem=xd)
                ht, hb, hd_ = hr.next()
                for cb in range(2):
                    csl = slice(cb * 512, (cb + 1) * 512)
                    pt, pb, _ = pH.next()
                    for kc in range(16):
                        P.mm(pt[:], MG[:, kc, t_ * 128:(t_ + 1) * 128], WOh[:, kc, csl], kc == 0, kc == 15,
                             r=[MGb, WOb[kc]], w=[pb])
                    P.op("dve", lambda e, ht=ht, xt=xt, pt=pt, csl=csl: e.scalar_tensor_tensor(
                        out=ht[:, csl], in0=xt[:, csl], scalar=DN_ALPHA, in1=pt[:], op0=ALU.mult, op1=ALU.add),
                        r=[xb, pb], w=[hb])
                P.dma(hs_[t_ * 128:(t_ + 1) * 128, hsl], ht[:], r=[hb], dsem=hd_)
        P.finalize()
    esMG.close()
    if stop_after == "E2a":
        return nc

    ln1g = din("ln1_g", [1, D])
    ln1b = din("ln1_b", [1, D])
    ln2g = din("ln2_g", [1, D])
    ln2b = din("ln2_b", [1, D])
    wR = din("wR", [128, 16 * 32])
    bR = din("bR", [1, 32])
    identF = din("identF", [128, 128])
    identB = din("identB", [128, 128], BF16)
    tri = din("tri", [128, 128], BF16)
    eb1 = din("eb1", [1, 32])
    x1s = dscr("x1s", [S, D], F32)
    NROW = NE * CAP + 128
    DUMMY = NE * CAP
    xg = dscr("xg", [NROW, D], BF16)
    ysc = dscr("ysc", [NROW, D], F32)
    esR = ExitStack()
    DEST = esR.enter_context(nc.sbuf_tensor("DEST", [128, 16, 4], I32))
    GATE = esR.enter_context(nc.sbuf_tensor("GATE", [128, 16, 4], F32))

    def layer_norm(P, src, srcb, dst, dstb, G_, B_, gb_, small, t_):
        st, stb, mv, mvb, rs, rsb = small
        for c4 in range(4):
            P.op("dve", lambda e, c4=c4: e.bn_stats(out=st[:, c4, :], in_=src[:, c4 * 512:(c4 + 1) * 512]), r=[srcb], w=[stb])
        P.op("dve", lambda e: e.bn_aggr(out=mv[:], in_=st[:].rearrange("p a b -> p (a b)")), r=[stb], w=[mvb])
        P.op("dve", lambda e: e.tensor_scalar(out=rs[:], in0=mv[:, 1:2], scalar1=LN_EPS, scalar2=None, op0=ALU.add), r=[mvb], w=[rsb])
        P.op("act", lambda e: e.activation(out=rs[:], in_=rs[:], func=AF.Sqrt), r=[rsb], w=[rsb])
        P.op("dve", lambda e: e.reciprocal(out=rs[:], in_=rs[:]), r=[rsb], w=[rsb])
        P.op("dve", lambda e: e.tensor_scalar(out=dst[:], in0=src[:], scalar1=mv[:, 0:1], scalar2=rs[:, 0:1],
                                              op0=ALU.subtract, op1=ALU.mult), r=[srcb, mvb, rsb], w=[dstb])
        P.op("pool", lambda e: e.tensor_tensor(out=dst[:], in0=dst[:], in1=G_[:], op=ALU.mult), r=[dstb, gb_], w=[dstb])
        P.op("pool", lambda e: e.tensor_tensor(out=dst[:], in0=dst[:], in1=B_[:], op=ALU.add), r=[dstb, gb_], w=[dstb])

    es = ExitStack()
    with es:
        P = Phase(nc, "E2b")
        cb_ = Buf()
        G1 = sb("G1", [128, D], F32)
        B1 = sb("B1", [128, D], F32)
        WR = sb("WR", [128, 16 * 32], F32)
        BR = sb("BR", [128, 32], F32)
        IDF = sb("IDF", [128, 128], F32)
        TRI = sb("TRI", [128, 128], BF16)
        ONESB = sb("ONESBe", [128, 128], BF16)
        EB1 = sb("EB1", [128, 32], F32)
        CNT = sb("CNT", [128, 32], F32)
        MASKB = sb("MASKB", [128, 16, 32], BF16)
        ZR = sb("ZR", [128, D], F32)
        P.dma(G1[:], ln1g.to_broadcast([128, D]), w=[cb_], dsem=P.dsem())
        P.dma(B1[:], ln1b.to_broadcast([128, D]), w=[cb_], dsem=P.dsem())
        P.dma(WR[:], wR, w=[cb_], dsem=P.dsem())
        P.dma(BR[:], bR.to_broadcast([128, 32]), w=[cb_], dsem=P.dsem())
        P.dma(IDF[:], identF, w=[cb_], dsem=P.dsem())
        P.dma(TRI[:], tri, w=[cb_], dsem=P.dsem())
        P.dma(EB1[:], eb1.to_broadcast([128, 32]), w=[cb_], dsem=P.dsem())
        P.op("pool", lambda e: e.memset(ONESB[:], 1.0), w=[cb_])
        cntb = Buf()
        P.op("pool", lambda e: e.memset(CNT[:], 0.0), w=[cntb])
        zrb = Buf()
        P.op("pool", lambda e: e.memset(ZR[:], 0.0), w=[zrb])
        P.dma(ysc[DUMMY:DUMMY + 128, :], ZR[:], r=[zrb], dsem=P.dsem())
        hr = Ring(P, [sb(f"e4h{i}", [128, D], F32) for i in range(2)], True)
        x1r = Ring(P, [sb(f"e4x1{i}", [128, D], F32) for i in range(2)], True)
        xbr = Ring(P, [sb(f"e4xb{i}", [128, D], BF16) for i in range(2)])
        xTr = Ring(P, [sb(f"e4xT{i}", [128, 16 * 128], F32) for i in range(2)])
        pT = Ring(P, [ps(f"e4pT{i}", [128, 512]) for i in range(2)])
        pL = Ring(P, [ps(f"e4pL{i}", [128, 512]) for i in range(2)])
        pPos = Ring(P, [ps(f"e4pP{i}", [128, 512]) for i in range(2)])
        pTot = Ring(P, [ps(f"e4pQ{i}", [128, 512]) for i in range(2)])
        scat_ds = [P.dsem() for _ in range(8)]
        sm = lambda n, sh, dt=F32: (sb(n, sh, dt), Buf())
        ST, STb = sm("e4st", [128, 4, 6])
        MV, MVb = sm("e4mv", [128, 2])
        RS, RSb = sm("e4rs", [128, 1])
        LG, LGb = sm("e4lg", [128, 32])
        T8, T8b = sm("e4t8", [128, 8])
        MK, MKb = sm("e4mk", [128, 32])
        NM, NMb = sm("e4nm", [128, 1])
        EX, EXb = sm("e4ex", [128, 32])
        DEN, DENb = sm("e4den", [128, 1])
        GG, GGb = sm("e4gg", [128, 32])
        PS_, PSb = sm("e4pos", [128, 32])
        VL, VLb = sm("e4vl", [128, 32])
        DV, DVb = sm("e4dv", [128, 32])
        D8, D8b = sm("e4d8", [128, 8])
        DF, DFb = sm("e4df", [128, 4])
        OH, OHb = sm("e4oh", [128, 32])
        destb = Buf()
        gateb = Buf()
        mbb = Buf()
        si = 0
        for t_ in range(16):
            ht, hb, hd_ = hr.next()
            P.dma(ht[:], hs_[t_ * 128:(t_ + 1) * 128, :], w=[hb], dsem=hd_)
            x1t, x1b_, x1d = x1r.next()
            layer_norm(P, ht, hb, x1t, x1b_, G1, B1, cb_, (ST, STb, MV, MVb, RS, RSb), t_)
            P.dma(x1s[t_ * 128:(t_ + 1) * 128, :], x1t[:], r=[x1b_], dsem=x1d)
            xbt, xbb, _ = xbr.next()
            P.op("act", lambda e, xbt=xbt, x1t=x1t: e.activation(out=xbt[:], in_=x1t[:], func=AF.Copy), r=[x1b_], w=[xbb])
            xT, xTb, _ = xTr.next()
            for q4 in range(4):
                pt, pb, _ = pT.next()
                for j in range(4):
                    kc = q4 * 4 + j
                    P.op("pe", lambda e, pt=pt, j=j, kc=kc, x1t=x1t: e.transpose(
                        out=pt[:, j * 128:(j + 1) * 128], in_=x1t[:, kc * 128:(kc + 1) * 128], identity=IDF[:]),
                        r=[x1b_, cb_], w=[pb])
                P.op("act" if q4 % 2 else "dve", (lambda e, xT=xT, pt=pt, q4=q4: e.activation(out=xT[:, q4 * 512:(q4 + 1) * 512], in_=pt[:], func=AF.Copy))
                     if q4 % 2 else (lambda e, xT=xT, pt=pt, q4=q4: e.tensor_copy(out=xT[:, q4 * 512:(q4 + 1) * 512], in_=pt[:])),
                     r=[pb], w=[xTb])
            pl, plb, _ = pL.next()
            for kc in range(16):
                P.mm(pl[:, 0:32], xT[:, kc * 128:(kc + 1) * 128], WR[:, kc * 32:(kc + 1) * 32], kc == 0, kc == 15, r=[xTb, cb_], w=[plb])
            P.op("dve", lambda e, pl=pl: e.tensor_tensor(out=LG[:], in0=pl[:, 0:32], in1=BR[:], op=ALU.add), r=[plb, cb_], w=[LGb])
            P.op("dve", lambda e: e.max(out=T8[:], in_=LG[:]), r=[LGb], w=[T8b])
            P.op("dve", lambda e: e.tensor_scalar(out=MK[:], in0=LG[:], scalar1=T8[:, 3:4], scalar2=None, op0=ALU.is_ge), r=[LGb, T8b], w=[MKb])
            P.op("dve", lambda e: e.tensor_scalar(out=NM[:], in0=T8[:, 0:1], scalar1=-1.0, scalar2=None, op0=ALU.mult), r=[T8b], w=[NMb])
            P.op("act", lambda e: e.activation(out=EX[:], in_=LG[:], func=AF.Exp, bias=NM[:, 0:1], scale=1.0), r=[LGb, NMb], w=[EXb])
            P.op("dve", lambda e: e.tensor_tensor(out=EX[:], in0=EX[:], in1=MK[:], op=ALU.mult), r=[EXb, MKb], w=[EXb])
            P.op("dve", lambda e: e.reduce_sum(out=DEN[:], in_=EX[:], axis=AX.X), r=[EXb], w=[DENb])
            P.op("dve", lambda e: e.reciprocal(out=DEN[:], in_=DEN[:]), r=[DENb], w=[DENb])
            P.op("pool", lambda e, t_=t_: e.tensor_copy(out=MASKB[:, t_, :], in_=MK[:]), r=[MKb], w=[mbb])
            pp_, ppb, _ = pPos.next()
            pq_, pqb, _ = pTot.next()
            P.mm(pp_[:, 0:32], TRI[:], MASKB[:, t_, :], True, True, r=[mbb, cb_], w=[ppb])
            P.mm(pq_[:, 0:32], ONESB[:], MASKB[:, t_, :], True, True, r=[mbb, cb_], w=[pqb])
            P.op("dve", lambda e, pp_=pp_: e.tensor_tensor(out=PS_[:], in0=pp_[:, 0:32], in1=CNT[:], op=ALU.add), r=[ppb, cntb], w=[PSb])
            P.op("dve", lambda e, pq_=pq_: e.tensor_tensor(out=CNT[:], in0=pq_[:, 0:32], in1=CNT[:], op=ALU.add), r=[pqb, cntb], w=[cntb])
            P.op("dve", lambda e: e.tensor_scalar(out=VL[:], in0=PS_[:], scalar1=float(CAP), scalar2=None, op0=ALU.is_lt), r=[PSb], w=[VLb])
            P.op("dve", lambda e: e.tensor_tensor(out=VL[:], in0=VL[:], in1=MK[:], op=ALU.mult), r=[VLb, MKb], w=[VLb])
            P.op("dve", lambda e: e.scalar_tensor_tensor(out=GG[:], in0=EX[:], scalar=DEN[:, 0:1], in1=VL[:], op0=ALU.mult, op1=ALU.mult),
                 r=[EXb, DENb, VLb], w=[GGb])
            P.op("dve", lambda e: e.tensor_tensor(out=DV[:], in0=PS_[:], in1=EB1[:], op=ALU.add), r=[PSb, cb_], w=[DVb])
            P.op("dve", lambda e: e.tensor_tensor(out=DV[:], in0=DV[:], in1=VL[:], op=ALU.mult), r=[DVb, VLb], w=[DVb])
            P.op("dve", lambda e: e.max(out=D8[:], in_=DV[:]), r=[DVb], w=[D8b])
            P.op("dve", lambda e: e.tensor_scalar(out=DF[:], in0=D8[:, 0:4], scalar1=0.0, scalar2=float(DUMMY + 1),
                                                  op0=ALU.is_equal, op1=ALU.mult), r=[D8b], w=[DFb])
            P.op("dve", lambda e: e.scalar_tensor_tensor(out=DF[:], in0=D8[:, 0:4], scalar=-1.0, in1=DF[:], op0=ALU.add, op1=ALU.add),
                 r=[D8b, DFb], w=[DFb])
            P.op("dve", lambda e, t_=t_: e.tensor_copy(out=DEST[:, t_, :], in_=DF[:]), r=[DFb], w=[destb])
            for k in range(4):
                P.op("dve", lambda e, k=k: e.tensor_scalar(out=OH[:], in0=DV[:], scalar1=D8[:, k:k + 1], scalar2=None, op0=ALU.is_equal),
                     r=[DVb, D8b], w=[OHb])
                P.op("dve", lambda e: e.tensor_tensor(out=OH[:], in0=OH[:], in1=GG[:], op=ALU.mult), r=[OHb, GGb], w=[OHb])
                P.op("dve", lambda e, k=k, t_=t_: e.reduce_sum(out=GATE[:, t_, k:k + 1], in_=OH[:], axis=AX.X), r=[OHb], w=[gateb])
            for k in range(4):
                ds = scat_ds[si % 8]
                si += 1
                P.op("pool", lambda e, xbt=xbt, t_=t_, k=k: e.indirect_dma_start(
                    out=xg, out_offset=bass.IndirectOffsetOnAxis(ap=DEST[:, t_, k:k + 1], axis=0),
                    in_=xbt[:], in_offset=None), r=[xbb, destb], w=[], dsem=ds)
        P.finalize()
    if stop_after == "E2":
        esR.close()
        return nc

    w1r = din("w1r", [NE, 16, 128, 4096])
    w2r = din("w2r", [NE, 8, 128, 4096])
    b1r = din("b1r", [128, NE * 32])
    b2 = din("b2", [NE, D])
    es = ExitStack()
    with es:
        P = Phase(nc, "G")
        cb_ = Buf()
        IDB = sb("IDB", [128, 128], BF16)
        B1A = sb("B1A", [128, NE * 32], F32)
        B1L = sb("B1L", [128, NE * 16], F32)
        P.dma(IDB[:], identB, w=[cb_], dsem=P.dsem())
        P.dma(B1A[:], b1r, w=[cb_], dsem=P.dsem())
        P.op("dve", lambda e: e.tensor_scalar(
            out=B1L[:].rearrange("p (e m) -> p e m", e=NE), in0=B1A[:].rearrange("p (e m) -> p e m", e=NE)[:, :, 16:32],
            scalar1=1.0, scalar2=None, op0=ALU.add), r=[cb_], w=[cb_])
        xgr = Ring(P, [sb(f"gx{i}", [128, D], BF16) for i in range(3)], True)
        xgT = Ring(P, [sb(f"gxT{i}", [128, 16, CAP], BF16) for i in range(2)])
        wst = Ring(P, [sb(f"gws{i}", [128, 4096], F32) for i in range(3)], True)
        wbf = Ring(P, [sb(f"gwb{i}", [128, 4096], BF16) for i in range(3)])
        atr = Ring(P, [sb(f"gat{i}", [128, 16, CAP], BF16) for i in range(2)])
        g1r = Ring(P, [sb(f"gg1{i}", [128, CAP], F32) for i in range(2)])
        sgr = Ring(P, [sb(f"gsg{i}", [128, CAP], F32) for i in range(2)])
        l1r = Ring(P, [sb(f"gl1{i}", [128, CAP], F32) for i in range(2)])
        ytr = Ring(P, [sb(f"gy{i}", [128, 256], F32) for i in range(4)], True)
        b2r = Ring(P, [sb(f"gb2{i}", [128, D], F32) for i in range(2)], True)
        pTr = Ring(P, [ps(f"gpT{i}", [128, 1024], BF16) for i in range(2)])
        pG = Ring(P, [ps(f"gpG{i}", [128, 512]) for i in range(2)])
        pLn = Ring(P, [ps(f"gpL{i}", [128, 512]) for i in range(2)])
        pYr = Ring(P, [ps(f"gpY{i}", [128, 512]) for i in range(2)])
        ci = 0
        cast_engs = ("dve", "pool", "act")

        def load_wblk(src):
            nonlocal ci
            t, b, ds = wst.next()
            P.dma(t[:], src, w=[b], dsem=ds)
            wt, wb, _ = wbf.next()
            for hf in range(2):
                eng = cast_engs[ci % 3]
                ci += 1
                hsl = slice(hf * 2048, (hf + 1) * 2048)
                if eng == "act":
                    P.op("act", lambda e, wt=wt, t=t, hsl=hsl: e.activation(out=wt[:, hsl], in_=t[:, hsl], func=AF.Copy), r=[b], w=[wb])
                else:
                    P.op(eng, lambda e, wt=wt, t=t, hsl=hsl: e.tensor_copy(out=wt[:, hsl], in_=t[:, hsl]), r=[b], w=[wb])
            return wt, wb

        for ex in range(NE):
            b2t, b2b, b2d = b2r.next()
            P.dma(b2t[:], b2[ex:ex + 1, :].to_broadcast([128, D]), w=[b2b], dsem=b2d)
            XT_, XTb_, _ = xgT.next()
            for st_ in range(NST):
                xt, xb, xd = xgr.next()
                r0 = ex * CAP + st_ * 128
                P.dma(xt[:], xg[r0:r0 + 128, :], w=[xb], dsem=xd)
                for h8 in range(2):
                    pt, pb, _ = pTr.next()
                    for j in range(8):
                        kc = h8 * 8 + j
                        P.op("pe", lambda e, pt=pt, j=j, kc=kc, xt=xt: e.transpose(
                            out=pt[:, j * 128:(j + 1) * 128], in_=xt[:, kc * 128:(kc + 1) * 128], identity=IDB[:]),
                            r=[xb, cb_], w=[pb])
                    o_ap = XT_[:, h8 * 8:(h8 + 1) * 8, st_ * 128:(st_ + 1) * 128]
                    i_ap = pt[:].rearrange("p (j c) -> p j c", j=8)
                    if h8 == 0:
                        P.op("dve", lambda e, o_ap=o_ap, i_ap=i_ap: e.tensor_copy(out=o_ap, in_=i_ap), r=[pb], w=[XTb_])
                    else:
                        P.op("act", lambda e, o_ap=o_ap, i_ap=i_ap: e.activation(out=o_ap, in_=i_ap, func=AF.Copy), r=[pb], w=[XTb_])
            AT, ATb, _ = atr.next()
            for m in range(16):
                wt, wb = load_wblk(w1r[ex, m])
                w3 = wt[:].rearrange("p (k c) -> p k c", k=16)
                pg, pgb, _ = pG.next()
                pl, plb, _ = pLn.next()
                for kc in range(16):
                    P.mm(pg[:, 0:CAP], w3[:, kc, 0:128], XT_[:, kc, :], kc == 0, kc == 15, r=[wb, XTb_], w=[pgb])
                for kc in range(16):
                    P.mm(pl[:, 0:CAP], w3[:, kc, 128:256], XT_[:, kc, :], kc == 0, kc == 15, r=[wb, XTb_], w=[plb])
                g1, g1b, _ = g1r.next()
                sgt, sgb, _ = sgr.next()
                l1, l1b, _ = l1r.next()
                bg = B1A[:, ex * 32 + m:ex * 32 + m + 1]
                bl = B1L[:, ex * 16 + m:ex * 16 + m + 1]
                P.op("dve", lambda e, g1=g1, pg=pg, bg=bg: e.tensor_scalar(out=g1[:], in0=pg[:, 0:CAP], scalar1=bg, scalar2=7.0,
                                                                            op0=ALU.add, op1=ALU.min), r=[pgb, cb_], w=[g1b])
                P.op("act", lambda e, sgt=sgt, g1=g1: e.activation(out=sgt[:], in_=g1[:], func=AF.Sigmoid, scale=1.702), r=[g1b], w=[sgb])
                P.op("dve", lambda e, l1=l1, pl=pl, bl=bl: e.tensor_scalar(out=l1[:], in0=pl[:, 0:CAP], scalar1=bl, scalar2=-6.0,
                                                                            op0=ALU.add, op1=ALU.max), r=[plb, cb_], w=[l1b])
                P.op("pool", lambda e, g1=g1, sgt=sgt: e.tensor_tensor(out=g1[:], in0=g1[:], in1=sgt[:], op=ALU.mult), r=[g1b, sgb], w=[g1b])
                P.op("dve", lambda e, AT=AT, m=m, l1=l1, g1=g1: e.scalar_tensor_tensor(
                    out=AT[:, m, :], in0=l1[:], scalar=8.0, in1=g1[:], op0=ALU.min, op1=ALU.mult), r=[l1b, g1b], w=[ATb])
            for cb in range(8):
                wt, wb = load_wblk(w2r[ex, cb])
                w3 = wt[:].rearrange("p (k c) -> p k c", k=16)
                for st_ in range(NST):
                    py, pyb, _ = pYr.next()
                    for kc in range(16):
                        P.mm(py[:, 0:256], AT[:, kc, st_ * 128:(st_ + 1) * 128], w3[:, kc, :], kc == 0, kc == 15, r=[ATb, wb], w=[pyb])
                    yt, yb, yd = ytr.next()
                    P.op("dve", lambda e, yt=yt, py=py, b2t=b2t, cb=cb: e.tensor_tensor(
                        out=yt[:], in0=py[:, 0:256], in1=b2t[:, cb * 256:(cb + 1) * 256], op=ALU.add), r=[pyb, b2b], w=[yb])
                    r0 = ex * CAP + st_ * 128
                    P.dma(ysc[r0:r0 + 128, cb * 256:(cb + 1) * 256], yt[:], r=[yb], dsem=yd)
        P.finalize()
    if stop_after == "G":
        esR.close()
        return nc

    es = ExitStack()
    with es:
        P = Phase(nc, "H")
        cb_ = Buf()
        rb_ = Buf()
        G2 = sb("G2", [128, D], F32)
        B2_ = sb("B2_", [128, D], F32)
        P.dma(G2[:], ln2g.to_broadcast([128, D]), w=[cb_], dsem=P.dsem())
        P.dma(B2_[:], ln2b.to_broadcast([128, D]), w=[cb_], dsem=P.dsem())
        ygr = Ring(P, [sb(f"hy{i}", [128, D], F32) for i in range(6)], True)
        x1r = Ring(P, [sb(f"hx{i}", [128, D], F32) for i in range(2)], True)
        acr = Ring(P, [sb(f"ha{i}", [128, D], F32) for i in range(2)])
        otr = Ring(P, [sb(f"ho{i}", [128, D], F32) for i in range(2)], True)
        ST = sb("hst", [128, 4, 6], F32)
        MV = sb("hmv", [128, 2], F32)
        RS = sb("hrs", [128, 1], F32)
        STb, MVb, RSb = Buf(), Buf(), Buf()
        for t_ in range(16):
            x1t, x1b_, x1d = x1r.next()
            P.dma(x1t[:], x1s[t_ * 128:(t_ + 1) * 128, :], w=[x1b_], dsem=x1d)
            ac, acb, _ = acr.next()
            P.op("act", lambda e, ac=ac, x1t=x1t: e.activation(out=ac[:], in_=x1t[:], func=AF.Copy, scale=DN_ALPHA), r=[x1b_], w=[acb])
            for k in range(4):
                yt, yb, yd = ygr.next()
                P.op("pool", lambda e, yt=yt, t_=t_, k=k: e.indirect_dma_start(
                    out=yt[:], out_offset=None, in_=ysc,
                    in_offset=bass.IndirectOffsetOnAxis(ap=DEST[:, t_, k:k + 1], axis=0)), r=[rb_], w=[yb], dsem=yd)
                P.op("dve", lambda e, ac=ac, yt=yt, t_=t_, k=k: e.scalar_tensor_tensor(
                    out=ac[:], in0=yt[:], scalar=GATE[:, t_, k:k + 1], in1=ac[:], op0=ALU.mult, op1=ALU.add), r=[yb, acb], w=[acb])
            ot, ob, od_ = otr.next()
            layer_norm(P, ac, acb, ot, ob, G2, B2_, cb_, (ST, STb, MV, MVb, RS, RSb), t_)
            P.dma(out[t_ * 128:(t_ + 1) * 128, :], ot[:], r=[ob], dsem=od_)
        P.finalize()
    esR.close()
    return nc


OFF_CQ, OFF_CKV, OFF_KPE, OFF_QD, OFF_KD, OFF_VD, OFF_GM, OFF_GD = 0, 768, 1280, 1344, 2880, 4416, 5952, 8000


def lhs_blocks(w, cols):
    K = w.shape[0]
    ws = w[:, cols]
    nb = ws.shape[1] // 128
    return np.ascontiguousarray(ws.reshape(K // 128, 128, nb, 128).transpose(2, 1, 0, 3))


def prep_shared(inp):
    w_in = np.asarray(inp["w_in"])[0]
    half = ROPE // 2
    kpe = np.arange(OFF_KPE, OFF_KPE + ROPE)
    kpe_perm = np.concatenate([kpe[half:], kpe[:half]])
    colsA = np.concatenate([
        np.arange(0, OFF_KPE), kpe, kpe_perm,
        np.arange(OFF_QD, OFF_QD + 1536), np.arange(OFF_KD, OFF_KD + 1536),
        np.arange(OFF_GM, OFF_GM + 4096)])
    sh = {}
    sh["wA"] = lhs_blocks(w_in, colsA)
    wv = w_in[:, OFF_VD:OFF_VD + 1536].reshape(4, 4, 128, 3, 512)
    sh["wV"] = np.ascontiguousarray(wv.transpose(3, 0, 2, 1, 4)).reshape(12, 128, 4, 512)
    w_uq = np.asarray(inp["w_uq"])[0]
    cols = []
    for h in range(MH):
        b = h * 192
        rope = np.arange(b + 128, b + 192)
        perm = np.concatenate([rope[half:], rope[:half]])
        cols.append(np.concatenate([np.arange(b, b + 128), rope, perm, perm, rope]))
    cols = np.concatenate(cols)
    wq = w_uq[:, cols].reshape(6, 128, MH, 384)
    sh["wUQ"] = np.ascontiguousarray(wq.transpose(2, 1, 0, 3)).reshape(MH, 128, 6 * 384)
    sh["gq"] = np.ascontiguousarray(np.asarray(inp["q_norm_g"])[0].reshape(6, 128).T)
    w_ukv = np.asarray(inp["w_ukv"])[0].reshape(4, 128, MH, 256)
    sh["wUK"] = np.ascontiguousarray(w_ukv[:, :, :, 0:128].transpose(2, 1, 0, 3)).reshape(MH, 128, 4 * 128)
    sh["wUV"] = np.ascontiguousarray(w_ukv[:, :, :, 128:256]).reshape(4, 128, MH * 128)
    sh["gkv"] = np.ascontiguousarray(np.asarray(inp["kv_norm_g"])[0].reshape(4, 128).T)
    inv = (np.float32(10000.0) ** (-np.arange(half, dtype=np.float32) / np.float32(half))).astype(np.float32)
    ang = (np.arange(S, dtype=np.float32)[:, None] * inv[None, :]).astype(np.float32)
    cs, sn = np.cos(ang).astype(np.float32).T, np.sin(ang).astype(np.float32).T
    sh["cosT"] = np.ascontiguousarray(np.concatenate([cs, cs], 0))
    sh["sinT"] = np.ascontiguousarray(np.concatenate([-sn, sn], 0))
    jj, ii = np.arange(128)[:, None], np.arange(128)[None, :]
    sh["maskc"] = (jj <= ii).astype(np.float32).astype(ml_dtypes.bfloat16)
    slopes = 2.0 ** (-8.0 * np.arange(1, DH + 1, dtype=np.float64) / DH)
    dm = np.zeros((20, 128, 128), np.float64)
    for hd in range(DH):
        dil = DIL[hd // 4][1]
        st = (ii - jj).astype(np.float64)
        dm[hd] = np.where(ii >= jj, np.exp(-slopes[hd] * dil * st), 0.0)
        if hd < 8:
            dm[12 + hd] = np.where(ii <= jj, np.exp(-slopes[hd] * dil * (st + 128.0)), 0.0)
    sh["dmask"] = np.ascontiguousarray(dm.transpose(1, 0, 2)).astype(np.float32)
    sh["wOUT"] = np.ascontiguousarray(np.asarray(inp["w_out"])[0].reshape(16, 128, 2048))
    for k in ("ln1_g", "ln1_b", "ln2_g", "ln2_b"):
        sh[k] = np.ascontiguousarray(np.asarray(inp[k]).reshape(1, D))
    sh["wR"] = np.ascontiguousarray(np.asarray(inp["w_router"])[0].reshape(16, 128, NE).transpose(1, 0, 2)).reshape(128, 16 * NE)
    sh["bR"] = np.ascontiguousarray(np.asarray(inp["b_router"]).reshape(1, NE))
    sh["identF"] = np.eye(128, dtype=np.float32)
    sh["identB"] = np.eye(128, dtype=np.float32).astype(ml_dtypes.bfloat16)
    sh["tri"] = (jj < ii).astype(np.float32).astype(ml_dtypes.bfloat16)
    sh["eb1"] = (np.arange(NE, dtype=np.float32) * CAP + 1.0).reshape(1, NE)
    w1 = np.asarray(inp["w1"])[0]
    w1 = w1.reshape(NE, 16, 128, 16, 128, 2)
    sh["w1r"] = np.ascontiguousarray(w1.transpose(0, 3, 2, 1, 5, 4)).reshape(NE, 16, 128, 4096)
    w2 = np.asarray(inp["w2"])[0].reshape(NE, 16, 128, 8, 256)
    sh["w2r"] = np.ascontiguousarray(w2.transpose(0, 3, 2, 1, 4)).reshape(NE, 8, 128, 4096)
    b1 = np.asarray(inp["b1"])[0].reshape(NE, 16, 128, 2)
    sh["b1r"] = np.ascontiguousarray(b1.transpose(2, 0, 3, 1)).reshape(128, NE * 32)
    sh["b2"] = np.ascontiguousarray(np.asarray(inp["b2"])[0])
    sh["wOM"] = lhs_blocks(np.asarray(inp["w_o_mla"])[0], np.arange(2048)).reshape(16, 128, 2048)
    sh["wOD"] = lhs_blocks(np.asarray(inp["w_o_dil"])[0], np.arange(2048)).reshape(16, 128, 1536)
    return sh


def make_in_maps(inp):
    sh = prep_shared(inp)
    x = np.asarray(inp["x"])
    maps = []
    for c in range(NCORES):
        m = dict(sh)
        m["x"] = np.ascontiguousarray(x[c])
        m["xT"] = np.ascontiguousarray(x[c].T)
        maps.append(m)
    return maps


def kernel(**inputs):
    nc = build()
    in_maps = make_in_maps(inputs)
    res = run_bass_kernel_spmd(nc, in_maps, core_ids=list(range(NCORES)))
    return np.stack([np.asarray(r["out"]) for r in res.results], axis=0).astype(np.float32)
```

```python
import math
from contextlib import ExitStack

import numpy as np
import ml_dtypes
import concourse.bass as bass
import concourse.mybir as mybir
from concourse.bass_utils import run_bass_kernel_spmd

F32 = mybir.dt.float32
BF16 = mybir.dt.bfloat16
I32 = mybir.dt.int32
U32 = mybir.dt.uint32
AF = mybir.ActivationFunctionType
ALU = mybir.AluOpType
AX = mybir.AxisListType

S = 2048
D = 2048
NCORES = 8
QR, KVR, ROPE = 768, 512, 64
MH = 16
DH = 12
NE = 32
CAP = 384
NST = CAP // 128
DFF = 2048
DN_ALPHA = 2.0 ** 0.25
LN_EPS = 1e-5
RMS_EPS = 1e-6
DIL = ((2048, 1), (512, 4), (128, 16))


class Buf:
    __slots__ = ("name", "ws", "rs")

    def __init__(self, name=""):
        self.name = name
        self.ws = []
        self.rs = []


class DSem:
    __slots__ = ("sem", "count")

    def __init__(self, sem):
        self.sem = sem
        self.count = 0


class Op:
    __slots__ = ("eng", "fn", "deps", "sig", "idx", "dsem", "dval", "waits", "ph")


class Phase:
    ENGS = ("pe", "act", "dve", "pool", "sp")

    POOL = None

    def __init__(self, nc, name):
        self.nc = nc
        self.name = name
        self.ops = []
        pool = Phase.POOL
        if pool is None or pool["nc"] is not nc:
            st = ExitStack()
            pool = Phase.POOL = {"nc": nc, "stack": st, "esem": {}, "ebase": {}, "dsems": []}
            for e in ("pe", "act", "dve", "pool"):
                pool["esem"][e] = st.enter_context(nc.semaphore(f"sem_{e}"))
                pool["ebase"][e] = 0
        self.pool = pool
        self.esem = pool["esem"]
        self.dsems = []
        self.nd = 0

    def dsem(self):
        pool = self.pool
        if self.nd == len(pool["dsems"]):
            pool["dsems"].append(DSem(pool["stack"].enter_context(self.nc.semaphore(f"sem_d{self.nd}"))))
        d = pool["dsems"][self.nd]
        self.nd += 1
        self.dsems.append(d)
        return d

    def op(self, eng, fn, r=(), w=(), dsem=None):
        o = Op()
        o.eng, o.fn, o.sig, o.idx, o.dsem, o.dval, o.waits = eng, fn, False, 0, dsem, 0, None
        o.ph = self
        isdma = dsem is not None
        if isdma:
            dsem.count += 16
            o.dval = dsem.count
        deps = {}

        def add(p, raw):
            if p is o or p.ph is not self:
                return
            if (not isdma) and p.dsem is None and p.eng == eng:
                if not raw or eng == "pe":
                    return
            deps[id(p)] = p

        for b in r:
            for p in b.ws:
                add(p, True)
        for b in w:
            for p in b.ws:
                add(p, False)
            for p in b.rs:
                add(p, False)
        o.deps = list(deps.values())
        for p in o.deps:
            if p.dsem is None:
                p.sig = True
        for b in w:
            if b.rs:
                b.ws = [o]
                b.rs = []
            else:
                b.ws = [p for p in b.ws if not (p.dsem is None and p.eng == eng and not isdma)] + [o]
        for b in r:
            if isdma:
                b.rs.append(o)
            else:
                b.rs = [p for p in b.rs if not (p.dsem is None and p.eng == eng and p.ph is self)] + [o]
        self.ops.append(o)
        return o

    def dma(self, out, in_, r=(), w=(), dsem=None, eng="sp", **kw):
        return self.op(eng, lambda e: e.dma_start(out=out, in_=in_, **kw), r, w, dsem=dsem)

    def mm(self, out, lhsT, rhs, start, stop, r=(), w=()):
        return self.op("pe", lambda e: e.matmul(out, lhsT, rhs, start=start, stop=stop), r, w)

    def finalize(self):
        nc = self.nc
        cnt = {e: self.pool["ebase"].get(e, 0) for e in self.ENGS}
        for o in self.ops:
            if o.dsem is None and o.sig:
                cnt[o.eng] += 1
                o.idx = cnt[o.eng]
        for e in self.pool["ebase"]:
            self.pool["ebase"][e] = cnt[e]
        seen = {}
        for o in self.ops:
            need = {}
            for p in o.deps:
                if p.dsem is not None:
                    key, sem, val = ("d", id(p.dsem)), p.dsem.sem, p.dval
                else:
                    key, sem, val = ("e", p.eng), self.esem[p.eng], p.idx
                if val > need.get(key, (None, 0))[1]:
                    need[key] = (sem, val)
            o.waits = []
            for key, (sem, val) in need.items():
                if val > seen.get((o.eng, key), 0):
                    seen[(o.eng, key)] = val
                    o.waits.append((sem, val))
        by = {e: [o for o in self.ops if o.eng == e] for e in self.ENGS}
        esem = self.esem
        dsems = self.dsems

        def mk(en):
            def body(e):
                for o in by[en]:
                    for sem, val in o.waits:
                        e.wait_ge(sem, val)
                    ins = o.fn(e)
                    if o.dsem is not None:
                        ins.then_inc(o.dsem.sem, 16)
                    elif o.sig:
                        ins.then_inc(esem[en], 1)
                if en == "sp":
                    for d in dsems:
                        if d.count:
                            e.wait_ge(d.sem, d.count)
            return body

        with nc.Block() as blk:
            blk.tensor(mk("pe"))
            blk.scalar(mk("act"))
            blk.vector(mk("dve"))
            blk.gpsimd(mk("pool"))
            blk.sync(mk("sp"))


class WStream:
    def __init__(self, P, srcs, wst, wbf, cast_fn):
        self.P, self.srcs, self.wst, self.wbf, self.cast_fn = P, srcs, wst, wbf, cast_fn
        self.st = {}
        self.bf = {}
        self.nl = 0
        self.ncst = 0

    def _cast(self, upto):
        upto = min(upto, len(self.srcs) - 1)
        while self.ncst <= upto:
            j = self.ncst
            self._load(j)
            t, b = self.st.pop(j)
            wt, wb, _ = self.wbf.next()
            self.cast_fn(j, t, b, wt, wb)
            self.bf[j] = (wt, wb)
            self.ncst += 1

    def _load(self, upto):
        upto = min(upto, len(self.srcs) - 1)
        while self.nl <= upto:
            k = self.nl
            self._cast(k - len(self.wst.tiles))
            t, b, ds = self.wst.next()
            self.P.dma(t[:], self.srcs[k], w=[b], dsem=ds)
            self.st[k] = (t, b)
            self.nl += 1

    def want(self, last_needed, ahead_load=2, ahead_cast=1):
        self._cast(last_needed)
        self._load(last_needed + ahead_load)
        self._cast(last_needed + ahead_cast)

    def get(self, i):
        return self.bf[i]


class Ring:
    def __init__(self, P, tiles, with_dsem=False):
        self.tiles = tiles
        self.bufs = [Buf() for _ in tiles]
        self.ds = [P.dsem() for _ in tiles] if with_dsem else None
        self.i = -1

    def next(self):
        self.i += 1
        k = self.i % len(self.tiles)
        return self.tiles[k], self.bufs[k], (self.ds[k] if self.ds else None)


def build(stop_after=None, debug=(), nblkA=67, doV=True, nhB2=16):
    nc = bass.Bass("TRN2", target_bir_lowering=False)

    def din(name, shape, dt=F32):
        return nc.dram_tensor(name, list(shape), dt, kind="ExternalInput").ap()

    def dscr(name, shape, dt):
        kind = "ExternalOutput" if name in debug else "Internal"
        return nc.dram_tensor(name, list(shape), dt, kind=kind).ap()

    xT = din("xT", [D, S])
    x_tm = din("x", [S, D])
    wA = din("wA", [67, 128, 16, 128])
    wV = din("wV", [12, 128, 4, 512])
    out = nc.dram_tensor("out", [S, D], F32, kind="ExternalOutput").ap()

    lat = dscr("lat", [1408, S], BF16)
    qd = dscr("qd", [1536, S], BF16)
    kd = dscr("kd", [1536, S], BF16)
    vd = dscr("vd", [3, S, 512], BF16)
    sg = dscr("sg", [4096, S], F32)

    es = ExitStack()

    def sb(name, shape, dt):
        return es.enter_context(nc.sbuf_tensor(name, list(shape), dt))

    def ps(name, shape, dt=F32):
        return es.enter_context(nc.psum_tensor(name, list(shape), dt))

    with es:
        P = Phase(nc, "A")
        XT = sb("XT", [128, 16, S], BF16)
        XTb = [Buf() for _ in range(16)]
        xst = Ring(P, [sb(f"xst{i}", [128, S], F32) for i in range(2)], True)
        wst = Ring(P, [sb(f"wst{i}", [128, 2048], F32) for i in range(3)], True)
        wbf = Ring(P, [sb(f"wbf{i}", [128, 2048], BF16) for i in range(6)])
        obf = Ring(P, [sb(f"obf{i}", [128, S], BF16) for i in range(2)], True)
        of32 = Ring(P, [sb(f"of{i}", [128, S], F32) for i in range(2)], True)
        ovd = Ring(P, [sb(f"ovd{i}", [128, 512], BF16) for i in range(4)], True)
        pbanks = Ring(P, [ps(f"pa{i}", [128, 512]) for i in range(8)])

        for kc in range(16):
            t, b, ds = xst.next()
            P.dma(t[:], xT[kc * 128:(kc + 1) * 128, :], w=[b], dsem=ds)
            eng = "dve" if kc % 2 == 0 else "pool"
            P.op(eng, lambda e, t=t, kc=kc: e.tensor_copy(out=XT[:, kc, :], in_=t[:]), r=[b], w=[XTb[kc]])

        cast_i = [0]

        def castA(j, t, b, t2, b2):
            eng = ("dve", "pool")[cast_i[0] % 2]
            cast_i[0] += 1
            P.op(eng, lambda e, t=t, t2=t2: e.tensor_copy(out=t2[:], in_=t[:]), r=[b], w=[b2])

        blkA = list(nblkA) if isinstance(nblkA, (list, tuple)) else list(range(nblkA))
        srcsA = [wA[i].rearrange("p k c -> p (k c)") for i in blkA]
        nA = len(srcsA)
        if doV:
            srcsA += [wV[j].rearrange("p k c -> p (k c)") for j in range(12)]
        wsA = WStream(P, srcsA, wst, wbf, castA)

        evac_i = [0]
        for bi_, blk_i in enumerate(blkA):
            wsA.want(bi_)
            wt, wb = wsA.get(bi_)
            wv = wt[:].rearrange("p (k c) -> p k c", k=16)
            banks = [pbanks.next() for _ in range(4)]
            for kc in range(16):
                for tb in range(4):
                    pt, pb, _ = banks[tb]
                    P.mm(pt[:], wv[:, kc, :], XT[:, kc, tb * 512:(tb + 1) * 512], kc == 0, kc == 15,
                         r=[wb, XTb[kc]], w=[pb])
            if blk_i < 11:
                kind, dst = "lat", lat[blk_i * 128:(blk_i + 1) * 128, :]
            elif blk_i < 23:
                kind, hd, dst = "qk", blk_i - 11, qd[(blk_i - 11) * 128:(blk_i - 10) * 128, :]
            elif blk_i < 35:
                kind, hd, dst = "qk", blk_i - 23, kd[(blk_i - 23) * 128:(blk_i - 22) * 128, :]
            else:
                kind, dst = "gate", sg[(blk_i - 35) * 128:(blk_i - 34) * 128, :]
            if kind == "gate":
                ot, ob, ods = of32.next()
            else:
                ot, ob, ods = obf.next()
            for tb in range(4):
                pt, pb, _ = banks[tb]
                if kind == "gate":
                    P.op("act", lambda e, pt=pt, ot=ot, tb=tb: e.activation(
                        out=ot[:, tb * 512:(tb + 1) * 512], in_=pt[:], func=AF.Sigmoid), r=[pb], w=[ob])
                    continue
                if kind == "qk" and hd >= 4:
                    dil = 4 if hd < 8 else 16
                    ni = 512 // dil
                    o_ap = ot[:].rearrange("p (r i) -> p r i", r=dil)[:, :, tb * ni:(tb + 1) * ni]
                    i_ap = pt[:].rearrange("p (i r) -> p r i", r=dil)
                else:
                    o_ap = ot[:, tb * 512:(tb + 1) * 512]
                    i_ap = pt[:]
                if evac_i[0] % 2 == 0:
                    P.op("dve", lambda e, o_ap=o_ap, i_ap=i_ap: e.tensor_copy(out=o_ap, in_=i_ap), r=[pb], w=[ob])
                else:
                    P.op("act", lambda e, o_ap=o_ap, i_ap=i_ap: e.activation(out=o_ap, in_=i_ap, func=AF.Copy),
                         r=[pb], w=[ob])
                evac_i[0] += 1
            P.dma(dst, ot[:], r=[ob], dsem=ods)

        for g in (range(3) if doV else ()):
            n, dil = DIL[g]
            wsA.want(nA + g * 4 + 3)
            wts = [wsA.get(nA + g * 4 + kq) for kq in range(4)]
            for T in range(16):
                r_, blk_ = divmod(T, n // 128)
                pt, pb, _ = pbanks.next()
                for kc in range(16):
                    wt, wb = wts[kc // 4]
                    rhs = wt[:].rearrange("p (k c) -> p k c", k=4)[:, kc % 4, :]
                    lhsT = XT[:, kc, :].rearrange("p (i r) -> p r i", r=dil)[:, r_, blk_ * 128:(blk_ + 1) * 128]
                    P.mm(pt[:], lhsT, rhs, kc == 0, kc == 15, r=[wb, XTb[kc]], w=[pb])
                ot, ob, ods = ovd.next()
                if T % 2 == 0:
                    P.op("dve", lambda e, ot=ot, pt=pt: e.tensor_copy(out=ot[:], in_=pt[:]), r=[pb], w=[ob])
                else:
                    P.op("act", lambda e, ot=ot, pt=pt: e.activation(out=ot[:], in_=pt[:], func=AF.Copy),
                         r=[pb], w=[ob])
                P.dma(vd[g, T * 128:(T + 1) * 128, :], ot[:], r=[ob], dsem=ods)
        P.finalize()
    if stop_after == "A":
        return nc

    wUQ = din("wUQ", [16, 128, 6 * 384])
    gq = din("gq", [128, 6])
    wUK = din("wUK", [16, 128, 4 * 128])
    wUV = din("wUV", [4, 128, 2048])
    gkv = din("gkv", [128, 4])
    cosT = din("cosT", [64, S])
    sinT = din("sinT", [64, S])
    maskc = din("maskc", [128, 128], BF16)
    qm = dscr("qm", [16, 192, S], BF16)
    km = dscr("km", [16, 128, S], BF16)
    krot = dscr("krot", [64, S], BF16)
    vm = dscr("vm", [S, 2048], BF16)
    om = dscr("om", [2048, S], BF16)

    es = ExitStack()
    with es:
        LAT = sb("LAT", [128, 10, S], BF16)
        LATb = [Buf() for _ in range(10)]
        KPE = sb("KPE", [64, S], BF16)
        KPEP = sb("KPEP", [64, S], BF16)
        COS = sb("COS", [64, S], F32)
        SIN = sb("SIN", [64, S], F32)
        GQ = sb("GQ", [128, 6], F32)
        GKV = sb("GKV", [128, 4], F32)
        ONESF = sb("ONESF", [128, 128], F32)
        cb_ = Buf()
        es1 = ExitStack()
        with es1:
            sb1 = lambda n, sh, dt: es1.enter_context(nc.sbuf_tensor(n, list(sh), dt))
            ps1 = lambda n, sh, dt=F32: es1.enter_context(nc.psum_tensor(n, list(sh), dt))
            P = Phase(nc, "B1")
            P.dma(LAT[:], lat[0:1280, :].rearrange("(k p) s -> p k s", p=128), w=LATb, dsem=P.dsem())
            P.dma(KPE[:], lat[1280:1344, :], w=[cb_], dsem=P.dsem())
            P.dma(KPEP[:], lat[1344:1408, :], w=[cb_], dsem=P.dsem())
            P.dma(COS[:], cosT, w=[cb_], dsem=P.dsem())
            P.dma(SIN[:], sinT, w=[cb_], dsem=P.dsem())
            P.dma(GQ[:], gq, w=[cb_], dsem=P.dsem())
            P.dma(GKV[:], gkv, w=[cb_], dsem=P.dsem())
            P.op("pool", lambda e: e.memset(ONESF[:], 1.0), w=[cb_])
            sq = Ring(P, [sb1(f"sq{i}", [128, 512], F32) for i in range(3)])
            rr = Ring(P, [sb1(f"rr{i}", [128, 512], F32) for i in range(2)])
            pss = Ring(P, [ps1(f"pss{i}", [128, 512]) for i in range(2)])
            k_ = 0
            for (c0, ncn, nfeat) in ((0, 6, 768), (6, 4, 512)):
                for tb in range(4):
                    pt, pb, _ = pss.next()
                    for kc in range(ncn):
                        st, sbuf_, _ = sq.next()
                        src = LAT[:, c0 + kc, tb * 512:(tb + 1) * 512]
                        if k_ % 2 == 0:
                            P.op("act", lambda e, st=st, src=src: e.activation(out=st[:], in_=src, func=AF.Square),
                                 r=[LATb[c0 + kc]], w=[sbuf_])
                        else:
                            P.op("dve", lambda e, st=st, src=src: e.tensor_tensor(out=st[:], in0=src, in1=src, op=ALU.mult),
                                 r=[LATb[c0 + kc]], w=[sbuf_])
                        k_ += 1
                        P.mm(pt[:], ONESF[:], st[:], kc == 0, kc == ncn - 1, r=[sbuf_, cb_], w=[pb])
                    rt, rb, _ = rr.next()
                    P.op("act", lambda e, rt=rt, pt=pt, nfeat=nfeat: e.activation(
                        out=rt[:], in_=pt[:], func=AF.Sqrt, bias=RMS_EPS, scale=1.0 / nfeat), r=[pb], w=[rb])
                    P.op("dve", lambda e, rt=rt: e.reciprocal(out=rt[:], in_=rt[:]), r=[rb], w=[rb])
                    for kc in range(ncn):
                        src = LAT[:, c0 + kc, tb * 512:(tb + 1) * 512]
                        eng = "dve" if kc % 2 == 0 else "pool"
                        P.op(eng, lambda e, src=src, rt=rt: e.tensor_tensor(out=src, in0=src, in1=rt[:], op=ALU.mult),
                             r=[rb, LATb[c0 + kc]], w=[LATb[c0 + kc]])
            WV = sb1("WV", [128, 4, 2048], BF16)
            WVb = Buf()
            wvs = Ring(P, [sb1(f"wvs{i}", [128, 2048], F32) for i in range(2)], True)
            for kc in range(4):
                t, b, ds = wvs.next()
                P.dma(t[:], wUV[kc], w=[b], dsem=ds)
                P.op("dve" if kc % 2 == 0 else "pool", lambda e, t=t, kc=kc: e.tensor_scalar(
                    out=WV[:, kc, :], in0=t[:], scalar1=GKV[:, kc:kc + 1], scalar2=None, op0=ALU.mult),
                    r=[b, cb_], w=[WVb])
            pv = Ring(P, [ps1(f"pv{i}", [128, 512]) for i in range(4)])
            vt = Ring(P, [sb1(f"vt{i}", [128, 2048], BF16) for i in range(2)], True)
            ev = 0
            for t in range(16):
                ot, ob, ods = vt.next()
                for hb in range(4):
                    pt, pb, _ = pv.next()
                    for kc in range(4):
                        P.mm(pt[:], LAT[:, 6 + kc, t * 128:(t + 1) * 128], WV[:, kc, hb * 512:(hb + 1) * 512],
                             kc == 0, kc == 3, r=[LATb[6 + kc], WVb], w=[pb])
                    o_ap = ot[:, hb * 512:(hb + 1) * 512]
                    if ev % 2 == 0:
                        P.op("dve", lambda e, o_ap=o_ap, pt=pt: e.tensor_copy(out=o_ap, in_=pt[:]), r=[pb], w=[ob])
                    else:
                        P.op("act", lambda e, o_ap=o_ap, pt=pt: e.activation(out=o_ap, in_=pt[:], func=AF.Copy), r=[pb], w=[ob])
                    ev += 1
                P.dma(vm[t * 128:(t + 1) * 128, :], ot[:], r=[ob], dsem=ods, eng="act")
            wks = Ring(P, [sb1(f"wks{i}", [128, 512], F32) for i in range(2)], True)
            wkb = Ring(P, [sb1(f"wkb{i}", [128, 512], BF16) for i in range(2)])
            kn = Ring(P, [sb1(f"kn{i}", [128, S], BF16) for i in range(2)], True)
            for h in range(16):
                t, b, ds = wks.next()
                P.dma(t[:], wUK[h], w=[b], dsem=ds)
                t2, b2, _ = wkb.next()
                for kc in range(4):
                    P.op("pool" if kc % 2 == 0 else "dve", lambda e, t=t, t2=t2, kc=kc: e.tensor_scalar(
                        out=t2[:, kc * 128:(kc + 1) * 128], in0=t[:, kc * 128:(kc + 1) * 128],
                        scalar1=GKV[:, kc:kc + 1], scalar2=None, op0=ALU.mult), r=[b, cb_], w=[b2])
                ot, ob, ods = kn.next()
                for tb in range(4):
                    pt, pb, _ = pv.next()
                    for kc in range(4):
                        P.mm(pt[:], t2[:, kc * 128:(kc + 1) * 128], LAT[:, 6 + kc, tb * 512:(tb + 1) * 512],
                             kc == 0, kc == 3, r=[b2, LATb[6 + kc]], w=[pb])
                    o_ap = ot[:, tb * 512:(tb + 1) * 512]
                    if ev % 2 == 0:
                        P.op("dve", lambda e, o_ap=o_ap, pt=pt: e.tensor_copy(out=o_ap, in_=pt[:]), r=[pb], w=[ob])
                    else:
                        P.op("act", lambda e, o_ap=o_ap, pt=pt: e.activation(out=o_ap, in_=pt[:], func=AF.Copy), r=[pb], w=[ob])
                    ev += 1
                P.dma(km[h], ot[:], r=[ob], dsem=ods, eng="act")
            P.finalize()
        if stop_after == "B1":
            return nc
        es2 = ExitStack()
        with es2:
            sb1 = lambda n, sh, dt: es2.enter_context(nc.sbuf_tensor(n, list(sh), dt))
            ps1 = lambda n, sh, dt=F32: es2.enter_context(nc.psum_tensor(n, list(sh), dt))
            P = Phase(nc, "B2")
            tmp = Ring(P, [sb1(f"tmp{i}", [64, 512], F32) for i in range(4)])
            KR = sb1("KR", [64, S], BF16)
            KRb = Buf()
            for tb in range(4):
                sl = slice(tb * 512, (tb + 1) * 512)
                t1, b1_, _ = tmp.next()
                t2, b2_, _ = tmp.next()
                P.op("dve", lambda e, t1=t1, sl=sl: e.tensor_tensor(out=t1[:], in0=KPE[:, sl], in1=COS[:, sl], op=ALU.mult), w=[b1_])
                P.op("pool", lambda e, t2=t2, sl=sl: e.tensor_tensor(out=t2[:], in0=KPEP[:, sl], in1=SIN[:, sl], op=ALU.mult), w=[b2_])
                P.op("dve", lambda e, t1=t1, t2=t2, sl=sl: e.tensor_tensor(out=KR[:, sl], in0=t1[:], in1=t2[:], op=ALU.add),
                     r=[b1_, b2_], w=[KRb])
            P.dma(krot, KR[:], r=[KRb], dsem=P.dsem())
            wqs = Ring(P, [sb1(f"wqs{i}", [128, 2304], F32) for i in range(2)], True)
            wqb = Ring(P, [sb1(f"wqb{i}", [128, 2304], BF16) for i in range(2)])
            qn = Ring(P, [sb1(f"qn{i}", [128, S], BF16) for i in range(2)], True)
            qr = Ring(P, [sb1(f"qr{i}", [64, S], BF16) for i in range(2)], True)
            pqn = Ring(P, [ps1(f"pqn{i}", [128, 512]) for i in range(2)])
            pqa = Ring(P, [ps1(f"pqa{i}", [128, 512]) for i in range(2)])
            pqb = Ring(P, [ps1(f"pqb{i}", [128, 512]) for i in range(2)])
            ev = 0
            for h in range(nhB2):
                t, b, ds = wqs.next()
                P.dma(t[:], wUQ[h], w=[b], dsem=ds)
                t2, b2, _ = wqb.next()
                for kc in range(6):
                    P.op("pool" if kc % 2 == 0 else "dve", lambda e, t=t, t2=t2, kc=kc: e.tensor_scalar(
                        out=t2[:, kc * 384:(kc + 1) * 384], in0=t[:, kc * 384:(kc + 1) * 384],
                        scalar1=GQ[:, kc:kc + 1], scalar2=None, op0=ALU.mult), r=[b], w=[b2])
                w3 = t2[:].rearrange("p (k c) -> p k c", k=6)
                qnt, qnb, qnd = qn.next()
                qrt, qrb, qrd = qr.next()
                for tb in range(4):
                    sl = slice(tb * 512, (tb + 1) * 512)
                    p1, pb1, _ = pqn.next()
                    p2, pb2, _ = pqa.next()
                    p3, pb3, _ = pqb.next()
                    for kc in range(6):
                        P.mm(p1[:], w3[:, kc, 0:128], LAT[:, kc, sl], kc == 0, kc == 5, r=[b2, LATb[kc]], w=[pb1])
                    for kc in range(6):
                        P.mm(p2[:], w3[:, kc, 128:256], LAT[:, kc, sl], kc == 0, kc == 5, r=[b2, LATb[kc]], w=[pb2])
                    for kc in range(6):
                        P.mm(p3[:], w3[:, kc, 256:384], LAT[:, kc, sl], kc == 0, kc == 5, r=[b2, LATb[kc]], w=[pb3])
                    P.op("act", lambda e, qnt=qnt, p1=p1, sl=sl: e.activation(out=qnt[:, sl], in_=p1[:], func=AF.Copy),
                         r=[pb1], w=[qnb])
                    t1, b1_, _ = tmp.next()
                    t2_, b2_, _ = tmp.next()
                    P.op("dve", lambda e, t1=t1, p2=p2, sl=sl: e.tensor_tensor(out=t1[:], in0=p2[0:64, :], in1=COS[:, sl], op=ALU.mult),
                         r=[pb2], w=[b1_])
                    P.op("dve", lambda e, t2_=t2_, p3=p3, sl=sl: e.tensor_tensor(out=t2_[:], in0=p3[0:64, :], in1=SIN[:, sl], op=ALU.mult),
                         r=[pb3], w=[b2_])
                    P.op("pool", lambda e, t1=t1, t2_=t2_, qrt=qrt, sl=sl: e.tensor_tensor(out=qrt[:, sl], in0=t1[:], in1=t2_[:], op=ALU.add),
                         r=[b1_, b2_], w=[qrb])
                P.dma(qm[h, 0:128, :], qnt[:], r=[qnb], dsem=qnd, eng="act")
                P.dma(qm[h, 128:192, :], qrt[:], r=[qrb], dsem=qrd, eng="act")
            P.finalize()
    if stop_after == "B":
        return nc

    es = ExitStack()
    with es:
        P = Phase(nc, "C")
        VALL = sb("VALL", [128, 16, 2048], BF16)
        KROT = sb("KROT", [64, S], BF16)
        MASKC = sb("MASKC", [128, 128], BF16)
        ONESB = sb("ONESB", [128, 128], BF16)
        cb_ = Buf()
        P.dma(VALL[:], vm.rearrange("(t p) c -> p t c", p=128), w=[cb_], dsem=P.dsem())
        P.dma(KROT[:], krot, w=[cb_], dsem=P.dsem())
        P.dma(MASKC[:], maskc, w=[cb_], dsem=P.dsem())
        P.op("pool", lambda e: e.memset(ONESB[:], 1.0), w=[cb_])
        qn = Ring(P, [sb(f"cqn{i}", [128, S], BF16) for i in range(2)], True)
        qr = Ring(P, [sb(f"cqr{i}", [64, S], BF16) for i in range(2)], True)
        kn = Ring(P, [sb(f"ckn{i}", [128, S], BF16) for i in range(2)], True)
        pr = Ring(P, [sb(f"cp{i}", [128, 512], BF16) for i in range(3)])
        oh = Ring(P, [sb(f"coh{i}", [128, S], BF16) for i in range(2)], True)
        rz = Ring(P, [sb(f"crz{i}", [128, 512], F32) for i in range(2)])
        pS = Ring(P, [ps(f"cS{i}", [128, 512]) for i in range(2)])
        pO = Ring(P, [ps(f"cO{i}", [128, 512]) for i in range(2)])
        pZ = Ring(P, [ps(f"cZ{i}", [128, 512]) for i in range(2)])
        sc_mla = 192.0 ** -0.5
        def loadC(h):
            qnt, qnb, d1 = qn.next()
            qrt, qrb, d2 = qr.next()
            knt, knb, d3 = kn.next()
            P.dma(qnt[:], qm[h, 0:128, :], w=[qnb], dsem=d1)
            P.dma(qrt[:], qm[h, 128:192, :], w=[qrb], dsem=d2)
            P.dma(knt[:], km[h], w=[knb], dsem=d3)
            return qnt, qnb, qrt, qrb, knt, knb

        headC = {}

        def need_head(h):
            if h < 16 and h not in headC:
                headC[h] = loadC(h)

        itemsC = [(h, Q, j) for h in range(16) for Q in range(4) for j in range(4 * Q + 4)]
        stC = {}

        def emitS(it):
            h, Q, j = it
            if Q == 0 and j == 0:
                need_head(h)
                need_head(h + 1)
            qnt, qnb, qrt, qrb, knt, knb = headC[h]
            q0 = max(512 * Q, 128 * j)
            wd = 512 * Q + 512 - q0
            pst, psb, _ = pS.next()
            P.mm(pst[:, 0:wd], knt[:, j * 128:(j + 1) * 128], qnt[:, q0:q0 + wd], True, False, r=[knb, qnb], w=[psb])
            P.mm(pst[:, 0:wd], KROT[:, j * 128:(j + 1) * 128], qrt[:, q0:q0 + wd], False, True, r=[cb_, qrb], w=[psb])
            pt, ptb, _ = pr.next()
            P.op("act", lambda e, pt=pt, pst=pst, wd=wd: e.activation(out=pt[:, 0:wd], in_=pst[:, 0:wd], func=AF.Exp, scale=sc_mla),
                 r=[psb], w=[ptb])
            if j >= 4 * Q:
                P.op("pool", lambda e, pt=pt: e.tensor_tensor(out=pt[:, 0:128], in0=pt[:, 0:128], in1=MASKC[:], op=ALU.mult),
                     r=[ptb, cb_], w=[ptb])
            stC[it] = (pt, ptb, wd, q0 - 512 * Q)

        accC = {}

        def emitPV(it):
            h, Q, j = it
            pt, ptb, wd, c0 = stC.pop(it)
            nj = 4 * Q + 4
            if j == 0:
                if Q == 0:
                    accC["oh"] = oh.next()
                accC["o"] = pO.next()
                accC["z"] = pZ.next()
            po, pob, _ = accC["o"]
            pz, pzb, _ = accC["z"]
            oht, ohb, ohd = accC["oh"]
            P.mm(po[:, c0:c0 + wd], VALL[:, j, h * 128:(h + 1) * 128], pt[:, 0:wd], j == 0, j == nj - 1, r=[cb_, ptb], w=[pob])
            P.mm(pz[:, c0:c0 + wd], ONESB[:], pt[:, 0:wd], j == 0, j == nj - 1, r=[cb_, ptb], w=[pzb])
            if j == nj - 1:
                rt, rb, _ = rz.next()
                P.op("dve", lambda e, rt=rt, pz=pz: e.reciprocal(out=rt[:], in_=pz[:]), r=[pzb], w=[rb])
                P.op("dve", lambda e, oht=oht, po=po, rt=rt, Q=Q: e.tensor_tensor(
                    out=oht[:, Q * 512:(Q + 1) * 512], in0=po[:], in1=rt[:], op=ALU.mult), r=[pob, rb], w=[ohb])
                if Q == 3:
                    P.dma(om[h * 128:(h + 1) * 128, :], oht[:], r=[ohb], dsem=ohd, eng="act")

        emitS(itemsC[0])
        for i_, it in enumerate(itemsC):
            if i_ + 1 < len(itemsC):
                emitS(itemsC[i_ + 1])
            emitPV(it)
        P.finalize()
    if stop_after == "C":
        return nc

    dmask = din("dmask", [128, 20, 128])
    od = dscr("od", [1536, S], BF16)
    es = ExitStack()
    with es:
        P = Phase(nc, "D")
        VD = sb("VD", [128, 3, 16, 512], BF16)
        MASKS = sb("MASKS", [128, 20, 128], F32)
        ONESB = sb("ONESBd", [128, 128], BF16)
        cb_ = Buf()
        for g in range(3):
            P.dma(VD[:, g], vd[g].rearrange("(t p) c -> p t c", p=128), w=[cb_], dsem=P.dsem())
        P.dma(MASKS[:], dmask, w=[cb_], dsem=P.dsem())
        P.op("pool", lambda e: e.memset(ONESB[:], 1.0), w=[cb_])
        UN = [sb(f"UN{g}", [128, S], F32) for g in range(3)]
        ZN = [sb(f"ZN{g}", [128, S], F32) for g in range(3)]
        UNb = [Buf() for _ in range(3)]
        ZNb = [Buf() for _ in range(3)]
        RT = sb("RTd", [128, S], F32)
        RTb = Buf()
        qdr = Ring(P, [sb(f"dq{i}", [128, S], BF16) for i in range(2)], True)
        kdr = Ring(P, [sb(f"dk{i}", [128, S], BF16) for i in range(2)], True)
        ecr = Ring(P, [sb(f"dec{i}", [128, 512], F32) for i in range(2)])
        epr = Ring(P, [sb(f"dep{i}", [128, 512], F32) for i in range(2)])
        pcr = Ring(P, [sb(f"dpc{i}", [128, 512], BF16) for i in range(2)])
        ppr = Ring(P, [sb(f"dpp{i}", [128, 512], BF16) for i in range(2)])
        odr = Ring(P, [sb(f"dod{i}", [128, S], BF16) for i in range(2)], True)
        pSc = Ring(P, [ps(f"dSc{i}", [128, 512]) for i in range(2)])
        pSp = Ring(P, [ps(f"dSp{i}", [128, 512]) for i in range(2)])
        pU = Ring(P, [ps(f"dU{i}", [128, 512]) for i in range(2)])
        pZ = Ring(P, [ps(f"dZ{i}", [128, 512]) for i in range(2)])
        sc_d = 128.0 ** -0.5
        for hs in range(4):
            for g in range(3):
                hd = g * 4 + hs
                n, dil = DIL[g]
                nb = n // 128
                qt, qb, d1 = qdr.next()
                kt, kb, d2 = kdr.next()
                P.dma(qt[:], qd[hd * 128:(hd + 1) * 128, :], w=[qb], dsem=d1)
                P.dma(kt[:], kd[hd * 128:(hd + 1) * 128, :], w=[kb], dsem=d2)
                for c in range(4):
                    us = [4 * c + s_ for s_ in range(4)]
                    hp = [u % nb != 0 for u in us]
                    s0 = hp.index(True) if any(hp) else 4
                    assert all(hp[s0:])
                    sc, scb, _ = pSc.next()
                    for s_, u in enumerate(us):
                        P.mm(sc[:, s_ * 128:(s_ + 1) * 128], kt[:, u * 128:(u + 1) * 128], qt[:, u * 128:(u + 1) * 128],
                             True, True, r=[kb, qb], w=[scb])
                    ec, ecb, _ = ecr.next()
                    P.op("act", lambda e, ec=ec, sc=sc: e.activation(out=ec[:], in_=sc[:], func=AF.Exp, scale=sc_d), r=[scb], w=[ecb])
                    pc, pcb, _ = pcr.next()
                    P.op("dve", lambda e, pc=pc, ec=ec, hd=hd: e.tensor_tensor(
                        out=pc[:].rearrange("p (s i) -> p s i", s=4), in0=ec[:].rearrange("p (s i) -> p s i", s=4),
                        in1=MASKS[:, hd:hd + 1, :].to_broadcast([128, 4, 128]), op=ALU.mult), r=[ecb, cb_], w=[pcb])
                    if s0 < 4:
                        sp, spb, _ = pSp.next()
                        for s_ in range(s0, 4):
                            u = us[s_]
                            P.mm(sp[:, s_ * 128:(s_ + 1) * 128], kt[:, (u - 1) * 128:u * 128], qt[:, u * 128:(u + 1) * 128],
                                 True, True, r=[kb, qb], w=[spb])
                        ep, epb, _ = epr.next()
                        P.op("act", lambda e, ep=ep, sp=sp, s0=s0: e.activation(
                            out=ep[:, s0 * 128:512], in_=sp[:, s0 * 128:512], func=AF.Exp, scale=sc_d), r=[spb], w=[epb])
                        pp, ppb, _ = ppr.next()
                        ns = 4 - s0
                        P.op("dve", lambda e, pp=pp, ep=ep, hd=hd, s0=s0, ns=ns: e.tensor_tensor(
                            out=pp[:, s0 * 128:512].rearrange("p (s i) -> p s i", s=ns),
                            in0=ep[:, s0 * 128:512].rearrange("p (s i) -> p s i", s=ns),
                            in1=MASKS[:, 12 + hd:13 + hd, :].to_broadcast([128, ns, 128]), op=ALU.mult), r=[epb, cb_], w=[ppb])
                    pu, pub, _ = pU.next()
                    pz, pzb, _ = pZ.next()
                    for s_, u in enumerate(us):
                        sl = slice(s_ * 128, (s_ + 1) * 128)
                        P.mm(pu[:, sl], VD[:, g, u, hs * 128:(hs + 1) * 128], pc[:, sl], True, not hp[s_], r=[cb_, pcb], w=[pub])
                        if hp[s_]:
                            P.mm(pu[:, sl], VD[:, g, u - 1, hs * 128:(hs + 1) * 128], pp[:, sl], False, True, r=[cb_, ppb], w=[pub])
                    for s_, u in enumerate(us):
                        sl = slice(s_ * 128, (s_ + 1) * 128)
                        P.mm(pz[:, sl], ONESB[:], pc[:, sl], True, not hp[s_], r=[cb_, pcb], w=[pzb])
                        if hp[s_]:
                            P.mm(pz[:, sl], ONESB[:], pp[:, sl], False, True, r=[cb_, ppb], w=[pzb])

                    def nat(t):
                        if g == 0:
                            return t[:, c * 512:(c + 1) * 512], None
                        if g == 1:
                            return t[:].rearrange("p (i r) -> p r i", r=4)[:, c, :], None
                        return t[:].rearrange("p (i r) -> p r i", r=16)[:, 4 * c:4 * c + 4, :], 4

                    uo, rs_ = nat(UN[g])
                    zo, _ = nat(ZN[g])
                    ui = pu[:] if rs_ is None else pu[:].rearrange("p (r i) -> p r i", r=4)
                    zi = pz[:] if rs_ is None else pz[:].rearrange("p (r i) -> p r i", r=4)
                    P.op("act", lambda e, uo=uo, ui=ui: e.activation(out=uo, in_=ui, func=AF.Copy), r=[pub], w=[UNb[g]])
                    P.op("dve", lambda e, zo=zo, zi=zi: e.tensor_copy(out=zo, in_=zi), r=[pzb], w=[ZNb[g]])
            P.op("pool", lambda e: e.tensor_tensor(out=RT[:], in0=ZN[0][:], in1=ZN[1][:], op=ALU.add), r=[ZNb[0], ZNb[1]], w=[RTb])
            P.op("pool", lambda e: e.tensor_tensor(out=RT[:], in0=RT[:], in1=ZN[2][:], op=ALU.add), r=[RTb, ZNb[2]], w=[RTb])
            P.op("dve", lambda e: e.reciprocal(out=RT[:], in_=RT[:]), r=[RTb], w=[RTb])
            for g in range(3):
                ot, ob, ods = odr.next()
                P.op("pool" if g != 1 else "dve", lambda e, ot=ot, g=g: e.tensor_tensor(out=ot[:], in0=UN[g][:], in1=RT[:], op=ALU.mult),
                     r=[UNb[g], RTb], w=[ob])
                P.dma(od[(g * 4 + hs) * 128:(g * 4 + hs + 1) * 128, :], ot[:], r=[ob], dsem=ods, eng="act")
        P.finalize()
    if stop_after == "D":
        return nc

    wOM = din("wOM", [16, 128, 2048])
    wOD = din("wOD", [16, 128, 1536])
    p1s = dscr("p1s", [2048, S], F32)
    mgd = dscr("mgd", [2048, S], BF16) if "mgd" in debug else None
    es = ExitStack()
    with es:
        P = Phase(nc, "E1a")
        OM = sb("OM", [128, 16, S], BF16)
        OMb = Buf()
        P.dma(OM[:], om.rearrange("(k p) s -> p k s", p=128), w=[OMb], dsem=P.dsem())
        wst = Ring(P, [sb(f"e1ws{i}", [128, 2048], F32) for i in range(3)], True)
        wbf = Ring(P, [sb(f"e1wb{i}", [128, 2048], BF16) for i in range(3)])
        sgr = Ring(P, [sb(f"e1sg{i}", [128, S], F32) for i in range(2)], True)
        otr = Ring(P, [sb(f"e1o{i}", [128, S], F32) for i in range(2)], True)
        pY = Ring(P, [ps(f"e1p{i}", [128, 512]) for i in range(4)])
        for m in range(16):
            t, b, ds = wst.next()
            P.dma(t[:], wOM[m], w=[b], dsem=ds)
            wt, wb, _ = wbf.next()
            P.op("pool", lambda e, t=t, wt=wt: e.tensor_copy(out=wt[:], in_=t[:]), r=[b], w=[wb])
            st, sbf, sds = sgr.next()
            P.dma(st[:], sg[m * 128:(m + 1) * 128, :], w=[sbf], dsem=sds)
            ot, ob, ods = otr.next()
            for tb in range(4):
                sl = slice(tb * 512, (tb + 1) * 512)
                pt, pb, _ = pY.next()
                for kc in range(16):
                    P.mm(pt[:], wt[:, kc * 128:(kc + 1) * 128], OM[:, kc, sl], kc == 0, kc == 15, r=[wb, OMb], w=[pb])
                P.op("dve", lambda e, ot=ot, pt=pt, st=st, sl=sl: e.tensor_tensor(out=ot[:, sl], in0=pt[:], in1=st[:, sl], op=ALU.mult),
                     r=[pb, sbf], w=[ob])
            P.dma(p1s[m * 128:(m + 1) * 128, :], ot[:], r=[ob], dsem=ods, eng="act")
        P.finalize()
    esMG = ExitStack()
    MG = esMG.enter_context(nc.sbuf_tensor("MG", [128, 16, S], BF16))
    es = ExitStack()
    with es:
        P = Phase(nc, "E1b")
        ODs = sb("ODs", [128, 12, S], BF16)
        ODb = Buf()
        MGb = Buf()
        P.dma(ODs[:], od.rearrange("(k p) s -> p k s", p=128), w=[ODb], dsem=P.dsem())
        wst = Ring(P, [sb(f"e2ws{i}", [128, 1536], F32) for i in range(3)], True)
        wbf = Ring(P, [sb(f"e2wb{i}", [128, 1536], BF16) for i in range(3)])
        sgr = Ring(P, [sb(f"e2sg{i}", [128, S], F32) for i in range(2)], True)
        p1r = Ring(P, [sb(f"e2p1{i}", [128, S], F32) for i in range(2)], True)
        tmr = Ring(P, [sb(f"e2t{i}", [128, 512], F32) for i in range(3)])
        pY = Ring(P, [ps(f"e2p{i}", [128, 512]) for i in range(4)])
        for m in range(16):
            t, b, ds = wst.next()
            P.dma(t[:], wOD[m], w=[b], dsem=ds)
            wt, wb, _ = wbf.next()
            P.op("pool", lambda e, t=t, wt=wt: e.tensor_copy(out=wt[:], in_=t[:]), r=[b], w=[wb])
            st, sbf, sds = sgr.next()
            P.dma(st[:], sg[2048 + m * 128:2048 + (m + 1) * 128, :], w=[sbf], dsem=sds)
            p1t, p1b, p1d = p1r.next()
            P.dma(p1t[:], p1s[m * 128:(m + 1) * 128, :], w=[p1b], dsem=p1d)
            for tb in range(4):
                sl = slice(tb * 512, (tb + 1) * 512)
                pt, pb, _ = pY.next()
                for kc in range(12):
                    P.mm(pt[:], wt[:, kc * 128:(kc + 1) * 128], ODs[:, kc, sl], kc == 0, kc == 11, r=[wb, ODb], w=[pb])
                tt, tbf, _ = tmr.next()
                P.op("dve", lambda e, tt=tt, pt=pt, st=st, sl=sl: e.tensor_tensor(out=tt[:], in0=pt[:], in1=st[:, sl], op=ALU.mult),
                     r=[pb, sbf], w=[tbf])
                P.op("pool", lambda e, tt=tt, p1t=p1t, sl=sl, m=m: e.tensor_tensor(out=MG[:, m, sl], in0=tt[:], in1=p1t[:, sl], op=ALU.add),
                     r=[tbf, p1b], w=[MGb])
        if mgd is not None:
            P.dma(mgd.rearrange("(k p) s -> p k s", p=128), MG[:], r=[MGb], dsem=P.dsem())
        P.finalize()
    if stop_after == "E1":
        esMG.close()
        return nc

    wOUT = din("wOUT", [16, 128, 2048])
    hs_ = dscr("hs", [S, D], F32)
    es = ExitStack()
    with es:
        P = Phase(nc, "E2a")
        MGb = Buf()
        WOh = sb("WOh", [128, 16, 1024], BF16)
        WOb = [Buf() for _ in range(16)]
        wst = Ring(P, [sb(f"e3ws{i}", [128, 1024], F32) for i in range(3)], True)
        xr = Ring(P, [sb(f"e3x{i}", [128, 1024], F32) for i in range(2)], True)
        hr = Ring(P, [sb(f"e3h{i}", [128, 1024], F32) for i in range(2)], True)
        pH = Ring(P, [ps(f"e3p{i}", [128, 512]) for i in range(4)])
        ci = 0
        for half in range(2):
            hsl = slice(half * 1024, (half + 1) * 1024)
            for kc in range(16):
                t, b, ds = wst.next()
                P.dma(t[:], wOUT[kc][:, hsl], w=[b], dsem=ds)
                eng = ("dve", "pool")[ci % 2]
                ci += 1
                P.op(eng, lambda e, t=t, kc=kc: e.tensor_copy(out=WOh[:, kc, :], in_=t[:]), r=[b], w=[WOb[kc]])
            for t_ in range(16):
                xt, xb, xd = xr.next()
                P.dma(xt[:], x_tm[t_ * 128:(t_ + 1) * 128, hsl], w=[xb], dsem=xd)
                ht, hb, hd_ = hr.next()
                for cb in range(2):
                    csl = slice(cb * 512, (cb + 1) * 512)
                    pt, pb, _ = pH.next()
                    for kc in range(16):
                        P.mm(pt[:], MG[:, kc, t_ * 128:(t_ + 1) * 128], WOh[:, kc, csl], kc == 0, kc == 15,
                             r=[MGb, WOb[kc]], w=[pb])
                    P.op("dve", lambda e, ht=ht, xt=xt, pt=pt, csl=csl: e.scalar_tensor_tensor(
                        out=ht[:, csl], in0=xt[:, csl], scalar=DN_ALPHA, in1=pt[:], op0=ALU.mult, op1=ALU.add),
                        r=[xb, pb], w=[hb])
                P.dma(hs_[t_ * 128:(t_ + 1) * 128, hsl], ht[:], r=[hb], dsem=hd_, eng="act")
        P.finalize()
    esMG.close()
    if stop_after == "E2a":
        return nc

    ln1g = din("ln1_g", [1, D])
    ln1b = din("ln1_b", [1, D])
    ln2g = din("ln2_g", [1, D])
    ln2b = din("ln2_b", [1, D])
    wR = din("wR", [128, 16 * 32])
    bR = din("bR", [1, 32])
    identF = din("identF", [128, 128])
    identB = din("identB", [128, 128], BF16)
    tri = din("tri", [128, 128], BF16)
    eb1 = din("eb1", [1, 32])
    x1s = dscr("x1s", [S, D], F32)
    NROW = NE * CAP + 128
    DUMMY = NE * CAP
    xg = dscr("xg", [NROW, D], BF16)
    ysc = dscr("ysc", [NROW, D], F32)
    esR = ExitStack()
    DEST = esR.enter_context(nc.sbuf_tensor("DEST", [128, 16, 4], I32))
    GATE = esR.enter_context(nc.sbuf_tensor("GATE", [128, 16, 4], F32))

    def layer_norm(P, src, srcb, dst, dstb, G_, B_, gb_, small, t_):
        st, stb, mv, mvb, rs, rsb = small
        for c4 in range(4):
            P.op("dve", lambda e, c4=c4: e.bn_stats(out=st[:, c4, :], in_=src[:, c4 * 512:(c4 + 1) * 512]), r=[srcb], w=[stb])
        P.op("dve", lambda e: e.bn_aggr(out=mv[:], in_=st[:].rearrange("p a b -> p (a b)")), r=[stb], w=[mvb])
        P.op("dve", lambda e: e.tensor_scalar(out=rs[:], in0=mv[:, 1:2], scalar1=LN_EPS, scalar2=None, op0=ALU.add), r=[mvb], w=[rsb])
        P.op("act", lambda e: e.activation(out=rs[:], in_=rs[:], func=AF.Sqrt), r=[rsb], w=[rsb])
        P.op("dve", lambda e: e.reciprocal(out=rs[:], in_=rs[:]), r=[rsb], w=[rsb])
        P.op("dve", lambda e: e.tensor_scalar(out=dst[:], in0=src[:], scalar1=mv[:, 0:1], scalar2=rs[:, 0:1],
                                              op0=ALU.subtract, op1=ALU.mult), r=[srcb, mvb, rsb], w=[dstb])
        P.op("pool", lambda e: e.tensor_tensor(out=dst[:], in0=dst[:], in1=G_[:], op=ALU.mult), r=[dstb, gb_], w=[dstb])
        P.op("pool", lambda e: e.tensor_tensor(out=dst[:], in0=dst[:], in1=B_[:], op=ALU.add), r=[dstb, gb_], w=[dstb])

    es = ExitStack()
    with es:
        P = Phase(nc, "E2b")
        cb_ = Buf()
        G1 = sb("G1", [128, D], F32)
        B1 = sb("B1", [128, D], F32)
        WR = sb("WR", [128, 16 * 32], F32)
        BR = sb("BR", [128, 32], F32)
        IDF = sb("IDF", [128, 128], F32)
        TRI = sb("TRI", [128, 128], BF16)
        ONESB = sb("ONESBe", [128, 128], BF16)
        EB1 = sb("EB1", [128, 32], F32)
        CNT = sb("CNT", [128, 32], F32)
        MASKB = sb("MASKB", [128, 16, 32], BF16)
        ZR = sb("ZR", [128, D], F32)
        P.dma(G1[:], ln1g.to_broadcast([128, D]), w=[cb_], dsem=P.dsem())
        P.dma(B1[:], ln1b.to_broadcast([128, D]), w=[cb_], dsem=P.dsem())
        P.dma(WR[:], wR, w=[cb_], dsem=P.dsem())
        P.dma(BR[:], bR.to_broadcast([128, 32]), w=[cb_], dsem=P.dsem())
        P.dma(IDF[:], identF, w=[cb_], dsem=P.dsem())
        P.dma(TRI[:], tri, w=[cb_], dsem=P.dsem())
        P.dma(EB1[:], eb1.to_broadcast([128, 32]), w=[cb_], dsem=P.dsem())
        P.op("pool", lambda e: e.memset(ONESB[:], 1.0), w=[cb_])
        cntb = Buf()
        P.op("pool", lambda e: e.memset(CNT[:], 0.0), w=[cntb])
        zrb = Buf()
        P.op("pool", lambda e: e.memset(ZR[:], 0.0), w=[zrb])
        P.dma(ysc[DUMMY:DUMMY + 128, :], ZR[:], r=[zrb], dsem=P.dsem())
        hr = Ring(P, [sb(f"e4h{i}", [128, D], F32) for i in range(2)], True)
        x1r = Ring(P, [sb(f"e4x1{i}", [128, D], F32) for i in range(2)], True)
        xbr = Ring(P, [sb(f"e4xb{i}", [128, D], BF16) for i in range(2)])
        xTr = Ring(P, [sb(f"e4xT{i}", [128, 16 * 128], F32) for i in range(2)])
        pT = Ring(P, [ps(f"e4pT{i}", [128, 512]) for i in range(2)])
        pL = Ring(P, [ps(f"e4pL{i}", [128, 512]) for i in range(2)])
        pPos = Ring(P, [ps(f"e4pP{i}", [128, 512]) for i in range(2)])
        pTot = Ring(P, [ps(f"e4pQ{i}", [128, 512]) for i in range(2)])
        scat_ds = [P.dsem() for _ in range(8)]
        sm = lambda n, sh, dt=F32: (sb(n, sh, dt), Buf())
        ST, STb = sm("e4st", [128, 4, 6])
        MV, MVb = sm("e4mv", [128, 2])
        RS, RSb = sm("e4rs", [128, 1])
        LG, LGb = sm("e4lg", [128, 32])
        T8, T8b = sm("e4t8", [128, 8])
        MK, MKb = sm("e4mk", [128, 32])
        NM, NMb = sm("e4nm", [128, 1])
        EX, EXb = sm("e4ex", [128, 32])
        DEN, DENb = sm("e4den", [128, 1])
        GG, GGb = sm("e4gg", [128, 32])
        PS_, PSb = sm("e4pos", [128, 32])
        VL, VLb = sm("e4vl", [128, 32])
        DV, DVb = sm("e4dv", [128, 32])
        D8, D8b = sm("e4d8", [128, 8])
        DF, DFb = sm("e4df", [128, 4])
        OH, OHb = sm("e4oh", [128, 32])
        destb = Buf()
        gateb = Buf()
        mbb = Buf()
        si = 0
        for t_ in range(16):
            ht, hb, hd_ = hr.next()
            P.dma(ht[:], hs_[t_ * 128:(t_ + 1) * 128, :], w=[hb], dsem=hd_)
            x1t, x1b_, x1d = x1r.next()
            layer_norm(P, ht, hb, x1t, x1b_, G1, B1, cb_, (ST, STb, MV, MVb, RS, RSb), t_)
            P.dma(x1s[t_ * 128:(t_ + 1) * 128, :], x1t[:], r=[x1b_], dsem=x1d, eng="act")
            xbt, xbb, _ = xbr.next()
            P.op("act", lambda e, xbt=xbt, x1t=x1t: e.activation(out=xbt[:], in_=x1t[:], func=AF.Copy), r=[x1b_], w=[xbb])
            xT, xTb, _ = xTr.next()
            for q4 in range(4):
                pt, pb, _ = pT.next()
                for j in range(4):
                    kc = q4 * 4 + j
                    P.op("pe", lambda e, pt=pt, j=j, kc=kc, x1t=x1t: e.transpose(
                        out=pt[:, j * 128:(j + 1) * 128], in_=x1t[:, kc * 128:(kc + 1) * 128], identity=IDF[:]),
                        r=[x1b_, cb_], w=[pb])
                P.op("act" if q4 % 2 else "dve", (lambda e, xT=xT, pt=pt, q4=q4: e.activation(out=xT[:, q4 * 512:(q4 + 1) * 512], in_=pt[:], func=AF.Copy))
                     if q4 % 2 else (lambda e, xT=xT, pt=pt, q4=q4: e.tensor_copy(out=xT[:, q4 * 512:(q4 + 1) * 512], in_=pt[:])),
                     r=[pb], w=[xTb])
            pl, plb, _ = pL.next()
            for kc in range(16):
                P.mm(pl[:, 0:32], xT[:, kc * 128:(kc + 1) * 128], WR[:, kc * 32:(kc + 1) * 32], kc == 0, kc == 15, r=[xTb, cb_], w=[plb])
            P.op("dve", lambda e, pl=pl: e.tensor_tensor(out=LG[:], in0=pl[:, 0:32], in1=BR[:], op=ALU.add), r=[plb, cb_], w=[LGb])
            P.op("dve", lambda e: e.max(out=T8[:], in_=LG[:]), r=[LGb], w=[T8b])
            P.op("dve", lambda e: e.tensor_scalar(out=MK[:], in0=LG[:], scalar1=T8[:, 3:4], scalar2=None, op0=ALU.is_ge), r=[LGb, T8b], w=[MKb])
            P.op("dve", lambda e: e.tensor_scalar(out=NM[:], in0=T8[:, 0:1], scalar1=-1.0, scalar2=None, op0=ALU.mult), r=[T8b], w=[NMb])
            P.op("act", lambda e: e.activation(out=EX[:], in_=LG[:], func=AF.Exp, bias=NM[:, 0:1], scale=1.0), r=[LGb, NMb], w=[EXb])
            P.op("dve", lambda e: e.tensor_tensor(out=EX[:], in0=EX[:], in1=MK[:], op=ALU.mult), r=[EXb, MKb], w=[EXb])
            P.op("dve", lambda e: e.reduce_sum(out=DEN[:], in_=EX[:], axis=AX.X), r=[EXb], w=[DENb])
            P.op("dve", lambda e: e.reciprocal(out=DEN[:], in_=DEN[:]), r=[DENb], w=[DENb])
            P.op("pool", lambda e, t_=t_: e.tensor_copy(out=MASKB[:, t_, :], in_=MK[:]), r=[MKb], w=[mbb])
            pp_, ppb, _ = pPos.next()
            pq_, pqb, _ = pTot.next()
            P.mm(pp_[:, 0:32], TRI[:], MASKB[:, t_, :], True, True, r=[mbb, cb_], w=[ppb])
            P.mm(pq_[:, 0:32], ONESB[:], MASKB[:, t_, :], True, True, r=[mbb, cb_], w=[pqb])
            P.op("dve", lambda e, pp_=pp_: e.tensor_tensor(out=PS_[:], in0=pp_[:, 0:32], in1=CNT[:], op=ALU.add), r=[ppb, cntb], w=[PSb])
            P.op("dve", lambda e, pq_=pq_: e.tensor_tensor(out=CNT[:], in0=pq_[:, 0:32], in1=CNT[:], op=ALU.add), r=[pqb, cntb], w=[cntb])
            P.op("dve", lambda e: e.tensor_scalar(out=VL[:], in0=PS_[:], scalar1=float(CAP), scalar2=None, op0=ALU.is_lt), r=[PSb], w=[VLb])
            P.op("dve", lambda e: e.tensor_tensor(out=VL[:], in0=VL[:], in1=MK[:], op=ALU.mult), r=[VLb, MKb], w=[VLb])
            P.op("dve", lambda e: e.scalar_tensor_tensor(out=GG[:], in0=EX[:], scalar=DEN[:, 0:1], in1=VL[:], op0=ALU.mult, op1=ALU.mult),
                 r=[EXb, DENb, VLb], w=[GGb])
            P.op("dve", lambda e: e.tensor_tensor(out=DV[:], in0=PS_[:], in1=EB1[:], op=ALU.add), r=[PSb, cb_], w=[DVb])
            P.op("dve", lambda e: e.tensor_tensor(out=DV[:], in0=DV[:], in1=VL[:], op=ALU.mult), r=[DVb, VLb], w=[DVb])
            P.op("dve", lambda e: e.max(out=D8[:], in_=DV[:]), r=[DVb], w=[D8b])
            P.op("dve", lambda e: e.tensor_scalar(out=DF[:], in0=D8[:, 0:4], scalar1=0.0, scalar2=float(DUMMY + 1),
                                                  op0=ALU.is_equal, op1=ALU.mult), r=[D8b], w=[DFb])
            P.op("dve", lambda e: e.scalar_tensor_tensor(out=DF[:], in0=D8[:, 0:4], scalar=-1.0, in1=DF[:], op0=ALU.add, op1=ALU.add),
                 r=[D8b, DFb], w=[DFb])
            P.op("dve", lambda e, t_=t_: e.tensor_copy(out=DEST[:, t_, :], in_=DF[:]), r=[DFb], w=[destb])
            for k in range(4):
                P.op("dve", lambda e, k=k: e.tensor_scalar(out=OH[:], in0=DV[:], scalar1=D8[:, k:k + 1], scalar2=None, op0=ALU.is_equal),
                     r=[DVb, D8b], w=[OHb])
                P.op("dve", lambda e: e.tensor_tensor(out=OH[:], in0=OH[:], in1=GG[:], op=ALU.mult), r=[OHb, GGb], w=[OHb])
                P.op("dve", lambda e, k=k, t_=t_: e.reduce_sum(out=GATE[:, t_, k:k + 1], in_=OH[:], axis=AX.X), r=[OHb], w=[gateb])
            for k in range(4):
                ds = scat_ds[si % 8]
                si += 1
                P.op("pool", lambda e, xbt=xbt, t_=t_, k=k: e.indirect_dma_start(
                    out=xg, out_offset=bass.IndirectOffsetOnAxis(ap=DEST[:, t_, k:k + 1], axis=0),
                    in_=xbt[:], in_offset=None), r=[xbb, destb], w=[], dsem=ds)
        P.finalize()
    if stop_after == "E2":
        esR.close()
        return nc

    w1r = din("w1r", [NE, 16, 128, 4096])
    w2r = din("w2r", [NE, 8, 128, 4096])
    b1r = din("b1r", [128, NE * 32])
    b2 = din("b2", [NE, D])
    es = ExitStack()
    with es:
        P = Phase(nc, "G")
        cb_ = Buf()
        IDB = sb("IDB", [128, 128], BF16)
        B1A = sb("B1A", [128, NE * 32], F32)
        B1L = sb("B1L", [128, NE * 16], F32)
        P.dma(IDB[:], identB, w=[cb_], dsem=P.dsem())
        P.dma(B1A[:], b1r, w=[cb_], dsem=P.dsem())
        P.op("dve", lambda e: e.tensor_scalar(
            out=B1L[:].rearrange("p (e m) -> p e m", e=NE), in0=B1A[:].rearrange("p (e m) -> p e m", e=NE)[:, :, 16:32],
            scalar1=1.0, scalar2=None, op0=ALU.add), r=[cb_], w=[cb_])
        xgr = Ring(P, [sb(f"gx{i}", [128, D], BF16) for i in range(3)], True)
        xgT = Ring(P, [sb(f"gxT{i}", [128, 16, CAP], BF16) for i in range(2)])
        wst = Ring(P, [sb(f"gws{i}", [128, 4096], F32) for i in range(3)], True)
        wbf = Ring(P, [sb(f"gwb{i}", [128, 4096], BF16) for i in range(3)])
        atr = Ring(P, [sb(f"gat{i}", [128, 16, CAP], BF16) for i in range(2)])
        g1r = Ring(P, [sb(f"gg1{i}", [128, CAP], F32) for i in range(2)])
        sgr = Ring(P, [sb(f"gsg{i}", [128, CAP], F32) for i in range(2)])
        l1r = Ring(P, [sb(f"gl1{i}", [128, CAP], F32) for i in range(2)])
        ytr = Ring(P, [sb(f"gy{i}", [128, 256], F32) for i in range(4)], True)
        b2r = Ring(P, [sb(f"gb2{i}", [128, D], F32) for i in range(2)], True)
        pTr = Ring(P, [ps(f"gpT{i}", [128, 1024], BF16) for i in range(2)])
        pG = Ring(P, [ps(f"gpG{i}", [128, 512]) for i in range(2)])
        pLn = Ring(P, [ps(f"gpL{i}", [128, 512]) for i in range(2)])
        pYr = Ring(P, [ps(f"gpY{i}", [128, 512]) for i in range(2)])
        cast_pat = ("act", "dve", "act", "dve", "act", "pool")
        cig = [0]

        def castG(j, t, b, wt, wb):
            for hf in range(2):
                eng = cast_pat[cig[0] % 6]
                cig[0] += 1
                hsl = slice(hf * 2048, (hf + 1) * 2048)
                if eng == "act":
                    P.op("act", lambda e, wt=wt, t=t, hsl=hsl: e.activation(out=wt[:, hsl], in_=t[:, hsl], func=AF.Copy), r=[b], w=[wb])
                else:
                    P.op(eng, lambda e, wt=wt, t=t, hsl=hsl: e.tensor_copy(out=wt[:, hsl], in_=t[:, hsl]), r=[b], w=[wb])

        srcsG = []
        for ex in range(NE):
            srcsG += [w1r[ex, m] for m in range(16)] + [w2r[ex, cb] for cb in range(8)]
        wsG = WStream(P, srcsG, wst, wbf, castG)

        def load_xg(ex):
            res_ = []
            for st_ in range(NST):
                xt, xb, xd = xgr.next()
                r0 = ex * CAP + st_ * 128
                P.dma(xt[:], xg[r0:r0 + 128, :], w=[xb], dsem=xd)
                res_.append((xt, xb))
            return res_

        nxt_xg = load_xg(0)
        for ex in range(NE):
            b2t, b2b, b2d = b2r.next()
            P.dma(b2t[:], b2[ex:ex + 1, :].to_broadcast([128, D]), w=[b2b], dsem=b2d)
            XT_, XTb_, _ = xgT.next()
            cur_xg = nxt_xg
            for st_ in range(NST):
                xt, xb = cur_xg[st_]
                for h8 in range(2):
                    pt, pb, _ = pTr.next()
                    for j in range(8):
                        kc = h8 * 8 + j
                        P.op("pe", lambda e, pt=pt, j=j, kc=kc, xt=xt: e.transpose(
                            out=pt[:, j * 128:(j + 1) * 128], in_=xt[:, kc * 128:(kc + 1) * 128], identity=IDB[:]),
                            r=[xb, cb_], w=[pb])
                    o_ap = XT_[:, h8 * 8:(h8 + 1) * 8, st_ * 128:(st_ + 1) * 128]
                    i_ap = pt[:].rearrange("p (j c) -> p j c", j=8)
                    if h8 == 0:
                        P.op("dve", lambda e, o_ap=o_ap, i_ap=i_ap: e.tensor_copy(out=o_ap, in_=i_ap), r=[pb], w=[XTb_])
                    else:
                        P.op("act", lambda e, o_ap=o_ap, i_ap=i_ap: e.activation(out=o_ap, in_=i_ap, func=AF.Copy), r=[pb], w=[XTb_])
            AT, ATb, _ = atr.next()
            for m in range(16):
                wsG.want(ex * 24 + m)
                wt, wb = wsG.get(ex * 24 + m)
                w3 = wt[:].rearrange("p (k c) -> p k c", k=16)
                pg, pgb, _ = pG.next()
                pl, plb, _ = pLn.next()
                for kc in range(16):
                    P.mm(pg[:, 0:CAP], w3[:, kc, 0:128], XT_[:, kc, :], kc == 0, kc == 15, r=[wb, XTb_], w=[pgb])
                for kc in range(16):
                    P.mm(pl[:, 0:CAP], w3[:, kc, 128:256], XT_[:, kc, :], kc == 0, kc == 15, r=[wb, XTb_], w=[plb])
                g1, g1b, _ = g1r.next()
                sgt, sgb, _ = sgr.next()
                l1, l1b, _ = l1r.next()
                bg = B1A[:, ex * 32 + m:ex * 32 + m + 1]
                bl = B1L[:, ex * 16 + m:ex * 16 + m + 1]
                P.op("dve", lambda e, g1=g1, pg=pg, bg=bg: e.tensor_scalar(out=g1[:], in0=pg[:, 0:CAP], scalar1=bg, scalar2=7.0,
                                                                            op0=ALU.add, op1=ALU.min), r=[pgb, cb_], w=[g1b])
                P.op("act", lambda e, sgt=sgt, g1=g1: e.activation(out=sgt[:], in_=g1[:], func=AF.Sigmoid, scale=1.702), r=[g1b], w=[sgb])
                P.op("dve", lambda e, l1=l1, pl=pl, bl=bl: e.tensor_scalar(out=l1[:], in0=pl[:, 0:CAP], scalar1=bl, scalar2=-6.0,
                                                                            op0=ALU.add, op1=ALU.max), r=[plb, cb_], w=[l1b])
                P.op("pool", lambda e, g1=g1, sgt=sgt: e.tensor_tensor(out=g1[:], in0=g1[:], in1=sgt[:], op=ALU.mult), r=[g1b, sgb], w=[g1b])
                P.op("dve", lambda e, AT=AT, m=m, l1=l1, g1=g1: e.scalar_tensor_tensor(
                    out=AT[:, m, :], in0=l1[:], scalar=8.0, in1=g1[:], op0=ALU.min, op1=ALU.mult), r=[l1b, g1b], w=[ATb])
            if ex + 1 < NE:
                nxt_xg = load_xg(ex + 1)
            for cb in range(8):
                wsG.want(ex * 24 + 16 + cb)
                wt, wb = wsG.get(ex * 24 + 16 + cb)
                w3 = wt[:].rearrange("p (k c) -> p k c", k=16)
                for st_ in range(NST):
                    py, pyb, _ = pYr.next()
                    for kc in range(16):
                        P.mm(py[:, 0:256], AT[:, kc, st_ * 128:(st_ + 1) * 128], w3[:, kc, :], kc == 0, kc == 15, r=[ATb, wb], w=[pyb])
                    yt, yb, yd = ytr.next()
                    P.op("dve", lambda e, yt=yt, py=py, b2t=b2t, cb=cb: e.tensor_tensor(
                        out=yt[:], in0=py[:, 0:256], in1=b2t[:, cb * 256:(cb + 1) * 256], op=ALU.add), r=[pyb, b2b], w=[yb])
                    r0 = ex * CAP + st_ * 128
                    P.dma(ysc[r0:r0 + 128, cb * 256:(cb + 1) * 256], yt[:], r=[yb], dsem=yd)
        P.finalize()
    if stop_after == "G":
        esR.close()
        return nc

    es = ExitStack()
    with es:
        P = Phase(nc, "H")
        cb_ = Buf()
        rb_ = Buf()
        G2 = sb("G2", [128, D], F32)
        B2_ = sb("B2_", [128, D], F32)
        P.dma(G2[:], ln2g.to_broadcast([128, D]), w=[cb_], dsem=P.dsem())
        P.dma(B2_[:], ln2b.to_broadcast([128, D]), w=[cb_], dsem=P.dsem())
        ygr = Ring(P, [sb(f"hy{i}", [128, D], F32) for i in range(8)], True)
        x1r = Ring(P, [sb(f"hx{i}", [128, D], F32) for i in range(2)], True)
        acr = Ring(P, [sb(f"ha{i}", [128, D], F32) for i in range(2)])
        otr = Ring(P, [sb(f"ho{i}", [128, D], F32) for i in range(2)], True)
        ST = sb("hst", [128, 4, 6], F32)
        MV = sb("hmv", [128, 2], F32)
        RS = sb("hrs", [128, 1], F32)
        STb, MVb, RSb = Buf(), Buf(), Buf()
        def issueH(t_):
            x1t, x1b_, x1d = x1r.next()
            P.dma(x1t[:], x1s[t_ * 128:(t_ + 1) * 128, :], w=[x1b_], dsem=x1d)
            ys = []
            for k in range(4):
                yt, yb, yd = ygr.next()
                P.op("pool", lambda e, yt=yt, t_=t_, k=k: e.indirect_dma_start(
                    out=yt[:], out_offset=None, in_=ysc,
                    in_offset=bass.IndirectOffsetOnAxis(ap=DEST[:, t_, k:k + 1], axis=0)), r=[rb_], w=[yb], dsem=yd)
                ys.append((yt, yb))
            return x1t, x1b_, ys

        nxtH = issueH(0)
        for t_ in range(16):
            x1t, x1b_, ys = nxtH
            if t_ + 1 < 16:
                nxtH = issueH(t_ + 1)
            ac, acb, _ = acr.next()
            P.op("act", lambda e, ac=ac, x1t=x1t: e.activation(out=ac[:], in_=x1t[:], func=AF.Copy, scale=DN_ALPHA), r=[x1b_], w=[acb])
            for k in range(4):
                yt, yb = ys[k]
                P.op("dve", lambda e, ac=ac, yt=yt, t_=t_, k=k: e.scalar_tensor_tensor(
                    out=ac[:], in0=yt[:], scalar=GATE[:, t_, k:k + 1], in1=ac[:], op0=ALU.mult, op1=ALU.add), r=[yb, acb], w=[acb])
            ot, ob, od_ = otr.next()
            layer_norm(P, ac, acb, ot, ob, G2, B2_, cb_, (ST, STb, MV, MVb, RS, RSb), t_)
            P.dma(out[t_ * 128:(t_ + 1) * 128, :], ot[:], r=[ob], dsem=od_, eng="act")
        P.finalize()
    esR.close()
    return nc


OFF_CQ, OFF_CKV, OFF_KPE, OFF_QD, OFF_KD, OFF_VD, OFF_GM, OFF_GD = 0, 768, 1280, 1344, 2880, 4416, 5952, 8000


def lhs_blocks(w, cols):
    K = w.shape[0]
    ws = w[:, cols]
    nb = ws.shape[1] // 128
    return np.ascontiguousarray(ws.reshape(K // 128, 128, nb, 128).transpose(2, 1, 0, 3))


def prep_shared(inp):
    w_in = np.asarray(inp["w_in"])[0]
    half = ROPE // 2
    kpe = np.arange(OFF_KPE, OFF_KPE + ROPE)
    kpe_perm = np.concatenate([kpe[half:], kpe[:half]])
    colsA = np.concatenate([
        np.arange(0, OFF_KPE), kpe, kpe_perm,
        np.arange(OFF_QD, OFF_QD + 1536), np.arange(OFF_KD, OFF_KD + 1536),
        np.arange(OFF_GM, OFF_GM + 4096)])
    sh = {}
    sh["wA"] = lhs_blocks(w_in, colsA)
    wv = w_in[:, OFF_VD:OFF_VD + 1536].reshape(4, 4, 128, 3, 512)
    sh["wV"] = np.ascontiguousarray(wv.transpose(3, 0, 2, 1, 4)).reshape(12, 128, 4, 512)
    w_uq = np.asarray(inp["w_uq"])[0]
    cols = []
    for h in range(MH):
        b = h * 192
        rope = np.arange(b + 128, b + 192)
        perm = np.concatenate([rope[half:], rope[:half]])
        cols.append(np.concatenate([np.arange(b, b + 128), rope, perm, perm, rope]))
    cols = np.concatenate(cols)
    wq = w_uq[:, cols].reshape(6, 128, MH, 384)
    sh["wUQ"] = np.ascontiguousarray(wq.transpose(2, 1, 0, 3)).reshape(MH, 128, 6 * 384)
    sh["gq"] = np.ascontiguousarray(np.asarray(inp["q_norm_g"])[0].reshape(6, 128).T)
    w_ukv = np.asarray(inp["w_ukv"])[0].reshape(4, 128, MH, 256)
    sh["wUK"] = np.ascontiguousarray(w_ukv[:, :, :, 0:128].transpose(2, 1, 0, 3)).reshape(MH, 128, 4 * 128)
    sh["wUV"] = np.ascontiguousarray(w_ukv[:, :, :, 128:256]).reshape(4, 128, MH * 128)
    sh["gkv"] = np.ascontiguousarray(np.asarray(inp["kv_norm_g"])[0].reshape(4, 128).T)
    inv = (np.float32(10000.0) ** (-np.arange(half, dtype=np.float32) / np.float32(half))).astype(np.float32)
    ang = (np.arange(S, dtype=np.float32)[:, None] * inv[None, :]).astype(np.float32)
    cs, sn = np.cos(ang).astype(np.float32).T, np.sin(ang).astype(np.float32).T
    sh["cosT"] = np.ascontiguousarray(np.concatenate([cs, cs], 0))
    sh["sinT"] = np.ascontiguousarray(np.concatenate([-sn, sn], 0))
    jj, ii = np.arange(128)[:, None], np.arange(128)[None, :]
    sh["maskc"] = (jj <= ii).astype(np.float32).astype(ml_dtypes.bfloat16)
    slopes = 2.0 ** (-8.0 * np.arange(1, DH + 1, dtype=np.float64) / DH)
    dm = np.zeros((20, 128, 128), np.float64)
    for hd in range(DH):
        dil = DIL[hd // 4][1]
        st = (ii - jj).astype(np.float64)
        dm[hd] = np.where(ii >= jj, np.exp(-slopes[hd] * dil * st), 0.0)
        if hd < 8:
            dm[12 + hd] = np.where(ii <= jj, np.exp(-slopes[hd] * dil * (st + 128.0)), 0.0)
    sh["dmask"] = np.ascontiguousarray(dm.transpose(1, 0, 2)).astype(np.float32)
    sh["wOUT"] = np.ascontiguousarray(np.asarray(inp["w_out"])[0].reshape(16, 128, 2048))
    for k in ("ln1_g", "ln1_b", "ln2_g", "ln2_b"):
        sh[k] = np.ascontiguousarray(np.asarray(inp[k]).reshape(1, D))
    sh["wR"] = np.ascontiguousarray(np.asarray(inp["w_router"])[0].reshape(16, 128, NE).transpose(1, 0, 2)).reshape(128, 16 * NE)
    sh["bR"] = np.ascontiguousarray(np.asarray(inp["b_router"]).reshape(1, NE))
    sh["identF"] = np.eye(128, dtype=np.float32)
    sh["identB"] = np.eye(128, dtype=np.float32).astype(ml_dtypes.bfloat16)
    sh["tri"] = (jj < ii).astype(np.float32).astype(ml_dtypes.bfloat16)
    sh["eb1"] = (np.arange(NE, dtype=np.float32) * CAP + 1.0).reshape(1, NE)
    w1 = np.asarray(inp["w1"])[0]
    w1 = w1.reshape(NE, 16, 128, 16, 128, 2)
    sh["w1r"] = np.ascontiguousarray(w1.transpose(0, 3, 2, 1, 5, 4)).reshape(NE, 16, 128, 4096)
    w2 = np.asarray(inp["w2"])[0].reshape(NE, 16, 128, 8, 256)
    sh["w2r"] = np.ascontiguousarray(w2.transpose(0, 3, 2, 1, 4)).reshape(NE, 8, 128, 4096)
    b1 = np.asarray(inp["b1"])[0].reshape(NE, 16, 128, 2)
    sh["b1r"] = np.ascontiguousarray(b1.transpose(2, 0, 3, 1)).reshape(128, NE * 32)
    sh["b2"] = np.ascontiguousarray(np.asarray(inp["b2"])[0])
    sh["wOM"] = lhs_blocks(np.asarray(inp["w_o_mla"])[0], np.arange(2048)).reshape(16, 128, 2048)
    sh["wOD"] = lhs_blocks(np.asarray(inp["w_o_dil"])[0], np.arange(2048)).reshape(16, 128, 1536)
    return sh


def make_in_maps(inp):
    sh = prep_shared(inp)
    x = np.asarray(inp["x"])
    maps = []
    for c in range(NCORES):
        m = dict(sh)
        m["x"] = np.ascontiguousarray(x[c])
        m["xT"] = np.ascontiguousarray(x[c].T)
        maps.append(m)
    return maps


def kernel(**inputs):
    nc = build()
    in_maps = make_in_maps(inputs)
    res = run_bass_kernel_spmd(nc, in_maps, core_ids=list(range(NCORES)))
    return np.stack([np.asarray(r["out"]) for r in res.results], axis=0).astype(np.float32)
```

```python
import math
from contextlib import ExitStack

import numpy as np
import ml_dtypes
import concourse.bass as bass
import concourse.mybir as mybir
from concourse.bass_utils import run_bass_kernel_spmd

F32 = mybir.dt.float32
BF16 = mybir.dt.bfloat16
I32 = mybir.dt.int32
U32 = mybir.dt.uint32
AF = mybir.ActivationFunctionType
ALU = mybir.AluOpType
AX = mybir.AxisListType

S = 2048
D = 2048
NCORES = 8
QR, KVR, ROPE = 768, 512, 64
MH = 16
DH = 12
NE = 32
CAP = 384
NST = CAP // 128
DFF = 2048
DN_ALPHA = 2.0 ** 0.25
LN_EPS = 1e-5
RMS_EPS = 1e-6
DIL = ((2048, 1), (512, 4), (128, 16))


class Buf:
    __slots__ = ("name", "ws", "rs")

    def __init__(self, name=""):
        self.name = name
        self.ws = []
        self.rs = []


class DSem:
    __slots__ = ("sem", "count")

    def __init__(self, sem):
        self.sem = sem
        self.count = 0


class Op:
    __slots__ = ("eng", "fn", "deps", "sig", "idx", "dsem", "dval", "waits", "ph")


class Phase:
    ENGS = ("pe", "act", "dve", "pool", "sp")

    POOL = None

    def __init__(self, nc, name):
        self.nc = nc
        self.name = name
        self.ops = []
        pool = Phase.POOL
        if pool is None or pool["nc"] is not nc:
            st = ExitStack()
            pool = Phase.POOL = {"nc": nc, "stack": st, "esem": {}, "ebase": {}, "dsems": []}
            for e in ("pe", "act", "dve", "pool"):
                pool["esem"][e] = st.enter_context(nc.semaphore(f"sem_{e}"))
                pool["ebase"][e] = 0
        self.pool = pool
        self.esem = pool["esem"]
        self.dsems = []
        self.nd = 0

    def dsem(self):
        pool = self.pool
        if self.nd == len(pool["dsems"]):
            pool["dsems"].append(DSem(pool["stack"].enter_context(self.nc.semaphore(f"sem_d{self.nd}"))))
        d = pool["dsems"][self.nd]
        self.nd += 1
        self.dsems.append(d)
        return d

    def op(self, eng, fn, r=(), w=(), dsem=None):
        o = Op()
        o.eng, o.fn, o.sig, o.idx, o.dsem, o.dval, o.waits = eng, fn, False, 0, dsem, 0, None
        o.ph = self
        isdma = dsem is not None
        if isdma:
            dsem.count += 16
            o.dval = dsem.count
        deps = {}

        def add(p, raw):
            if p is o or p.ph is not self:
                return
            if (not isdma) and p.dsem is None and p.eng == eng:
                if not raw or eng == "pe":
                    return
            deps[id(p)] = p

        for b in r:
            for p in b.ws:
                add(p, True)
        for b in w:
            for p in b.ws:
                add(p, False)
            for p in b.rs:
                add(p, False)
        o.deps = list(deps.values())
        for p in o.deps:
            if p.dsem is None:
                p.sig = True
        for b in w:
            if b.rs:
                b.ws = [o]
                b.rs = []
            else:
                b.ws = [p for p in b.ws if not (p.dsem is None and p.eng == eng and not isdma)] + [o]
        for b in r:
            if isdma:
                b.rs.append(o)
            else:
                b.rs = [p for p in b.rs if not (p.dsem is None and p.eng == eng and p.ph is self)] + [o]
        self.ops.append(o)
        return o

    def dma(self, out, in_, r=(), w=(), dsem=None, eng="sp", **kw):
        return self.op(eng, lambda e: e.dma_start(out=out, in_=in_, **kw), r, w, dsem=dsem)

    def mm(self, out, lhsT, rhs, start, stop, r=(), w=()):
        return self.op("pe", lambda e: e.matmul(out, lhsT, rhs, start=start, stop=stop), r, w)

    def finalize(self):
        nc = self.nc
        cnt = {e: self.pool["ebase"].get(e, 0) for e in self.ENGS}
        for o in self.ops:
            if o.dsem is None and o.sig:
                cnt[o.eng] += 1
                o.idx = cnt[o.eng]
        for e in self.pool["ebase"]:
            self.pool["ebase"][e] = cnt[e]
        seen = {}
        for o in self.ops:
            need = {}
            for p in o.deps:
                if p.dsem is not None:
                    key, sem, val = ("d", id(p.dsem)), p.dsem.sem, p.dval
                else:
                    key, sem, val = ("e", p.eng), self.esem[p.eng], p.idx
                if val > need.get(key, (None, 0))[1]:
                    need[key] = (sem, val)
            o.waits = []
            for key, (sem, val) in need.items():
                if val > seen.get((o.eng, key), 0):
                    seen[(o.eng, key)] = val
                    o.waits.append((sem, val))
        by = {e: [o for o in self.ops if o.eng == e] for e in self.ENGS}
        esem = self.esem
        dsems = self.dsems

        def mk(en):
            def body(e):
                for o in by[en]:
                    for sem, val in o.waits:
                        e.wait_ge(sem, val)
                    ins = o.fn(e)
                    if o.dsem is not None:
                        ins.then_inc(o.dsem.sem, 16)
                    elif o.sig:
                        ins.then_inc(esem[en], 1)
                if en == "sp":
                    for d in dsems:
                        if d.count:
                            e.wait_ge(d.sem, d.count)
            return body

        with nc.Block() as blk:
            blk.tensor(mk("pe"))
            blk.scalar(mk("act"))
            blk.vector(mk("dve"))
            blk.gpsimd(mk("pool"))
            blk.sync(mk("sp"))


class WStream:
    def __init__(self, P, srcs, wst, wbf, cast_fn):
        self.P, self.srcs, self.wst, self.wbf, self.cast_fn = P, srcs, wst, wbf, cast_fn
        self.st = {}
        self.bf = {}
        self.nl = 0
        self.ncst = 0

    def _cast(self, upto):
        upto = min(upto, len(self.srcs) - 1)
        while self.ncst <= upto:
            j = self.ncst
            self._load(j)
            t, b = self.st.pop(j)
            wt, wb, _ = self.wbf.next()
            self.cast_fn(j, t, b, wt, wb)
            self.bf[j] = (wt, wb)
            self.ncst += 1

    def _load(self, upto):
        upto = min(upto, len(self.srcs) - 1)
        while self.nl <= upto:
            k = self.nl
            self._cast(k - len(self.wst.tiles))
            t, b, ds = self.wst.next()
            self.P.dma(t[:], self.srcs[k], w=[b], dsem=ds)
            self.st[k] = (t, b)
            self.nl += 1

    def want(self, last_needed, ahead_load=2, ahead_cast=1):
        self._cast(last_needed)
        self._load(last_needed + ahead_load)
        self._cast(last_needed + ahead_cast)

    def get(self, i):
        return self.bf[i]


class Ring:
    def __init__(self, P, tiles, with_dsem=False):
        self.tiles = tiles
        self.bufs = [Buf() for _ in tiles]
        self.ds = [P.dsem() for _ in tiles] if with_dsem else None
        self.i = -1

    def next(self):
        self.i += 1
        k = self.i % len(self.tiles)
        return self.tiles[k], self.bufs[k], (self.ds[k] if self.ds else None)


def build(stop_after=None, debug=(), nblkA=67, doV=True, nhB2=16):
    nc = bass.Bass("TRN2", target_bir_lowering=False)

    def din(name, shape, dt=F32):
        return nc.dram_tensor(name, list(shape), dt, kind="ExternalInput").ap()

    def dscr(name, shape, dt):
        kind = "ExternalOutput" if name in debug else "Internal"
        return nc.dram_tensor(name, list(shape), dt, kind=kind).ap()

    xT = din("xT", [D, S])
    x_tm = din("x", [S, D])
    wA = din("wA", [67, 128, 16, 128])
    wV = din("wV", [12, 128, 4, 512])
    out = nc.dram_tensor("out", [S, D], F32, kind="ExternalOutput").ap()

    lat = dscr("lat", [1408, S], BF16)
    qd = dscr("qd", [1536, S], BF16)
    kd = dscr("kd", [1536, S], BF16)
    vd = dscr("vd", [3, S, 512], BF16)
    sg = dscr("sg", [4096, S], F32)

    es = ExitStack()

    def sb(name, shape, dt):
        return es.enter_context(nc.sbuf_tensor(name, list(shape), dt))

    def ps(name, shape, dt=F32):
        return es.enter_context(nc.psum_tensor(name, list(shape), dt))

    with es:
        P = Phase(nc, "A")
        XT = sb("XT", [128, 16, S], BF16)
        XTb = [Buf() for _ in range(16)]
        xst = Ring(P, [sb(f"xst{i}", [128, S], F32) for i in range(2)], True)
        wst = Ring(P, [sb(f"wst{i}", [128, 2048], F32) for i in range(3)], True)
        wbf = Ring(P, [sb(f"wbf{i}", [128, 2048], BF16) for i in range(6)])
        obf = Ring(P, [sb(f"obf{i}", [128, S], BF16) for i in range(2)], True)
        of32 = Ring(P, [sb(f"of{i}", [128, S], F32) for i in range(2)], True)
        ovd = Ring(P, [sb(f"ovd{i}", [128, 512], BF16) for i in range(4)], True)
        pbanks = Ring(P, [ps(f"pa{i}", [128, 512]) for i in range(8)])

        for kc in range(16):
            t, b, ds = xst.next()
            P.dma(t[:], xT[kc * 128:(kc + 1) * 128, :], w=[b], dsem=ds)
            eng = "dve" if kc % 2 == 0 else "pool"
            P.op(eng, lambda e, t=t, kc=kc: e.tensor_copy(out=XT[:, kc, :], in_=t[:]), r=[b], w=[XTb[kc]])

        cast_i = [0]

        def castA(j, t, b, t2, b2):
            eng = ("dve", "pool")[cast_i[0] % 2]
            cast_i[0] += 1
            P.op(eng, lambda e, t=t, t2=t2: e.tensor_copy(out=t2[:], in_=t[:]), r=[b], w=[b2])

        blkA = list(nblkA) if isinstance(nblkA, (list, tuple)) else list(range(nblkA))
        srcsA = [wA[i].rearrange("p k c -> p (k c)") for i in blkA]
        nA = len(srcsA)
        if doV:
            srcsA += [wV[j].rearrange("p k c -> p (k c)") for j in range(12)]
        wsA = WStream(P, srcsA, wst, wbf, castA)

        evac_i = [0]
        for bi_, blk_i in enumerate(blkA):
            wsA.want(bi_)
            wt, wb = wsA.get(bi_)
            wv = wt[:].rearrange("p (k c) -> p k c", k=16)
            banks = [pbanks.next() for _ in range(4)]
            for kc in range(16):
                for tb in range(4):
                    pt, pb, _ = banks[tb]
                    P.mm(pt[:], wv[:, kc, :], XT[:, kc, tb * 512:(tb + 1) * 512], kc == 0, kc == 15,
                         r=[wb, XTb[kc]], w=[pb])
            if blk_i < 11:
                kind, dst = "lat", lat[blk_i * 128:(blk_i + 1) * 128, :]
            elif blk_i < 23:
                kind, hd, dst = "qk", blk_i - 11, qd[(blk_i - 11) * 128:(blk_i - 10) * 128, :]
            elif blk_i < 35:
                kind, hd, dst = "qk", blk_i - 23, kd[(blk_i - 23) * 128:(blk_i - 22) * 128, :]
            else:
                kind, dst = "gate", sg[(blk_i - 35) * 128:(blk_i - 34) * 128, :]
            if kind == "gate":
                ot, ob, ods = of32.next()
            else:
                ot, ob, ods = obf.next()
            for tb in range(4):
                pt, pb, _ = banks[tb]
                if kind == "gate":
                    P.op("act", lambda e, pt=pt, ot=ot, tb=tb: e.activation(
                        out=ot[:, tb * 512:(tb + 1) * 512], in_=pt[:], func=AF.Sigmoid), r=[pb], w=[ob])
                    continue
                if kind == "qk" and hd >= 4:
                    dil = 4 if hd < 8 else 16
                    ni = 512 // dil
                    o_ap = ot[:].rearrange("p (r i) -> p r i", r=dil)[:, :, tb * ni:(tb + 1) * ni]
                    i_ap = pt[:].rearrange("p (i r) -> p r i", r=dil)
                else:
                    o_ap = ot[:, tb * 512:(tb + 1) * 512]
                    i_ap = pt[:]
                if evac_i[0] % 2 == 0:
                    P.op("dve", lambda e, o_ap=o_ap, i_ap=i_ap: e.tensor_copy(out=o_ap, in_=i_ap), r=[pb], w=[ob])
                else:
                    P.op("act", lambda e, o_ap=o_ap, i_ap=i_ap: e.activation(out=o_ap, in_=i_ap, func=AF.Copy),
                         r=[pb], w=[ob])
                evac_i[0] += 1
            P.dma(dst, ot[:], r=[ob], dsem=ods)

        for g in (range(3) if doV else ()):
            n, dil = DIL[g]
            wsA.want(nA + g * 4 + 3)
            wts = [wsA.get(nA + g * 4 + kq) for kq in range(4)]
            for T in range(16):
                r_, blk_ = divmod(T, n // 128)
                pt, pb, _ = pbanks.next()
                for kc in range(16):
                    wt, wb = wts[kc // 4]
                    rhs = wt[:].rearrange("p (k c) -> p k c", k=4)[:, kc % 4, :]
                    lhsT = XT[:, kc, :].rearrange("p (i r) -> p r i", r=dil)[:, r_, blk_ * 128:(blk_ + 1) * 128]
                    P.mm(pt[:], lhsT, rhs, kc == 0, kc == 15, r=[wb, XTb[kc]], w=[pb])
                ot, ob, ods = ovd.next()
                if T % 2 == 0:
                    P.op("dve", lambda e, ot=ot, pt=pt: e.tensor_copy(out=ot[:], in_=pt[:]), r=[pb], w=[ob])
                else:
                    P.op("act", lambda e, ot=ot, pt=pt: e.activation(out=ot[:], in_=pt[:], func=AF.Copy),
                         r=[pb], w=[ob])
                P.dma(vd[g, T * 128:(T + 1) * 128, :], ot[:], r=[ob], dsem=ods)
        P.finalize()
    if stop_after == "A":
        return nc

    wUQ = din("wUQ", [16, 128, 6 * 384])
    gq = din("gq", [128, 6])
    wUK = din("wUK", [16, 128, 4 * 128])
    wUV = din("wUV", [4, 128, 2048])
    gkv = din("gkv", [128, 4])
    cosT = din("cosT", [64, S])
    sinT = din("sinT", [64, S])
    maskc = din("maskc", [128, 128], BF16)
    qm = dscr("qm", [16, 192, S], BF16)
    km = dscr("km", [16, 128, S], BF16)
    krot = dscr("krot", [64, S], BF16)
    vm = dscr("vm", [S, 2048], BF16)
    om = dscr("om", [2048, S], BF16)

    es = ExitStack()
    with es:
        LAT = sb("LAT", [128, 10, S], BF16)
        LATb = [Buf() for _ in range(10)]
        KPE = sb("KPE", [64, S], BF16)
        KPEP = sb("KPEP", [64, S], BF16)
        COS = sb("COS", [64, S], F32)
        SIN = sb("SIN", [64, S], F32)
        GQ = sb("GQ", [128, 6], F32)
        GKV = sb("GKV", [128, 4], F32)
        ONESF = sb("ONESF", [128, 128], F32)
        cb_ = Buf()
        es1 = ExitStack()
        with es1:
            sb1 = lambda n, sh, dt: es1.enter_context(nc.sbuf_tensor(n, list(sh), dt))
            ps1 = lambda n, sh, dt=F32: es1.enter_context(nc.psum_tensor(n, list(sh), dt))
            P = Phase(nc, "B1")
            P.dma(LAT[:], lat[0:1280, :].rearrange("(k p) s -> p k s", p=128), w=LATb, dsem=P.dsem())
            P.dma(KPE[:], lat[1280:1344, :], w=[cb_], dsem=P.dsem())
            P.dma(KPEP[:], lat[1344:1408, :], w=[cb_], dsem=P.dsem())
            P.dma(COS[:], cosT, w=[cb_], dsem=P.dsem())
            P.dma(SIN[:], sinT, w=[cb_], dsem=P.dsem())
            P.dma(GQ[:], gq, w=[cb_], dsem=P.dsem())
            P.dma(GKV[:], gkv, w=[cb_], dsem=P.dsem())
            P.op("pool", lambda e: e.memset(ONESF[:], 1.0), w=[cb_])
            sq = Ring(P, [sb1(f"sq{i}", [128, 512], F32) for i in range(3)])
            rr = Ring(P, [sb1(f"rr{i}", [128, 512], F32) for i in range(2)])
            pss = Ring(P, [ps1(f"pss{i}", [128, 512]) for i in range(2)])
            k_ = 0
            for (c0, ncn, nfeat) in ((0, 6, 768), (6, 4, 512)):
                for tb in range(4):
                    pt, pb, _ = pss.next()
                    for kc in range(ncn):
                        st, sbuf_, _ = sq.next()
                        src = LAT[:, c0 + kc, tb * 512:(tb + 1) * 512]
                        if k_ % 2 == 0:
                            P.op("act", lambda e, st=st, src=src: e.activation(out=st[:], in_=src, func=AF.Square),
                                 r=[LATb[c0 + kc]], w=[sbuf_])
                        else:
                            P.op("dve", lambda e, st=st, src=src: e.tensor_tensor(out=st[:], in0=src, in1=src, op=ALU.mult),
                                 r=[LATb[c0 + kc]], w=[sbuf_])
                        k_ += 1
                        P.mm(pt[:], ONESF[:], st[:], kc == 0, kc == ncn - 1, r=[sbuf_, cb_], w=[pb])
                    rt, rb, _ = rr.next()
                    P.op("act", lambda e, rt=rt, pt=pt, nfeat=nfeat: e.activation(
                        out=rt[:], in_=pt[:], func=AF.Sqrt, bias=RMS_EPS, scale=1.0 / nfeat), r=[pb], w=[rb])
                    P.op("dve", lambda e, rt=rt: e.reciprocal(out=rt[:], in_=rt[:]), r=[rb], w=[rb])
                    for kc in range(ncn):
                        src = LAT[:, c0 + kc, tb * 512:(tb + 1) * 512]
                        eng = "dve" if kc % 2 == 0 else "pool"
                        P.op(eng, lambda e, src=src, rt=rt: e.tensor_tensor(out=src, in0=src, in1=rt[:], op=ALU.mult),
                             r=[rb, LATb[c0 + kc]], w=[LATb[c0 + kc]])
            WV = sb1("WV", [128, 4, 2048], BF16)
            WVb = Buf()
            wvs = Ring(P, [sb1(f"wvs{i}", [128, 2048], F32) for i in range(2)], True)
            for kc in range(4):
                t, b, ds = wvs.next()
                P.dma(t[:], wUV[kc], w=[b], dsem=ds)
                P.op("dve" if kc % 2 == 0 else "pool", lambda e, t=t, kc=kc: e.tensor_scalar(
                    out=WV[:, kc, :], in0=t[:], scalar1=GKV[:, kc:kc + 1], scalar2=None, op0=ALU.mult),
                    r=[b, cb_], w=[WVb])
            pv = Ring(P, [ps1(f"pv{i}", [128, 512]) for i in range(4)])
            vt = Ring(P, [sb1(f"vt{i}", [128, 2048], BF16) for i in range(2)], True)
            ev = 0
            for t in range(16):
                ot, ob, ods = vt.next()
                for hb in range(4):
                    pt, pb, _ = pv.next()
                    for kc in range(4):
                        P.mm(pt[:], LAT[:, 6 + kc, t * 128:(t + 1) * 128], WV[:, kc, hb * 512:(hb + 1) * 512],
                             kc == 0, kc == 3, r=[LATb[6 + kc], WVb], w=[pb])
                    o_ap = ot[:, hb * 512:(hb + 1) * 512]
                    if ev % 2 == 0:
                        P.op("dve", lambda e, o_ap=o_ap, pt=pt: e.tensor_copy(out=o_ap, in_=pt[:]), r=[pb], w=[ob])
                    else:
                        P.op("act", lambda e, o_ap=o_ap, pt=pt: e.activation(out=o_ap, in_=pt[:], func=AF.Copy), r=[pb], w=[ob])
                    ev += 1
                P.dma(vm[t * 128:(t + 1) * 128, :], ot[:], r=[ob], dsem=ods, eng="act")
            wks = Ring(P, [sb1(f"wks{i}", [128, 512], F32) for i in range(3)], True)
            wkb = Ring(P, [sb1(f"wkb{i}", [128, 512], BF16) for i in range(3)])
            kn = Ring(P, [sb1(f"kn{i}", [128, S], BF16) for i in range(2)], True)

            def castK(j, t, b, t2, b2):
                for kc in range(4):
                    P.op("pool" if kc % 2 == 0 else "dve", lambda e, t=t, t2=t2, kc=kc: e.tensor_scalar(
                        out=t2[:, kc * 128:(kc + 1) * 128], in0=t[:, kc * 128:(kc + 1) * 128],
                        scalar1=GKV[:, kc:kc + 1], scalar2=None, op0=ALU.mult), r=[b, cb_], w=[b2])

            wsK = WStream(P, [wUK[h] for h in range(16)], wks, wkb, castK)
            for h in range(16):
                wsK.want(h)
                t2, b2 = wsK.get(h)
                ot, ob, ods = kn.next()
                for tb in range(4):
                    pt, pb, _ = pv.next()
                    for kc in range(4):
                        P.mm(pt[:], t2[:, kc * 128:(kc + 1) * 128], LAT[:, 6 + kc, tb * 512:(tb + 1) * 512],
                             kc == 0, kc == 3, r=[b2, LATb[6 + kc]], w=[pb])
                    o_ap = ot[:, tb * 512:(tb + 1) * 512]
                    if ev % 2 == 0:
                        P.op("dve", lambda e, o_ap=o_ap, pt=pt: e.tensor_copy(out=o_ap, in_=pt[:]), r=[pb], w=[ob])
                    else:
                        P.op("act", lambda e, o_ap=o_ap, pt=pt: e.activation(out=o_ap, in_=pt[:], func=AF.Copy), r=[pb], w=[ob])
                    ev += 1
                P.dma(km[h], ot[:], r=[ob], dsem=ods, eng="act")
            P.finalize()
        if stop_after == "B1":
            return nc
        es2 = ExitStack()
        with es2:
            sb1 = lambda n, sh, dt: es2.enter_context(nc.sbuf_tensor(n, list(sh), dt))
            ps1 = lambda n, sh, dt=F32: es2.enter_context(nc.psum_tensor(n, list(sh), dt))
            P = Phase(nc, "B2")
            tmp = Ring(P, [sb1(f"tmp{i}", [64, 512], F32) for i in range(4)])
            KR = sb1("KR", [64, S], BF16)
            KRb = Buf()
            for tb in range(4):
                sl = slice(tb * 512, (tb + 1) * 512)
                t1, b1_, _ = tmp.next()
                t2, b2_, _ = tmp.next()
                P.op("dve", lambda e, t1=t1, sl=sl: e.tensor_tensor(out=t1[:], in0=KPE[:, sl], in1=COS[:, sl], op=ALU.mult), w=[b1_])
                P.op("pool", lambda e, t2=t2, sl=sl: e.tensor_tensor(out=t2[:], in0=KPEP[:, sl], in1=SIN[:, sl], op=ALU.mult), w=[b2_])
                P.op("dve", lambda e, t1=t1, t2=t2, sl=sl: e.tensor_tensor(out=KR[:, sl], in0=t1[:], in1=t2[:], op=ALU.add),
                     r=[b1_, b2_], w=[KRb])
            P.dma(krot, KR[:], r=[KRb], dsem=P.dsem())
            wqs = Ring(P, [sb1(f"wqs{i}", [128, 2304], F32) for i in range(3)], True)
            wqb = Ring(P, [sb1(f"wqb{i}", [128, 2304], BF16) for i in range(3)])
            qn = Ring(P, [sb1(f"qn{i}", [128, S], BF16) for i in range(2)], True)
            qr = Ring(P, [sb1(f"qr{i}", [64, S], BF16) for i in range(2)], True)
            pqn = Ring(P, [ps1(f"pqn{i}", [128, 512]) for i in range(2)])
            pqa = Ring(P, [ps1(f"pqa{i}", [128, 512]) for i in range(2)])
            pqb = Ring(P, [ps1(f"pqb{i}", [128, 512]) for i in range(2)])
            ev = 0

            def castQ(j, t, b, t2, b2):
                for kc in range(6):
                    P.op("pool" if kc % 3 == 0 else "dve", lambda e, t=t, t2=t2, kc=kc: e.tensor_scalar(
                        out=t2[:, kc * 384:(kc + 1) * 384], in0=t[:, kc * 384:(kc + 1) * 384],
                        scalar1=GQ[:, kc:kc + 1], scalar2=None, op0=ALU.mult), r=[b], w=[b2])

            wsQ = WStream(P, [wUQ[h] for h in range(nhB2)], wqs, wqb, castQ)
            for h in range(nhB2):
                wsQ.want(h)
                t2, b2 = wsQ.get(h)
                w3 = t2[:].rearrange("p (k c) -> p k c", k=6)
                qnt, qnb, qnd = qn.next()
                qrt, qrb, qrd = qr.next()
                for tb in range(4):
                    sl = slice(tb * 512, (tb + 1) * 512)
                    p1, pb1, _ = pqn.next()
                    p2, pb2, _ = pqa.next()
                    p3, pb3, _ = pqb.next()
                    for kc in range(6):
                        P.mm(p1[:], w3[:, kc, 0:128], LAT[:, kc, sl], kc == 0, kc == 5, r=[b2, LATb[kc]], w=[pb1])
                    for kc in range(6):
                        P.mm(p2[:], w3[:, kc, 128:256], LAT[:, kc, sl], kc == 0, kc == 5, r=[b2, LATb[kc]], w=[pb2])
                    for kc in range(6):
                        P.mm(p3[:], w3[:, kc, 256:384], LAT[:, kc, sl], kc == 0, kc == 5, r=[b2, LATb[kc]], w=[pb3])
                    P.op("act", lambda e, qnt=qnt, p1=p1, sl=sl: e.activation(out=qnt[:, sl], in_=p1[:], func=AF.Copy),
                         r=[pb1], w=[qnb])
                    t1, b1_, _ = tmp.next()
                    t2_, b2_, _ = tmp.next()
                    P.op("dve", lambda e, t1=t1, p2=p2, sl=sl: e.tensor_tensor(out=t1[:], in0=p2[0:64, :], in1=COS[:, sl], op=ALU.mult),
                         r=[pb2], w=[b1_])
                    P.op("dve", lambda e, t2_=t2_, p3=p3, sl=sl: e.tensor_tensor(out=t2_[:], in0=p3[0:64, :], in1=SIN[:, sl], op=ALU.mult),
                         r=[pb3], w=[b2_])
                    P.op("pool", lambda e, t1=t1, t2_=t2_, qrt=qrt, sl=sl: e.tensor_tensor(out=qrt[:, sl], in0=t1[:], in1=t2_[:], op=ALU.add),
                         r=[b1_, b2_], w=[qrb])
                P.dma(qm[h, 0:128, :], qnt[:], r=[qnb], dsem=qnd, eng="act")
                P.dma(qm[h, 128:192, :], qrt[:], r=[qrb], dsem=qrd, eng="act")
            P.finalize()
    if stop_after == "B":
        return nc

    es = ExitStack()
    with es:
        P = Phase(nc, "C")
        VALL = sb("VALL", [128, 16, 2048], BF16)
        KROT = sb("KROT", [64, S], BF16)
        MASKC = sb("MASKC", [128, 128], BF16)
        ONESB = sb("ONESB", [128, 128], BF16)
        cb_ = Buf()
        P.dma(VALL[:], vm.rearrange("(t p) c -> p t c", p=128), w=[cb_], dsem=P.dsem())
        P.dma(KROT[:], krot, w=[cb_], dsem=P.dsem())
        P.dma(MASKC[:], maskc, w=[cb_], dsem=P.dsem())
        P.op("pool", lambda e: e.memset(ONESB[:], 1.0), w=[cb_])
        qn = Ring(P, [sb(f"cqn{i}", [128, S], BF16) for i in range(2)], True)
        qr = Ring(P, [sb(f"cqr{i}", [64, S], BF16) for i in range(2)], True)
        kn = Ring(P, [sb(f"ckn{i}", [128, S], BF16) for i in range(2)], True)
        pr = Ring(P, [sb(f"cp{i}", [128, 512], BF16) for i in range(3)])
        oh = Ring(P, [sb(f"coh{i}", [128, S], BF16) for i in range(2)], True)
        rz = Ring(P, [sb(f"crz{i}", [128, 512], F32) for i in range(2)])
        pS = Ring(P, [ps(f"cS{i}", [128, 512]) for i in range(2)])
        pO = Ring(P, [ps(f"cO{i}", [128, 512]) for i in range(2)])
        pZ = Ring(P, [ps(f"cZ{i}", [128, 512]) for i in range(2)])
        sc_mla = 192.0 ** -0.5
        def loadC(h):
            qnt, qnb, d1 = qn.next()
            qrt, qrb, d2 = qr.next()
            knt, knb, d3 = kn.next()
            P.dma(qnt[:], qm[h, 0:128, :], w=[qnb], dsem=d1)
            P.dma(qrt[:], qm[h, 128:192, :], w=[qrb], dsem=d2)
            P.dma(knt[:], km[h], w=[knb], dsem=d3)
            return qnt, qnb, qrt, qrb, knt, knb

        headC = {}

        def need_head(h):
            if h < 16 and h not in headC:
                headC[h] = loadC(h)

        itemsC = [(h, Q, j) for h in range(16) for Q in range(4) for j in range(4 * Q + 4)]
        stC = {}

        def emitS(it):
            h, Q, j = it
            if Q == 0 and j == 0:
                need_head(h)
                need_head(h + 1)
            qnt, qnb, qrt, qrb, knt, knb = headC[h]
            q0 = max(512 * Q, 128 * j)
            wd = 512 * Q + 512 - q0
            pst, psb, _ = pS.next()
            P.mm(pst[:, 0:wd], knt[:, j * 128:(j + 1) * 128], qnt[:, q0:q0 + wd], True, False, r=[knb, qnb], w=[psb])
            P.mm(pst[:, 0:wd], KROT[:, j * 128:(j + 1) * 128], qrt[:, q0:q0 + wd], False, True, r=[cb_, qrb], w=[psb])
            pt, ptb, _ = pr.next()
            P.op("act", lambda e, pt=pt, pst=pst, wd=wd: e.activation(out=pt[:, 0:wd], in_=pst[:, 0:wd], func=AF.Exp, scale=sc_mla),
                 r=[psb], w=[ptb])
            if j >= 4 * Q:
                P.op("pool", lambda e, pt=pt: e.tensor_tensor(out=pt[:, 0:128], in0=pt[:, 0:128], in1=MASKC[:], op=ALU.mult),
                     r=[ptb, cb_], w=[ptb])
            stC[it] = (pt, ptb, wd, q0 - 512 * Q)

        accC = {}

        def emitPV(it):
            h, Q, j = it
            pt, ptb, wd, c0 = stC.pop(it)
            nj = 4 * Q + 4
            if j == 0:
                if Q == 0:
                    accC["oh"] = oh.next()
                accC["o"] = pO.next()
                accC["z"] = pZ.next()
            po, pob, _ = accC["o"]
            pz, pzb, _ = accC["z"]
            oht, ohb, ohd = accC["oh"]
            P.mm(po[:, c0:c0 + wd], VALL[:, j, h * 128:(h + 1) * 128], pt[:, 0:wd], j == 0, j == nj - 1, r=[cb_, ptb], w=[pob])
            P.mm(pz[:, c0:c0 + wd], ONESB[:], pt[:, 0:wd], j == 0, j == nj - 1, r=[cb_, ptb], w=[pzb])
            if j == nj - 1:
                rt, rb, _ = rz.next()
                P.op("dve", lambda e, rt=rt, pz=pz: e.reciprocal(out=rt[:], in_=pz[:]), r=[pzb], w=[rb])
                P.op("dve", lambda e, oht=oht, po=po, rt=rt, Q=Q: e.tensor_tensor(
                    out=oht[:, Q * 512:(Q + 1) * 512], in0=po[:], in1=rt[:], op=ALU.mult), r=[pob, rb], w=[ohb])
                if Q == 3:
                    P.dma(om[h * 128:(h + 1) * 128, :], oht[:], r=[ohb], dsem=ohd, eng="act")

        emitS(itemsC[0])
        for i_, it in enumerate(itemsC):
            if i_ + 1 < len(itemsC):
                emitS(itemsC[i_ + 1])
            emitPV(it)
        P.finalize()
    if stop_after == "C":
        return nc

    dmask = din("dmask", [128, 20, 128])
    od = dscr("od", [1536, S], BF16)
    es = ExitStack()
    with es:
        P = Phase(nc, "D")
        VD = sb("VD", [128, 3, 16, 512], BF16)
        MASKS = sb("MASKS", [128, 20, 128], F32)
        ONESB = sb("ONESBd", [128, 128], BF16)
        cb_ = Buf()
        for g in range(3):
            P.dma(VD[:, g], vd[g].rearrange("(t p) c -> p t c", p=128), w=[cb_], dsem=P.dsem())
        P.dma(MASKS[:], dmask, w=[cb_], dsem=P.dsem())
        P.op("pool", lambda e: e.memset(ONESB[:], 1.0), w=[cb_])
        UN = [sb(f"UN{g}", [128, S], F32) for g in range(3)]
        ZN = [sb(f"ZN{g}", [128, S], F32) for g in range(3)]
        UNb = [Buf() for _ in range(3)]
        ZNb = [Buf() for _ in range(3)]
        RT = sb("RTd", [128, S], F32)
        RTb = Buf()
        qdr = Ring(P, [sb(f"dq{i}", [128, S], BF16) for i in range(2)], True)
        kdr = Ring(P, [sb(f"dk{i}", [128, S], BF16) for i in range(2)], True)
        ecr = Ring(P, [sb(f"dec{i}", [128, 512], F32) for i in range(2)])
        epr = Ring(P, [sb(f"dep{i}", [128, 512], F32) for i in range(2)])
        pcr = Ring(P, [sb(f"dpc{i}", [128, 512], BF16) for i in range(2)])
        ppr = Ring(P, [sb(f"dpp{i}", [128, 512], BF16) for i in range(2)])
        odr = Ring(P, [sb(f"dod{i}", [128, S], BF16) for i in range(2)], True)
        pSc = Ring(P, [ps(f"dSc{i}", [128, 512]) for i in range(2)])
        pSp = Ring(P, [ps(f"dSp{i}", [128, 512]) for i in range(2)])
        pU = Ring(P, [ps(f"dU{i}", [128, 512]) for i in range(2)])
        pZ = Ring(P, [ps(f"dZ{i}", [128, 512]) for i in range(2)])
        sc_d = 128.0 ** -0.5
        for hs in range(4):
            for g in range(3):
                hd = g * 4 + hs
                n, dil = DIL[g]
                nb = n // 128
                qt, qb, d1 = qdr.next()
                kt, kb, d2 = kdr.next()
                P.dma(qt[:], qd[hd * 128:(hd + 1) * 128, :], w=[qb], dsem=d1)
                P.dma(kt[:], kd[hd * 128:(hd + 1) * 128, :], w=[kb], dsem=d2)
                for c in range(4):
                    us = [4 * c + s_ for s_ in range(4)]
                    hp = [u % nb != 0 for u in us]
                    s0 = hp.index(True) if any(hp) else 4
                    assert all(hp[s0:])
                    sc, scb, _ = pSc.next()
                    for s_, u in enumerate(us):
                        P.mm(sc[:, s_ * 128:(s_ + 1) * 128], kt[:, u * 128:(u + 1) * 128], qt[:, u * 128:(u + 1) * 128],
                             True, True, r=[kb, qb], w=[scb])
                    ec, ecb, _ = ecr.next()
                    P.op("act", lambda e, ec=ec, sc=sc: e.activation(out=ec[:], in_=sc[:], func=AF.Exp, scale=sc_d), r=[scb], w=[ecb])
                    pc, pcb, _ = pcr.next()
                    P.op("dve", lambda e, pc=pc, ec=ec, hd=hd: e.tensor_tensor(
                        out=pc[:].rearrange("p (s i) -> p s i", s=4), in0=ec[:].rearrange("p (s i) -> p s i", s=4),
                        in1=MASKS[:, hd:hd + 1, :].to_broadcast([128, 4, 128]), op=ALU.mult), r=[ecb, cb_], w=[pcb])
                    if s0 < 4:
                        sp, spb, _ = pSp.next()
                        for s_ in range(s0, 4):
                            u = us[s_]
                            P.mm(sp[:, s_ * 128:(s_ + 1) * 128], kt[:, (u - 1) * 128:u * 128], qt[:, u * 128:(u + 1) * 128],
                                 True, True, r=[kb, qb], w=[spb])
                        ep, epb, _ = epr.next()
                        P.op("act", lambda e, ep=ep, sp=sp, s0=s0: e.activation(
                            out=ep[:, s0 * 128:512], in_=sp[:, s0 * 128:512], func=AF.Exp, scale=sc_d), r=[spb], w=[epb])
                        pp, ppb, _ = ppr.next()
                        ns = 4 - s0
                        P.op("dve", lambda e, pp=pp, ep=ep, hd=hd, s0=s0, ns=ns: e.tensor_tensor(
                            out=pp[:, s0 * 128:512].rearrange("p (s i) -> p s i", s=ns),
                            in0=ep[:, s0 * 128:512].rearrange("p (s i) -> p s i", s=ns),
                            in1=MASKS[:, 12 + hd:13 + hd, :].to_broadcast([128, ns, 128]), op=ALU.mult), r=[epb, cb_], w=[ppb])
                    pu, pub, _ = pU.next()
                    pz, pzb, _ = pZ.next()
                    for s_, u in enumerate(us):
                        sl = slice(s_ * 128, (s_ + 1) * 128)
                        P.mm(pu[:, sl], VD[:, g, u, hs * 128:(hs + 1) * 128], pc[:, sl], True, not hp[s_], r=[cb_, pcb], w=[pub])
                        if hp[s_]:
                            P.mm(pu[:, sl], VD[:, g, u - 1, hs * 128:(hs + 1) * 128], pp[:, sl], False, True, r=[cb_, ppb], w=[pub])
                    for s_, u in enumerate(us):
                        sl = slice(s_ * 128, (s_ + 1) * 128)
                        P.mm(pz[:, sl], ONESB[:], pc[:, sl], True, not hp[s_], r=[cb_, pcb], w=[pzb])
                        if hp[s_]:
                            P.mm(pz[:, sl], ONESB[:], pp[:, sl], False, True, r=[cb_, ppb], w=[pzb])

                    def nat(t):
                        if g == 0:
                            return t[:, c * 512:(c + 1) * 512], None
                        if g == 1:
                            return t[:].rearrange("p (i r) -> p r i", r=4)[:, c, :], None
                        return t[:].rearrange("p (i r) -> p r i", r=16)[:, 4 * c:4 * c + 4, :], 4

                    uo, rs_ = nat(UN[g])
                    zo, _ = nat(ZN[g])
                    ui = pu[:] if rs_ is None else pu[:].rearrange("p (r i) -> p r i", r=4)
                    zi = pz[:] if rs_ is None else pz[:].rearrange("p (r i) -> p r i", r=4)
                    P.op("act", lambda e, uo=uo, ui=ui: e.activation(out=uo, in_=ui, func=AF.Copy), r=[pub], w=[UNb[g]])
                    P.op("dve", lambda e, zo=zo, zi=zi: e.tensor_copy(out=zo, in_=zi), r=[pzb], w=[ZNb[g]])
            P.op("pool", lambda e: e.tensor_tensor(out=RT[:], in0=ZN[0][:], in1=ZN[1][:], op=ALU.add), r=[ZNb[0], ZNb[1]], w=[RTb])
            P.op("pool", lambda e: e.tensor_tensor(out=RT[:], in0=RT[:], in1=ZN[2][:], op=ALU.add), r=[RTb, ZNb[2]], w=[RTb])
            P.op("dve", lambda e: e.reciprocal(out=RT[:], in_=RT[:]), r=[RTb], w=[RTb])
            for g in range(3):
                ot, ob, ods = odr.next()
                P.op("pool" if g != 1 else "dve", lambda e, ot=ot, g=g: e.tensor_tensor(out=ot[:], in0=UN[g][:], in1=RT[:], op=ALU.mult),
                     r=[UNb[g], RTb], w=[ob])
                P.dma(od[(g * 4 + hs) * 128:(g * 4 + hs + 1) * 128, :], ot[:], r=[ob], dsem=ods, eng="act")
        P.finalize()
    if stop_after == "D":
        return nc

    wOM = din("wOM", [16, 128, 2048])
    wOD = din("wOD", [16, 128, 1536])
    p1s = dscr("p1s", [2048, S], F32)
    mgd = dscr("mgd", [2048, S], BF16) if "mgd" in debug else None
    es = ExitStack()
    with es:
        P = Phase(nc, "E1a")
        OM = sb("OM", [128, 16, S], BF16)
        OMb = Buf()
        P.dma(OM[:], om.rearrange("(k p) s -> p k s", p=128), w=[OMb], dsem=P.dsem())
        wst = Ring(P, [sb(f"e1ws{i}", [128, 2048], F32) for i in range(3)], True)
        wbf = Ring(P, [sb(f"e1wb{i}", [128, 2048], BF16) for i in range(3)])
        sgr = Ring(P, [sb(f"e1sg{i}", [128, S], F32) for i in range(2)], True)
        otr = Ring(P, [sb(f"e1o{i}", [128, S], F32) for i in range(2)], True)
        pY = Ring(P, [ps(f"e1p{i}", [128, 512]) for i in range(4)])
        def castE(j, t, b, wt, wb):
            P.op("pool", lambda e, t=t, wt=wt: e.tensor_copy(out=wt[:, 0:768], in_=t[:, 0:768]), r=[b], w=[wb])
            P.op("act", lambda e, t=t, wt=wt: e.activation(out=wt[:, 768:], in_=t[:, 768:], func=AF.Copy), r=[b], w=[wb])

        wsE = WStream(P, [wOM[m] for m in range(16)], wst, wbf, castE)
        for m in range(16):
            wsE.want(m)
            wt, wb = wsE.get(m)
            st, sbf, sds = sgr.next()
            P.dma(st[:], sg[m * 128:(m + 1) * 128, :], w=[sbf], dsem=sds)
            ot, ob, ods = otr.next()
            for tb in range(4):
                sl = slice(tb * 512, (tb + 1) * 512)
                pt, pb, _ = pY.next()
                for kc in range(16):
                    P.mm(pt[:], wt[:, kc * 128:(kc + 1) * 128], OM[:, kc, sl], kc == 0, kc == 15, r=[wb, OMb], w=[pb])
                P.op("dve", lambda e, ot=ot, pt=pt, st=st, sl=sl: e.tensor_tensor(out=ot[:, sl], in0=pt[:], in1=st[:, sl], op=ALU.mult),
                     r=[pb, sbf], w=[ob])
            P.dma(p1s[m * 128:(m + 1) * 128, :], ot[:], r=[ob], dsem=ods, eng="act")
        P.finalize()
    esMG = ExitStack()
    MG = esMG.enter_context(nc.sbuf_tensor("MG", [128, 16, S], BF16))
    es = ExitStack()
    with es:
        P = Phase(nc, "E1b")
        ODs = sb("ODs", [128, 12, S], BF16)
        ODb = Buf()
        MGb = Buf()
        P.dma(ODs[:], od.rearrange("(k p) s -> p k s", p=128), w=[ODb], dsem=P.dsem())
        wst = Ring(P, [sb(f"e2ws{i}", [128, 1536], F32) for i in range(3)], True)
        wbf = Ring(P, [sb(f"e2wb{i}", [128, 1536], BF16) for i in range(3)])
        sgr = Ring(P, [sb(f"e2sg{i}", [128, S], F32) for i in range(2)], True)
        p1r = Ring(P, [sb(f"e2p1{i}", [128, S], F32) for i in range(2)], True)
        tmr = Ring(P, [sb(f"e2t{i}", [128, 512], F32) for i in range(3)])
        pY = Ring(P, [ps(f"e2p{i}", [128, 512]) for i in range(4)])
        def castE(j, t, b, wt, wb):
            P.op("pool", lambda e, t=t, wt=wt: e.tensor_copy(out=wt[:, 0:512], in_=t[:, 0:512]), r=[b], w=[wb])
            P.op("act", lambda e, t=t, wt=wt: e.activation(out=wt[:, 512:], in_=t[:, 512:], func=AF.Copy), r=[b], w=[wb])

        wsE = WStream(P, [wOD[m] for m in range(16)], wst, wbf, castE)
        for m in range(16):
            wsE.want(m)
            wt, wb = wsE.get(m)
            st, sbf, sds = sgr.next()
            P.dma(st[:], sg[2048 + m * 128:2048 + (m + 1) * 128, :], w=[sbf], dsem=sds)
            p1t, p1b, p1d = p1r.next()
            P.dma(p1t[:], p1s[m * 128:(m + 1) * 128, :], w=[p1b], dsem=p1d)
            for tb in range(4):
                sl = slice(tb * 512, (tb + 1) * 512)
                pt, pb, _ = pY.next()
                for kc in range(12):
                    P.mm(pt[:], wt[:, kc * 128:(kc + 1) * 128], ODs[:, kc, sl], kc == 0, kc == 11, r=[wb, ODb], w=[pb])
                tt, tbf, _ = tmr.next()
                P.op("dve", lambda e, tt=tt, pt=pt, st=st, sl=sl: e.tensor_tensor(out=tt[:], in0=pt[:], in1=st[:, sl], op=ALU.mult),
                     r=[pb, sbf], w=[tbf])
                P.op("pool", lambda e, tt=tt, p1t=p1t, sl=sl, m=m: e.tensor_tensor(out=MG[:, m, sl], in0=tt[:], in1=p1t[:, sl], op=ALU.add),
                     r=[tbf, p1b], w=[MGb])
        if mgd is not None:
            P.dma(mgd.rearrange("(k p) s -> p k s", p=128), MG[:], r=[MGb], dsem=P.dsem())
        P.finalize()
    if stop_after == "E1":
        esMG.close()
        return nc

    wOUT = din("wOUT", [16, 128, 2048])
    hs_ = dscr("hs", [S, D], F32)
    es = ExitStack()
    with es:
        P = Phase(nc, "E2a")
        MGb = Buf()
        WOh = sb("WOh", [128, 16, 1024], BF16)
        WOb = [Buf() for _ in range(16)]
        wst = Ring(P, [sb(f"e3ws{i}", [128, 1024], F32) for i in range(3)], True)
        xr = Ring(P, [sb(f"e3x{i}", [128, 1024], F32) for i in range(2)], True)
        hr = Ring(P, [sb(f"e3h{i}", [128, 1024], F32) for i in range(2)], True)
        pH = Ring(P, [ps(f"e3p{i}", [128, 512]) for i in range(4)])
        ci = 0
        for half in range(2):
            hsl = slice(half * 1024, (half + 1) * 1024)
            for kc in range(16):
                t, b, ds = wst.next()
                P.dma(t[:], wOUT[kc][:, hsl], w=[b], dsem=ds)
                eng = ("dve", "pool")[ci % 2]
                ci += 1
                P.op(eng, lambda e, t=t, kc=kc: e.tensor_copy(out=WOh[:, kc, :], in_=t[:]), r=[b], w=[WOb[kc]])
            for t_ in range(16):
                xt, xb, xd = xr.next()
                P.dma(xt[:], x_tm[t_ * 128:(t_ + 1) * 128, hsl], w=[xb], dsem=xd)
                ht, hb, hd_ = hr.next()
                for cb in range(2):
                    csl = slice(cb * 512, (cb + 1) * 512)
                    pt, pb, _ = pH.next()
                    for kc in range(16):
                        P.mm(pt[:], MG[:, kc, t_ * 128:(t_ + 1) * 128], WOh[:, kc, csl], kc == 0, kc == 15,
                             r=[MGb, WOb[kc]], w=[pb])
                    P.op("dve", lambda e, ht=ht, xt=xt, pt=pt, csl=csl: e.scalar_tensor_tensor(
                        out=ht[:, csl], in0=xt[:, csl], scalar=DN_ALPHA, in1=pt[:], op0=ALU.mult, op1=ALU.add),
                        r=[xb, pb], w=[hb])
                P.dma(hs_[t_ * 128:(t_ + 1) * 128, hsl], ht[:], r=[hb], dsem=hd_, eng="act")
        P.finalize()
    esMG.close()
    if stop_after == "E2a":
        return nc

    ln1g = din("ln1_g", [1, D])
    ln1b = din("ln1_b", [1, D])
    ln2g = din("ln2_g", [1, D])
    ln2b = din("ln2_b", [1, D])
    wR = din("wR", [128, 16 * 32])
    bR = din("bR", [1, 32])
    identF = din("identF", [128, 128])
    identB = din("identB", [128, 128], BF16)
    tri = din("tri", [128, 128], BF16)
    eb1 = din("eb1", [1, 32])
    x1s = dscr("x1s", [S, D], F32)
    NROW = NE * CAP + 128
    DUMMY = NE * CAP
    xg = dscr("xg", [NROW, D], BF16)
    ysc = dscr("ysc", [NROW, D], F32)
    esR = ExitStack()
    DEST = esR.enter_context(nc.sbuf_tensor("DEST", [128, 16, 4], I32))
    GATE = esR.enter_context(nc.sbuf_tensor("GATE", [128, 16, 4], F32))

    def layer_norm(P, src, srcb, dst, dstb, G_, B_, gb_, small, t_):
        st, stb, mv, mvb, rs, rsb = small
        for c4 in range(4):
            P.op("dve", lambda e, c4=c4: e.bn_stats(out=st[:, c4, :], in_=src[:, c4 * 512:(c4 + 1) * 512]), r=[srcb], w=[stb])
        P.op("dve", lambda e: e.bn_aggr(out=mv[:], in_=st[:].rearrange("p a b -> p (a b)")), r=[stb], w=[mvb])
        P.op("dve", lambda e: e.tensor_scalar(out=rs[:], in0=mv[:, 1:2], scalar1=LN_EPS, scalar2=None, op0=ALU.add), r=[mvb], w=[rsb])
        P.op("act", lambda e: e.activation(out=rs[:], in_=rs[:], func=AF.Sqrt), r=[rsb], w=[rsb])
        P.op("dve", lambda e: e.reciprocal(out=rs[:], in_=rs[:]), r=[rsb], w=[rsb])
        P.op("dve", lambda e: e.tensor_scalar(out=dst[:], in0=src[:], scalar1=mv[:, 0:1], scalar2=rs[:, 0:1],
                                              op0=ALU.subtract, op1=ALU.mult), r=[srcb, mvb, rsb], w=[dstb])
        hA, hB = slice(0, 1280), slice(1280, D)
        for op_, T_ in ((ALU.mult, G_), (ALU.add, B_)):
            P.op("dve", lambda e, op_=op_, T_=T_: e.tensor_tensor(out=dst[:, hA], in0=dst[:, hA], in1=T_[:, hA], op=op_),
                 r=[dstb, gb_], w=[dstb])
            P.op("pool", lambda e, op_=op_, T_=T_: e.tensor_tensor(out=dst[:, hB], in0=dst[:, hB], in1=T_[:, hB], op=op_),
                 r=[dstb, gb_], w=[dstb])

    es = ExitStack()
    with es:
        P = Phase(nc, "E2b")
        cb_ = Buf()
        G1 = sb("G1", [128, D], F32)
        B1 = sb("B1", [128, D], F32)
        WR = sb("WR", [128, 16 * 32], F32)
        BR = sb("BR", [128, 32], F32)
        IDF = sb("IDF", [128, 128], F32)
        TRI = sb("TRI", [128, 128], BF16)
        ONESB = sb("ONESBe", [128, 128], BF16)
        EB1 = sb("EB1", [128, 32], F32)
        CNT = sb("CNT", [128, 32], F32)
        MASKB = sb("MASKB", [128, 16, 32], BF16)
        ZR = sb("ZR", [128, D], F32)
        P.dma(G1[:], ln1g.to_broadcast([128, D]), w=[cb_], dsem=P.dsem())
        P.dma(B1[:], ln1b.to_broadcast([128, D]), w=[cb_], dsem=P.dsem())
        P.dma(WR[:], wR, w=[cb_], dsem=P.dsem())
        P.dma(BR[:], bR.to_broadcast([128, 32]), w=[cb_], dsem=P.dsem())
        P.dma(IDF[:], identF, w=[cb_], dsem=P.dsem())
        P.dma(TRI[:], tri, w=[cb_], dsem=P.dsem())
        P.dma(EB1[:], eb1.to_broadcast([128, 32]), w=[cb_], dsem=P.dsem())
        P.op("pool", lambda e: e.memset(ONESB[:], 1.0), w=[cb_])
        cntb = Buf()
        P.op("pool", lambda e: e.memset(CNT[:], 0.0), w=[cntb])
        zrb = Buf()
        P.op("pool", lambda e: e.memset(ZR[:], 0.0), w=[zrb])
        P.dma(ysc[DUMMY:DUMMY + 128, :], ZR[:], r=[zrb], dsem=P.dsem())
        hr = Ring(P, [sb(f"e4h{i}", [128, D], F32) for i in range(2)], True)
        x1r = Ring(P, [sb(f"e4x1{i}", [128, D], F32) for i in range(2)], True)
        xbr = Ring(P, [sb(f"e4xb{i}", [128, D], BF16) for i in range(2)])
        xTr = Ring(P, [sb(f"e4xT{i}", [128, 16 * 128], F32) for i in range(2)])
        pT = Ring(P, [ps(f"e4pT{i}", [128, 512]) for i in range(2)])
        pL = Ring(P, [ps(f"e4pL{i}", [128, 512]) for i in range(2)])
        pPos = Ring(P, [ps(f"e4pP{i}", [128, 512]) for i in range(2)])
        pTot = Ring(P, [ps(f"e4pQ{i}", [128, 512]) for i in range(2)])
        scat_ds = [P.dsem() for _ in range(8)]
        sm = lambda n, sh, dt=F32: (sb(n, sh, dt), Buf())
        ST, STb = sm("e4st", [128, 4, 6])
        MV, MVb = sm("e4mv", [128, 2])
        RS, RSb = sm("e4rs", [128, 1])
        LG, LGb = sm("e4lg", [128, 32])
        T8, T8b = sm("e4t8", [128, 8])
        MK, MKb = sm("e4mk", [128, 32])
        NM, NMb = sm("e4nm", [128, 1])
        EX, EXb = sm("e4ex", [128, 32])
        DEN, DENb = sm("e4den", [128, 1])
        GG, GGb = sm("e4gg", [128, 32])
        PS_, PSb = sm("e4pos", [128, 32])
        VL, VLb = sm("e4vl", [128, 32])
        DV, DVb = sm("e4dv", [128, 32])
        D8, D8b = sm("e4d8", [128, 8])
        DF, DFb = sm("e4df", [128, 4])
        OH, OHb = sm("e4oh", [128, 32])
        destb = Buf()
        gateb = Buf()
        mbb = Buf()
        si = 0
        def stage1(t_):
            ht, hb, hd_ = hr.next()
            P.dma(ht[:], hs_[t_ * 128:(t_ + 1) * 128, :], w=[hb], dsem=hd_)
            x1t, x1b_, x1d = x1r.next()
            layer_norm(P, ht, hb, x1t, x1b_, G1, B1, cb_, (ST, STb, MV, MVb, RS, RSb), t_)
            P.dma(x1s[t_ * 128:(t_ + 1) * 128, :], x1t[:], r=[x1b_], dsem=x1d, eng="act")
            xbt, xbb, _ = xbr.next()
            P.op("act", lambda e, xbt=xbt, x1t=x1t: e.activation(out=xbt[:], in_=x1t[:], func=AF.Copy), r=[x1b_], w=[xbb])
            xT, xTb, _ = xTr.next()
            for q4 in range(4):
                pt, pb, _ = pT.next()
                for j in range(4):
                    kc = q4 * 4 + j
                    P.op("pe", lambda e, pt=pt, j=j, kc=kc, x1t=x1t: e.transpose(
                        out=pt[:, j * 128:(j + 1) * 128], in_=x1t[:, kc * 128:(kc + 1) * 128], identity=IDF[:]),
                        r=[x1b_, cb_], w=[pb])
                P.op("act" if q4 % 2 else "dve", (lambda e, xT=xT, pt=pt, q4=q4: e.activation(out=xT[:, q4 * 512:(q4 + 1) * 512], in_=pt[:], func=AF.Copy))
                     if q4 % 2 else (lambda e, xT=xT, pt=pt, q4=q4: e.tensor_copy(out=xT[:, q4 * 512:(q4 + 1) * 512], in_=pt[:])),
                     r=[pb], w=[xTb])
            return xbt, xbb, xT, xTb

        def stage2(t_, xbt, xbb, xT, xTb):
            nonlocal si
            pl, plb, _ = pL.next()
            for kc in range(16):
                P.mm(pl[:, 0:32], xT[:, kc * 128:(kc + 1) * 128], WR[:, kc * 32:(kc + 1) * 32], kc == 0, kc == 15, r=[xTb, cb_], w=[plb])
            P.op("dve", lambda e, pl=pl: e.tensor_tensor(out=LG[:], in0=pl[:, 0:32], in1=BR[:], op=ALU.add), r=[plb, cb_], w=[LGb])
            P.op("dve", lambda e: e.max(out=T8[:], in_=LG[:]), r=[LGb], w=[T8b])
            P.op("dve", lambda e: e.tensor_scalar(out=MK[:], in0=LG[:], scalar1=T8[:, 3:4], scalar2=None, op0=ALU.is_ge), r=[LGb, T8b], w=[MKb])
            P.op("dve", lambda e: e.tensor_scalar(out=NM[:], in0=T8[:, 0:1], scalar1=-1.0, scalar2=None, op0=ALU.mult), r=[T8b], w=[NMb])
            P.op("act", lambda e: e.activation(out=EX[:], in_=LG[:], func=AF.Exp, bias=NM[:, 0:1], scale=1.0), r=[LGb, NMb], w=[EXb])
            P.op("dve", lambda e: e.tensor_tensor(out=EX[:], in0=EX[:], in1=MK[:], op=ALU.mult), r=[EXb, MKb], w=[EXb])
            P.op("dve", lambda e: e.reduce_sum(out=DEN[:], in_=EX[:], axis=AX.X), r=[EXb], w=[DENb])
            P.op("dve", lambda e: e.reciprocal(out=DEN[:], in_=DEN[:]), r=[DENb], w=[DENb])
            P.op("pool", lambda e, t_=t_: e.tensor_copy(out=MASKB[:, t_, :], in_=MK[:]), r=[MKb], w=[mbb])
            pp_, ppb, _ = pPos.next()
            pq_, pqb, _ = pTot.next()
            P.mm(pp_[:, 0:32], TRI[:], MASKB[:, t_, :], True, True, r=[mbb, cb_], w=[ppb])
            P.mm(pq_[:, 0:32], ONESB[:], MASKB[:, t_, :], True, True, r=[mbb, cb_], w=[pqb])
            P.op("dve", lambda e, pp_=pp_: e.tensor_tensor(out=PS_[:], in0=pp_[:, 0:32], in1=CNT[:], op=ALU.add), r=[ppb, cntb], w=[PSb])
            P.op("dve", lambda e, pq_=pq_: e.tensor_tensor(out=CNT[:], in0=pq_[:, 0:32], in1=CNT[:], op=ALU.add), r=[pqb, cntb], w=[cntb])
            P.op("dve", lambda e: e.tensor_scalar(out=VL[:], in0=PS_[:], scalar1=float(CAP), scalar2=None, op0=ALU.is_lt), r=[PSb], w=[VLb])
            P.op("dve", lambda e: e.tensor_tensor(out=VL[:], in0=VL[:], in1=MK[:], op=ALU.mult), r=[VLb, MKb], w=[VLb])
            P.op("dve", lambda e: e.scalar_tensor_tensor(out=GG[:], in0=EX[:], scalar=DEN[:, 0:1], in1=VL[:], op0=ALU.mult, op1=ALU.mult),
                 r=[EXb, DENb, VLb], w=[GGb])
            P.op("dve", lambda e: e.tensor_tensor(out=DV[:], in0=PS_[:], in1=EB1[:], op=ALU.add), r=[PSb, cb_], w=[DVb])
            P.op("dve", lambda e: e.tensor_tensor(out=DV[:], in0=DV[:], in1=VL[:], op=ALU.mult), r=[DVb, VLb], w=[DVb])
            P.op("dve", lambda e: e.max(out=D8[:], in_=DV[:]), r=[DVb], w=[D8b])
            P.op("dve", lambda e: e.tensor_scalar(out=DF[:], in0=D8[:, 0:4], scalar1=0.0, scalar2=float(DUMMY + 1),
                                                  op0=ALU.is_equal, op1=ALU.mult), r=[D8b], w=[DFb])
            P.op("dve", lambda e: e.scalar_tensor_tensor(out=DF[:], in0=D8[:, 0:4], scalar=-1.0, in1=DF[:], op0=ALU.add, op1=ALU.add),
                 r=[D8b, DFb], w=[DFb])
            P.op("dve", lambda e, t_=t_: e.tensor_copy(out=DEST[:, t_, :], in_=DF[:]), r=[DFb], w=[destb])
            for k in range(4):
                P.op("dve", lambda e, k=k: e.tensor_scalar(out=OH[:], in0=DV[:], scalar1=D8[:, k:k + 1], scalar2=None, op0=ALU.is_equal),
                     r=[DVb, D8b], w=[OHb])
                P.op("dve", lambda e: e.tensor_tensor(out=OH[:], in0=OH[:], in1=GG[:], op=ALU.mult), r=[OHb, GGb], w=[OHb])
                P.op("dve", lambda e, k=k, t_=t_: e.reduce_sum(out=GATE[:, t_, k:k + 1], in_=OH[:], axis=AX.X), r=[OHb], w=[gateb])
            for k in range(4):
                ds = scat_ds[si % 8]
                si += 1
                P.op("pool", lambda e, xbt=xbt, t_=t_, k=k: e.indirect_dma_start(
                    out=xg, out_offset=bass.IndirectOffsetOnAxis(ap=DEST[:, t_, k:k + 1], axis=0),
                    in_=xbt[:], in_offset=None), r=[xbb, destb], w=[], dsem=ds)
        nx1 = stage1(0)
        for t_ in range(16):
            cur1 = nx1
            if t_ + 1 < 16:
                nx1 = stage1(t_ + 1)
            stage2(t_, *cur1)
        P.finalize()
    if stop_after == "E2":
        esR.close()
        return nc

    w1r = din("w1r", [NE, 16, 128, 4096])
    w2r = din("w2r", [NE, 8, 128, 4096])
    b1r = din("b1r", [128, NE * 32])
    b2 = din("b2", [NE, D])
    es = ExitStack()
    with es:
        P = Phase(nc, "G")
        cb_ = Buf()
        IDB = sb("IDB", [128, 128], BF16)
        B1A = sb("B1A", [128, NE * 32], F32)
        B1L = sb("B1L", [128, NE * 16], F32)
        P.dma(IDB[:], identB, w=[cb_], dsem=P.dsem())
        P.dma(B1A[:], b1r, w=[cb_], dsem=P.dsem())
        P.op("dve", lambda e: e.tensor_scalar(
            out=B1L[:].rearrange("p (e m) -> p e m", e=NE), in0=B1A[:].rearrange("p (e m) -> p e m", e=NE)[:, :, 16:32],
            scalar1=1.0, scalar2=None, op0=ALU.add), r=[cb_], w=[cb_])
        xgr = Ring(P, [sb(f"gx{i}", [128, D], BF16) for i in range(3)], True)
        xgT = Ring(P, [sb(f"gxT{i}", [128, 16, CAP], BF16) for i in range(2)])
        wst = Ring(P, [sb(f"gws{i}", [128, 4096], F32) for i in range(3)], True)
        wbf = Ring(P, [sb(f"gwb{i}", [128, 4096], BF16) for i in range(3)])
        atr = Ring(P, [sb(f"gat{i}", [128, 16, CAP], BF16) for i in range(2)])
        g1r = Ring(P, [sb(f"gg1{i}", [128, CAP], F32) for i in range(2)])
        sgr = Ring(P, [sb(f"gsg{i}", [128, CAP], F32) for i in range(2)])
        l1r = Ring(P, [sb(f"gl1{i}", [128, CAP], F32) for i in range(2)])
        ytr = Ring(P, [sb(f"gy{i}", [128, 256], F32) for i in range(4)], True)
        b2r = Ring(P, [sb(f"gb2{i}", [128, D], F32) for i in range(2)], True)
        pTr = Ring(P, [ps(f"gpT{i}", [128, 1024], BF16) for i in range(2)])
        pG = Ring(P, [ps(f"gpG{i}", [128, 512]) for i in range(2)])
        pLn = Ring(P, [ps(f"gpL{i}", [128, 512]) for i in range(2)])
        pYr = Ring(P, [ps(f"gpY{i}", [128, 512]) for i in range(2)])
        cast_pat = ("act", "dve", "act", "dve", "act", "pool")
        cig = [0]

        def castG(j, t, b, wt, wb):
            for hf in range(2):
                eng = cast_pat[cig[0] % 6]
                cig[0] += 1
                hsl = slice(hf * 2048, (hf + 1) * 2048)
                if eng == "act":
                    P.op("act", lambda e, wt=wt, t=t, hsl=hsl: e.activation(out=wt[:, hsl], in_=t[:, hsl], func=AF.Copy), r=[b], w=[wb])
                else:
                    P.op(eng, lambda e, wt=wt, t=t, hsl=hsl: e.tensor_copy(out=wt[:, hsl], in_=t[:, hsl]), r=[b], w=[wb])

        srcsG = []
        for ex in range(NE):
            srcsG += [w1r[ex, m] for m in range(16)] + [w2r[ex, cb] for cb in range(8)]
        wsG = WStream(P, srcsG, wst, wbf, castG)

        def load_xg(ex):
            res_ = []
            for st_ in range(NST):
                xt, xb, xd = xgr.next()
                r0 = ex * CAP + st_ * 128
                P.dma(xt[:], xg[r0:r0 + 128, :], w=[xb], dsem=xd)
                res_.append((xt, xb))
            return res_

        nxt_xg = load_xg(0)
        for ex in range(NE):
            b2t, b2b, b2d = b2r.next()
            P.dma(b2t[:], b2[ex:ex + 1, :].to_broadcast([128, D]), w=[b2b], dsem=b2d)
            XT_, XTb_, _ = xgT.next()
            cur_xg = nxt_xg
            for st_ in range(NST):
                xt, xb = cur_xg[st_]
                for h8 in range(2):
                    pt, pb, _ = pTr.next()
                    for j in range(8):
                        kc = h8 * 8 + j
                        P.op("pe", lambda e, pt=pt, j=j, kc=kc, xt=xt: e.transpose(
                            out=pt[:, j * 128:(j + 1) * 128], in_=xt[:, kc * 128:(kc + 1) * 128], identity=IDB[:]),
                            r=[xb, cb_], w=[pb])
                    o_ap = XT_[:, h8 * 8:(h8 + 1) * 8, st_ * 128:(st_ + 1) * 128]
                    i_ap = pt[:].rearrange("p (j c) -> p j c", j=8)
                    if h8 == 0:
                        P.op("dve", lambda e, o_ap=o_ap, i_ap=i_ap: e.tensor_copy(out=o_ap, in_=i_ap), r=[pb], w=[XTb_])
                    else:
                        P.op("act", lambda e, o_ap=o_ap, i_ap=i_ap: e.activation(out=o_ap, in_=i_ap, func=AF.Copy), r=[pb], w=[XTb_])
            AT, ATb, _ = atr.next()
            for m in range(16):
                wsG.want(ex * 24 + m)
                wt, wb = wsG.get(ex * 24 + m)
                w3 = wt[:].rearrange("p (k c) -> p k c", k=16)
                pg, pgb, _ = pG.next()
                pl, plb, _ = pLn.next()
                for kc in range(16):
                    P.mm(pg[:, 0:CAP], w3[:, kc, 0:128], XT_[:, kc, :], kc == 0, kc == 15, r=[wb, XTb_], w=[pgb])
                for kc in range(16):
                    P.mm(pl[:, 0:CAP], w3[:, kc, 128:256], XT_[:, kc, :], kc == 0, kc == 15, r=[wb, XTb_], w=[plb])
                g1, g1b, _ = g1r.next()
                sgt, sgb, _ = sgr.next()
                l1, l1b, _ = l1r.next()
                bg = B1A[:, ex * 32 + m:ex * 32 + m + 1]
                bl = B1L[:, ex * 16 + m:ex * 16 + m + 1]
                P.op("dve", lambda e, g1=g1, pg=pg, bg=bg: e.tensor_scalar(out=g1[:], in0=pg[:, 0:CAP], scalar1=bg, scalar2=7.0,
                                                                            op0=ALU.add, op1=ALU.min), r=[pgb, cb_], w=[g1b])
                P.op("act", lambda e, sgt=sgt, g1=g1: e.activation(out=sgt[:], in_=g1[:], func=AF.Sigmoid, scale=1.702), r=[g1b], w=[sgb])
                P.op("dve", lambda e, l1=l1, pl=pl, bl=bl: e.tensor_scalar(out=l1[:], in0=pl[:, 0:CAP], scalar1=bl, scalar2=-6.0,
                                                                            op0=ALU.add, op1=ALU.max), r=[plb, cb_], w=[l1b])
                P.op("pool", lambda e, g1=g1, sgt=sgt: e.tensor_tensor(out=g1[:], in0=g1[:], in1=sgt[:], op=ALU.mult), r=[g1b, sgb], w=[g1b])
                P.op("dve", lambda e, AT=AT, m=m, l1=l1, g1=g1: e.scalar_tensor_tensor(
                    out=AT[:, m, :], in0=l1[:], scalar=8.0, in1=g1[:], op0=ALU.min, op1=ALU.mult), r=[l1b, g1b], w=[ATb])
            if ex + 1 < NE:
                nxt_xg = load_xg(ex + 1)
            for cb in range(8):
                wsG.want(ex * 24 + 16 + cb)
                wt, wb = wsG.get(ex * 24 + 16 + cb)
                w3 = wt[:].rearrange("p (k c) -> p k c", k=16)
                for st_ in range(NST):
                    py, pyb, _ = pYr.next()
                    for kc in range(16):
                        P.mm(py[:, 0:256], AT[:, kc, st_ * 128:(st_ + 1) * 128], w3[:, kc, :], kc == 0, kc == 15, r=[ATb, wb], w=[pyb])
                    yt, yb, yd = ytr.next()
                    P.op("dve", lambda e, yt=yt, py=py, b2t=b2t, cb=cb: e.tensor_tensor(
                        out=yt[:], in0=py[:, 0:256], in1=b2t[:, cb * 256:(cb + 1) * 256], op=ALU.add), r=[pyb, b2b], w=[yb])
                    r0 = ex * CAP + st_ * 128
                    P.dma(ysc[r0:r0 + 128, cb * 256:(cb + 1) * 256], yt[:], r=[yb], dsem=yd, eng="act")
        P.finalize()
    if stop_after == "G":
        esR.close()
        return nc

    es = ExitStack()
    with es:
        P = Phase(nc, "H")
        cb_ = Buf()
        rb_ = Buf()
        G2 = sb("G2", [128, D], F32)
        B2_ = sb("B2_", [128, D], F32)
        P.dma(G2[:], ln2g.to_broadcast([128, D]), w=[cb_], dsem=P.dsem())
        P.dma(B2_[:], ln2b.to_broadcast([128, D]), w=[cb_], dsem=P.dsem())
        ygr = Ring(P, [sb(f"hy{i}", [128, D], F32) for i in range(8)], True)
        x1r = Ring(P, [sb(f"hx{i}", [128, D], F32) for i in range(2)], True)
        acr = Ring(P, [sb(f"ha{i}", [128, D], F32) for i in range(2)])
        otr = Ring(P, [sb(f"ho{i}", [128, D], F32) for i in range(2)], True)
        ST = sb("hst", [128, 4, 6], F32)
        MV = sb("hmv", [128, 2], F32)
        RS = sb("hrs", [128, 1], F32)
        STb, MVb, RSb = Buf(), Buf(), Buf()
        def issueH(t_):
            x1t, x1b_, x1d = x1r.next()
            P.dma(x1t[:], x1s[t_ * 128:(t_ + 1) * 128, :], w=[x1b_], dsem=x1d)
            ys = []
            for k in range(4):
                yt, yb, yd = ygr.next()
                P.op("pool", lambda e, yt=yt, t_=t_, k=k: e.indirect_dma_start(
                    out=yt[:], out_offset=None, in_=ysc,
                    in_offset=bass.IndirectOffsetOnAxis(ap=DEST[:, t_, k:k + 1], axis=0)), r=[rb_], w=[yb], dsem=yd)
                ys.append((yt, yb))
            return x1t, x1b_, ys

        nxtH = issueH(0)
        for t_ in range(16):
            x1t, x1b_, ys = nxtH
            if t_ + 1 < 16:
                nxtH = issueH(t_ + 1)
            ac, acb, _ = acr.next()
            P.op("act", lambda e, ac=ac, x1t=x1t: e.activation(out=ac[:], in_=x1t[:], func=AF.Copy, scale=DN_ALPHA), r=[x1b_], w=[acb])
            for k in range(4):
                yt, yb = ys[k]
                P.op("dve", lambda e, ac=ac, yt=yt, t_=t_, k=k: e.scalar_tensor_tensor(
                    out=ac[:], in0=yt[:], scalar=GATE[:, t_, k:k + 1], in1=ac[:], op0=ALU.mult, op1=ALU.add), r=[yb, acb], w=[acb])
            ot, ob, od_ = otr.next()
            layer_norm(P, ac, acb, ot, ob, G2, B2_, cb_, (ST, STb, MV, MVb, RS, RSb), t_)
            P.dma(out[t_ * 128:(t_ + 1) * 128, :], ot[:], r=[ob], dsem=od_, eng="act")
        P.finalize()
    esR.close()
    return nc


OFF_CQ, OFF_CKV, OFF_KPE, OFF_QD, OFF_KD, OFF_VD, OFF_GM, OFF_GD = 0, 768, 1280, 1344, 2880, 4416, 5952, 8000


def lhs_blocks(w, cols):
    K = w.shape[0]
    ws = w[:, cols]
    nb = ws.shape[1] // 128
    return np.ascontiguousarray(ws.reshape(K // 128, 128, nb, 128).transpose(2, 1, 0, 3))


def prep_shared(inp):
    w_in = np.asarray(inp["w_in"])[0]
    half = ROPE // 2
    kpe = np.arange(OFF_KPE, OFF_KPE + ROPE)
    kpe_perm = np.concatenate([kpe[half:], kpe[:half]])
    colsA = np.concatenate([
        np.arange(0, OFF_KPE), kpe, kpe_perm,
        np.arange(OFF_QD, OFF_QD + 1536), np.arange(OFF_KD, OFF_KD + 1536),
        np.arange(OFF_GM, OFF_GM + 4096)])
    sh = {}
    sh["wA"] = lhs_blocks(w_in, colsA)
    wv = w_in[:, OFF_VD:OFF_VD + 1536].reshape(4, 4, 128, 3, 512)
    sh["wV"] = np.ascontiguousarray(wv.transpose(3, 0, 2, 1, 4)).reshape(12, 128, 4, 512)
    w_uq = np.asarray(inp["w_uq"])[0]
    cols = []
    for h in range(MH):
        b = h * 192
        rope = np.arange(b + 128, b + 192)
        perm = np.concatenate([rope[half:], rope[:half]])
        cols.append(np.concatenate([np.arange(b, b + 128), rope, perm, perm, rope]))
    cols = np.concatenate(cols)
    wq = w_uq[:, cols].reshape(6, 128, MH, 384)
    sh["wUQ"] = np.ascontiguousarray(wq.transpose(2, 1, 0, 3)).reshape(MH, 128, 6 * 384)
    sh["gq"] = np.ascontiguousarray(np.asarray(inp["q_norm_g"])[0].reshape(6, 128).T)
    w_ukv = np.asarray(inp["w_ukv"])[0].reshape(4, 128, MH, 256)
    sh["wUK"] = np.ascontiguousarray(w_ukv[:, :, :, 0:128].transpose(2, 1, 0, 3)).reshape(MH, 128, 4 * 128)
    sh["wUV"] = np.ascontiguousarray(w_ukv[:, :, :, 128:256]).reshape(4, 128, MH * 128)
    sh["gkv"] = np.ascontiguousarray(np.asarray(inp["kv_norm_g"])[0].reshape(4, 128).T)
    inv = (np.float32(10000.0) ** (-np.arange(half, dtype=np.float32) / np.float32(half))).astype(np.float32)
    ang = (np.arange(S, dtype=np.float32)[:, None] * inv[None, :]).astype(np.float32)
    cs, sn = np.cos(ang).astype(np.float32).T, np.sin(ang).astype(np.float32).T
    sh["cosT"] = np.ascontiguousarray(np.concatenate([cs, cs], 0))
    sh["sinT"] = np.ascontiguousarray(np.concatenate([-sn, sn], 0))
    jj, ii = np.arange(128)[:, None], np.arange(128)[None, :]
    sh["maskc"] = (jj <= ii).astype(np.float32).astype(ml_dtypes.bfloat16)
    slopes = 2.0 ** (-8.0 * np.arange(1, DH + 1, dtype=np.float64) / DH)
    dm = np.zeros((20, 128, 128), np.float64)
    for hd in range(DH):
        dil = DIL[hd // 4][1]
        st = (ii - jj).astype(np.float64)
        dm[hd] = np.where(ii >= jj, np.exp(-slopes[hd] * dil * st), 0.0)
        if hd < 8:
            dm[12 + hd] = np.where(ii <= jj, np.exp(-slopes[hd] * dil * (st + 128.0)), 0.0)
    sh["dmask"] = np.ascontiguousarray(dm.transpose(1, 0, 2)).astype(np.float32)
    sh["wOUT"] = np.ascontiguousarray(np.asarray(inp["w_out"])[0].reshape(16, 128, 2048))
    for k in ("ln1_g", "ln1_b", "ln2_g", "ln2_b"):
        sh[k] = np.ascontiguousarray(np.asarray(inp[k]).reshape(1, D))
    sh["wR"] = np.ascontiguousarray(np.asarray(inp["w_router"])[0].reshape(16, 128, NE).transpose(1, 0, 2)).reshape(128, 16 * NE)
    sh["bR"] = np.ascontiguousarray(np.asarray(inp["b_router"]).reshape(1, NE))
    sh["identF"] = np.eye(128, dtype=np.float32)
    sh["identB"] = np.eye(128, dtype=np.float32).astype(ml_dtypes.bfloat16)
    sh["tri"] = (jj < ii).astype(np.float32).astype(ml_dtypes.bfloat16)
    sh["eb1"] = (np.arange(NE, dtype=np.float32) * CAP + 1.0).reshape(1, NE)
    w1 = np.asarray(inp["w1"])[0]
    w1 = w1.reshape(NE, 16, 128, 16, 128, 2)
    sh["w1r"] = np.ascontiguousarray(w1.transpose(0, 3, 2, 1, 5, 4)).reshape(NE, 16, 128, 4096)
    w2 = np.asarray(inp["w2"])[0].reshape(NE, 16, 128, 8, 256)
    sh["w2r"] = np.ascontiguousarray(w2.transpose(0, 3, 2, 1, 4)).reshape(NE, 8, 128, 4096)
    b1 = np.asarray(inp["b1"])[0].reshape(NE, 16, 128, 2)
    sh["b1r"] = np.ascontiguousarray(b1.transpose(2, 0, 3, 1)).reshape(128, NE * 32)
    sh["b2"] = np.ascontiguousarray(np.asarray(inp["b2"])[0])
    sh["wOM"] = lhs_blocks(np.asarray(inp["w_o_mla"])[0], np.arange(2048)).reshape(16, 128, 2048)
    sh["wOD"] = lhs_blocks(np.asarray(inp["w_o_dil"])[0], np.arange(2048)).reshape(16, 128, 1536)
    return sh


def make_in_maps(inp):
    sh = prep_shared(inp)
    x = np.asarray(inp["x"])
    maps = []
    for c in range(NCORES):
        m = dict(sh)
        m["x"] = np.ascontiguousarray(x[c])
        m["xT"] = np.ascontiguousarray(x[c].T)
        maps.append(m)
    return maps


def kernel(**inputs):
    nc = build()
    in_maps = make_in_maps(inputs)
    res = run_bass_kernel_spmd(nc, in_maps, core_ids=list(range(NCORES)))
    return np.stack([np.asarray(r["out"]) for r in res.results], axis=0).astype(np.float32)
```

```python
import math
from contextlib import ExitStack

import numpy as np
import ml_dtypes
import concourse.bass as bass
import concourse.mybir as mybir
from concourse.bass_utils import run_bass_kernel_spmd

F32 = mybir.dt.float32
BF16 = mybir.dt.bfloat16
I32 = mybir.dt.int32
U32 = mybir.dt.uint32
AF = mybir.ActivationFunctionType
ALU = mybir.AluOpType
AX = mybir.AxisListType

S = 2048
D = 2048
NCORES = 8
QR, KVR, ROPE = 768, 512, 64
MH = 16
DH = 12
NE = 32
CAP = 384
NST = CAP // 128
DFF = 2048
DN_ALPHA = 2.0 ** 0.25
LN_EPS = 1e-5
RMS_EPS = 1e-6
DIL = ((2048, 1), (512, 4), (128, 16))


class Buf:
    __slots__ = ("name", "ws", "rs")

    def __init__(self, name=""):
        self.name = name
        self.ws = []
        self.rs = []


class DSem:
    __slots__ = ("sem", "count")

    def __init__(self, sem):
        self.sem = sem
        self.count = 0


class Op:
    __slots__ = ("eng", "fn", "deps", "sig", "idx", "dsem", "dval", "waits", "ph")


class Phase:
    ENGS = ("pe", "act", "dve", "pool", "sp")

    POOL = None

    def __init__(self, nc, name):
        self.nc = nc
        self.name = name
        self.ops = []
        pool = Phase.POOL
        if pool is None or pool["nc"] is not nc:
            st = ExitStack()
            pool = Phase.POOL = {"nc": nc, "stack": st, "esem": {}, "ebase": {}, "dsems": []}
            for e in ("pe", "act", "dve", "pool"):
                pool["esem"][e] = st.enter_context(nc.semaphore(f"sem_{e}"))
                pool["ebase"][e] = 0
        self.pool = pool
        self.esem = pool["esem"]
        self.dsems = []
        self.nd = 0

    def dsem(self):
        pool = self.pool
        if self.nd == len(pool["dsems"]):
            pool["dsems"].append(DSem(pool["stack"].enter_context(self.nc.semaphore(f"sem_d{self.nd}"))))
        d = pool["dsems"][self.nd]
        self.nd += 1
        self.dsems.append(d)
        return d

    def op(self, eng, fn, r=(), w=(), dsem=None):
        o = Op()
        o.eng, o.fn, o.sig, o.idx, o.dsem, o.dval, o.waits = eng, fn, False, 0, dsem, 0, None
        o.ph = self
        isdma = dsem is not None
        if isdma:
            dsem.count += 16
            o.dval = dsem.count
        deps = {}

        def add(p, raw):
            if p is o or p.ph is not self:
                return
            if (not isdma) and p.dsem is None and p.eng == eng:
                if not raw or eng == "pe":
                    return
            deps[id(p)] = p

        for b in r:
            for p in b.ws:
                add(p, True)
        for b in w:
            for p in b.ws:
                add(p, False)
            for p in b.rs:
                add(p, False)
        o.deps = list(deps.values())
        for p in o.deps:
            if p.dsem is None:
                p.sig = True
        for b in w:
            if b.rs:
                b.ws = [o]
                b.rs = []
            else:
                b.ws = [p for p in b.ws if not (p.dsem is None and p.eng == eng and not isdma)] + [o]
        for b in r:
            if isdma:
                b.rs.append(o)
            else:
                b.rs = [p for p in b.rs if not (p.dsem is None and p.eng == eng and p.ph is self)] + [o]
        self.ops.append(o)
        return o

    def dma(self, out, in_, r=(), w=(), dsem=None, eng="sp", **kw):
        return self.op(eng, lambda e: e.dma_start(out=out, in_=in_, **kw), r, w, dsem=dsem)

    def mm(self, out, lhsT, rhs, start, stop, r=(), w=()):
        return self.op("pe", lambda e: e.matmul(out, lhsT, rhs, start=start, stop=stop), r, w)

    def finalize(self):
        nc = self.nc
        cnt = {e: self.pool["ebase"].get(e, 0) for e in self.ENGS}
        for o in self.ops:
            if o.dsem is None and o.sig:
                cnt[o.eng] += 1
                o.idx = cnt[o.eng]
        for e in self.pool["ebase"]:
            self.pool["ebase"][e] = cnt[e]
        seen = {}
        for o in self.ops:
            need = {}
            for p in o.deps:
                if p.dsem is not None:
                    key, sem, val = ("d", id(p.dsem)), p.dsem.sem, p.dval
                else:
                    key, sem, val = ("e", p.eng), self.esem[p.eng], p.idx
                if val > need.get(key, (None, 0))[1]:
                    need[key] = (sem, val)
            o.waits = []
            for key, (sem, val) in need.items():
                if val > seen.get((o.eng, key), 0):
                    seen[(o.eng, key)] = val
                    o.waits.append((sem, val))
        by = {e: [o for o in self.ops if o.eng == e] for e in self.ENGS}
        esem = self.esem
        dsems = self.dsems

        def mk(en):
            def body(e):
                for o in by[en]:
                    for sem, val in o.waits:
                        e.wait_ge(sem, val)
                    ins = o.fn(e)
                    if o.dsem is not None:
                        ins.then_inc(o.dsem.sem, 16)
                    elif o.sig:
                        ins.then_inc(esem[en], 1)
                if en == "sp":
                    for d in dsems:
                        if d.count:
                            e.wait_ge(d.sem, d.count)
            return body

        with nc.Block() as blk:
            blk.tensor(mk("pe"))
            blk.scalar(mk("act"))
            blk.vector(mk("dve"))
            blk.gpsimd(mk("pool"))
            blk.sync(mk("sp"))


class WStream:
    def __init__(self, P, srcs, wst, wbf, cast_fn):
        self.P, self.srcs, self.wst, self.wbf, self.cast_fn = P, srcs, wst, wbf, cast_fn
        self.st = {}
        self.bf = {}
        self.nl = 0
        self.ncst = 0

    def _cast(self, upto):
        upto = min(upto, len(self.srcs) - 1)
        while self.ncst <= upto:
            j = self.ncst
            self._load(j)
            t, b = self.st.pop(j)
            wt, wb, _ = self.wbf.next()
            self.cast_fn(j, t, b, wt, wb)
            self.bf[j] = (wt, wb)
            self.ncst += 1

    def _load(self, upto):
        upto = min(upto, len(self.srcs) - 1)
        while self.nl <= upto:
            k = self.nl
            self._cast(k - len(self.wst.tiles))
            t, b, ds = self.wst.next()
            self.P.dma(t[:], self.srcs[k], w=[b], dsem=ds)
            self.st[k] = (t, b)
            self.nl += 1

    def want(self, last_needed, ahead_load=2, ahead_cast=1):
        self._cast(last_needed)
        self._load(last_needed + ahead_load)
        self._cast(last_needed + ahead_cast)

    def get(self, i):
        return self.bf[i]


class Ring:
    def __init__(self, P, tiles, with_dsem=False):
        self.tiles = tiles
        self.bufs = [Buf() for _ in tiles]
        self.ds = [P.dsem() for _ in tiles] if with_dsem else None
        self.i = -1

    def next(self):
        self.i += 1
        k = self.i % len(self.tiles)
        return self.tiles[k], self.bufs[k], (self.ds[k] if self.ds else None)


def build(stop_after=None, debug=(), nblkA=67, doV=True, nhB2=16):
    nc = bass.Bass("TRN2", target_bir_lowering=False)

    def din(name, shape, dt=F32):
        return nc.dram_tensor(name, list(shape), dt, kind="ExternalInput").ap()

    def dscr(name, shape, dt):
        kind = "ExternalOutput" if name in debug else "Internal"
        return nc.dram_tensor(name, list(shape), dt, kind=kind).ap()

    xT = din("xT", [D, S])
    x_tm = din("x", [S, D])
    wA = din("wA", [67, 128, 16, 128])
    wV = din("wV", [12, 128, 4, 512])
    out = nc.dram_tensor("out", [S, D], F32, kind="ExternalOutput").ap()

    lat = dscr("lat", [1408, S], BF16)
    qd = dscr("qd", [1536, S], BF16)
    kd = dscr("kd", [1536, S], BF16)
    vd = dscr("vd", [3, S, 512], BF16)
    sg = dscr("sg", [4096, S], F32)

    es = ExitStack()

    def sb(name, shape, dt):
        return es.enter_context(nc.sbuf_tensor(name, list(shape), dt))

    def ps(name, shape, dt=F32):
        return es.enter_context(nc.psum_tensor(name, list(shape), dt))

    with es:
        P = Phase(nc, "A")
        XT = sb("XT", [128, 16, S], BF16)
        XTb = [Buf() for _ in range(16)]
        xst = Ring(P, [sb(f"xst{i}", [128, S], F32) for i in range(2)], True)
        wst = Ring(P, [sb(f"wst{i}", [128, 2048], F32) for i in range(3)], True)
        wbf = Ring(P, [sb(f"wbf{i}", [128, 2048], BF16) for i in range(6)])
        obf = Ring(P, [sb(f"obf{i}", [128, S], BF16) for i in range(2)], True)
        of32 = Ring(P, [sb(f"of{i}", [128, S], F32) for i in range(2)], True)
        ovd = Ring(P, [sb(f"ovd{i}", [128, 512], BF16) for i in range(4)], True)
        pbanks = Ring(P, [ps(f"pa{i}", [128, 512]) for i in range(8)])

        for kc in range(16):
            t, b, ds = xst.next()
            P.dma(t[:], xT[kc * 128:(kc + 1) * 128, :], w=[b], dsem=ds)
            eng = "dve" if kc % 2 == 0 else "pool"
            P.op(eng, lambda e, t=t, kc=kc: e.tensor_copy(out=XT[:, kc, :], in_=t[:]), r=[b], w=[XTb[kc]])

        cast_i = [0]

        def castA(j, t, b, t2, b2):
            eng = ("dve", "pool")[cast_i[0] % 2]
            cast_i[0] += 1
            P.op(eng, lambda e, t=t, t2=t2: e.tensor_copy(out=t2[:], in_=t[:]), r=[b], w=[b2])

        blkA = list(nblkA) if isinstance(nblkA, (list, tuple)) else list(range(nblkA))
        srcsA = [wA[i].rearrange("p k c -> p (k c)") for i in blkA]
        nA = len(srcsA)
        if doV:
            srcsA += [wV[j].rearrange("p k c -> p (k c)") for j in range(12)]
        wsA = WStream(P, srcsA, wst, wbf, castA)

        evac_i = [0]
        for bi_, blk_i in enumerate(blkA):
            wsA.want(bi_)
            wt, wb = wsA.get(bi_)
            wv = wt[:].rearrange("p (k c) -> p k c", k=16)
            banks = [pbanks.next() for _ in range(4)]
            for kc in range(16):
                for tb in range(4):
                    pt, pb, _ = banks[tb]
                    P.mm(pt[:], wv[:, kc, :], XT[:, kc, tb * 512:(tb + 1) * 512], kc == 0, kc == 15,
                         r=[wb, XTb[kc]], w=[pb])
            if blk_i < 11:
                kind, dst = "lat", lat[blk_i * 128:(blk_i + 1) * 128, :]
            elif blk_i < 23:
                kind, hd, dst = "qk", blk_i - 11, qd[(blk_i - 11) * 128:(blk_i - 10) * 128, :]
            elif blk_i < 35:
                kind, hd, dst = "qk", blk_i - 23, kd[(blk_i - 23) * 128:(blk_i - 22) * 128, :]
            else:
                kind, dst = "gate", sg[(blk_i - 35) * 128:(blk_i - 34) * 128, :]
            if kind == "gate":
                ot, ob, ods = of32.next()
            else:
                ot, ob, ods = obf.next()
            for tb in range(4):
                pt, pb, _ = banks[tb]
                if kind == "gate":
                    P.op("act", lambda e, pt=pt, ot=ot, tb=tb: e.activation(
                        out=ot[:, tb * 512:(tb + 1) * 512], in_=pt[:], func=AF.Sigmoid), r=[pb], w=[ob])
                    continue
                if kind == "qk" and hd >= 4:
                    dil = 4 if hd < 8 else 16
                    ni = 512 // dil
                    o_ap = ot[:].rearrange("p (r i) -> p r i", r=dil)[:, :, tb * ni:(tb + 1) * ni]
                    i_ap = pt[:].rearrange("p (i r) -> p r i", r=dil)
                else:
                    o_ap = ot[:, tb * 512:(tb + 1) * 512]
                    i_ap = pt[:]
                if evac_i[0] % 2 == 0:
                    P.op("dve", lambda e, o_ap=o_ap, i_ap=i_ap: e.tensor_copy(out=o_ap, in_=i_ap), r=[pb], w=[ob])
                else:
                    P.op("act", lambda e, o_ap=o_ap, i_ap=i_ap: e.activation(out=o_ap, in_=i_ap, func=AF.Copy),
                         r=[pb], w=[ob])
                evac_i[0] += 1
            P.dma(dst, ot[:], r=[ob], dsem=ods)

        for g in (range(3) if doV else ()):
            n, dil = DIL[g]
            wsA.want(nA + g * 4 + 3)
            wts = [wsA.get(nA + g * 4 + kq) for kq in range(4)]
            for T in range(16):
                r_, blk_ = divmod(T, n // 128)
                pt, pb, _ = pbanks.next()
                for kc in range(16):
                    wt, wb = wts[kc // 4]
                    rhs = wt[:].rearrange("p (k c) -> p k c", k=4)[:, kc % 4, :]
                    lhsT = XT[:, kc, :].rearrange("p (i r) -> p r i", r=dil)[:, r_, blk_ * 128:(blk_ + 1) * 128]
                    P.mm(pt[:], lhsT, rhs, kc == 0, kc == 15, r=[wb, XTb[kc]], w=[pb])
                ot, ob, ods = ovd.next()
                if T % 2 == 0:
                    P.op("dve", lambda e, ot=ot, pt=pt: e.tensor_copy(out=ot[:], in_=pt[:]), r=[pb], w=[ob])
                else:
                    P.op("act", lambda e, ot=ot, pt=pt: e.activation(out=ot[:], in_=pt[:], func=AF.Copy),
                         r=[pb], w=[ob])
                P.dma(vd[g, T * 128:(T + 1) * 128, :], ot[:], r=[ob], dsem=ods)
        P.finalize()
    if stop_after == "A":
        return nc

    wUQ = din("wUQ", [16, 128, 6 * 384])
    gq = din("gq", [128, 6])
    wUK = din("wUK", [16, 128, 4 * 128])
    wUV = din("wUV", [4, 128, 2048])
    gkv = din("gkv", [128, 4])
    cosT = din("cosT", [64, S])
    sinT = din("sinT", [64, S])
    maskc = din("maskc", [128, 128], BF16)
    qm = dscr("qm", [16, 192, S], BF16)
    km = dscr("km", [16, 128, S], BF16)
    krot = dscr("krot", [64, S], BF16)
    vm = dscr("vm", [S, 2048], BF16)
    om = dscr("om", [2048, S], BF16)

    es = ExitStack()
    with es:
        LAT = sb("LAT", [128, 10, S], BF16)
        LATb = [Buf() for _ in range(10)]
        KPE = sb("KPE", [64, S], BF16)
        KPEP = sb("KPEP", [64, S], BF16)
        COS = sb("COS", [64, S], F32)
        SIN = sb("SIN", [64, S], F32)
        GQ = sb("GQ", [128, 6], F32)
        GKV = sb("GKV", [128, 4], F32)
        ONESF = sb("ONESF", [128, 128], F32)
        cb_ = Buf()
        es1 = ExitStack()
        with es1:
            sb1 = lambda n, sh, dt: es1.enter_context(nc.sbuf_tensor(n, list(sh), dt))
            ps1 = lambda n, sh, dt=F32: es1.enter_context(nc.psum_tensor(n, list(sh), dt))
            P = Phase(nc, "B1")
            P.dma(LAT[:], lat[0:1280, :].rearrange("(k p) s -> p k s", p=128), w=LATb, dsem=P.dsem())
            P.dma(KPE[:], lat[1280:1344, :], w=[cb_], dsem=P.dsem())
            P.dma(KPEP[:], lat[1344:1408, :], w=[cb_], dsem=P.dsem())
            P.dma(COS[:], cosT, w=[cb_], dsem=P.dsem())
            P.dma(SIN[:], sinT, w=[cb_], dsem=P.dsem())
            P.dma(GQ[:], gq, w=[cb_], dsem=P.dsem())
            P.dma(GKV[:], gkv, w=[cb_], dsem=P.dsem())
            P.op("pool", lambda e: e.memset(ONESF[:], 1.0), w=[cb_])
            sq = Ring(P, [sb1(f"sq{i}", [128, 512], F32) for i in range(3)])
            rr = Ring(P, [sb1(f"rr{i}", [128, 512], F32) for i in range(2)])
            pss = Ring(P, [ps1(f"pss{i}", [128, 512]) for i in range(2)])
            k_ = 0
            for (c0, ncn, nfeat) in ((0, 6, 768), (6, 4, 512)):
                for tb in range(4):
                    pt, pb, _ = pss.next()
                    for kc in range(ncn):
                        st, sbuf_, _ = sq.next()
                        src = LAT[:, c0 + kc, tb * 512:(tb + 1) * 512]
                        if k_ % 2 == 0:
                            P.op("act", lambda e, st=st, src=src: e.activation(out=st[:], in_=src, func=AF.Square),
                                 r=[LATb[c0 + kc]], w=[sbuf_])
                        else:
                            P.op("dve", lambda e, st=st, src=src: e.tensor_tensor(out=st[:], in0=src, in1=src, op=ALU.mult),
                                 r=[LATb[c0 + kc]], w=[sbuf_])
                        k_ += 1
                        P.mm(pt[:], ONESF[:], st[:], kc == 0, kc == ncn - 1, r=[sbuf_, cb_], w=[pb])
                    rt, rb, _ = rr.next()
                    P.op("act", lambda e, rt=rt, pt=pt, nfeat=nfeat: e.activation(
                        out=rt[:], in_=pt[:], func=AF.Sqrt, bias=RMS_EPS, scale=1.0 / nfeat), r=[pb], w=[rb])
                    P.op("dve", lambda e, rt=rt: e.reciprocal(out=rt[:], in_=rt[:]), r=[rb], w=[rb])
                    for kc in range(ncn):
                        src = LAT[:, c0 + kc, tb * 512:(tb + 1) * 512]
                        eng = "dve" if kc % 2 == 0 else "pool"
                        P.op(eng, lambda e, src=src, rt=rt: e.tensor_tensor(out=src, in0=src, in1=rt[:], op=ALU.mult),
                             r=[rb, LATb[c0 + kc]], w=[LATb[c0 + kc]])
            WV = sb1("WV", [128, 4, 2048], BF16)
            WVb = Buf()
            wvs = Ring(P, [sb1(f"wvs{i}", [128, 2048], F32) for i in range(2)], True)
            for kc in range(4):
                t, b, ds = wvs.next()
                P.dma(t[:], wUV[kc], w=[b], dsem=ds)
                P.op("dve" if kc % 2 == 0 else "pool", lambda e, t=t, kc=kc: e.tensor_scalar(
                    out=WV[:, kc, :], in0=t[:], scalar1=GKV[:, kc:kc + 1], scalar2=None, op0=ALU.mult),
                    r=[b, cb_], w=[WVb])
            pv = Ring(P, [ps1(f"pv{i}", [128, 512]) for i in range(4)])
            vt = Ring(P, [sb1(f"vt{i}", [128, 2048], BF16) for i in range(2)], True)
            ev = 0
            for t in range(16):
                ot, ob, ods = vt.next()
                for hb in range(4):
                    pt, pb, _ = pv.next()
                    for kc in range(4):
                        P.mm(pt[:], LAT[:, 6 + kc, t * 128:(t + 1) * 128], WV[:, kc, hb * 512:(hb + 1) * 512],
                             kc == 0, kc == 3, r=[LATb[6 + kc], WVb], w=[pb])
                    o_ap = ot[:, hb * 512:(hb + 1) * 512]
                    if ev % 2 == 0:
                        P.op("dve", lambda e, o_ap=o_ap, pt=pt: e.tensor_copy(out=o_ap, in_=pt[:]), r=[pb], w=[ob])
                    else:
                        P.op("act", lambda e, o_ap=o_ap, pt=pt: e.activation(out=o_ap, in_=pt[:], func=AF.Copy), r=[pb], w=[ob])
                    ev += 1
                P.dma(vm[t * 128:(t + 1) * 128, :], ot[:], r=[ob], dsem=ods, eng="act")
            wks = Ring(P, [sb1(f"wks{i}", [128, 512], F32) for i in range(3)], True)
            wkb = Ring(P, [sb1(f"wkb{i}", [128, 512], BF16) for i in range(3)])
            kn = Ring(P, [sb1(f"kn{i}", [128, S], BF16) for i in range(2)], True)

            def castK(j, t, b, t2, b2):
                for kc in range(4):
                    P.op("pool" if kc % 2 == 0 else "dve", lambda e, t=t, t2=t2, kc=kc: e.tensor_scalar(
                        out=t2[:, kc * 128:(kc + 1) * 128], in0=t[:, kc * 128:(kc + 1) * 128],
                        scalar1=GKV[:, kc:kc + 1], scalar2=None, op0=ALU.mult), r=[b, cb_], w=[b2])

            wsK = WStream(P, [wUK[h] for h in range(16)], wks, wkb, castK)
            for h in range(16):
                wsK.want(h)
                t2, b2 = wsK.get(h)
                ot, ob, ods = kn.next()
                for tb in range(4):
                    pt, pb, _ = pv.next()
                    for kc in range(4):
                        P.mm(pt[:], t2[:, kc * 128:(kc + 1) * 128], LAT[:, 6 + kc, tb * 512:(tb + 1) * 512],
                             kc == 0, kc == 3, r=[b2, LATb[6 + kc]], w=[pb])
                    o_ap = ot[:, tb * 512:(tb + 1) * 512]
                    if ev % 2 == 0:
                        P.op("dve", lambda e, o_ap=o_ap, pt=pt: e.tensor_copy(out=o_ap, in_=pt[:]), r=[pb], w=[ob])
                    else:
                        P.op("act", lambda e, o_ap=o_ap, pt=pt: e.activation(out=o_ap, in_=pt[:], func=AF.Copy), r=[pb], w=[ob])
                    ev += 1
                P.dma(km[h], ot[:], r=[ob], dsem=ods, eng="act")
            P.finalize()
        if stop_after == "B1":
            return nc
        es2 = ExitStack()
        with es2:
            sb1 = lambda n, sh, dt: es2.enter_context(nc.sbuf_tensor(n, list(sh), dt))
            ps1 = lambda n, sh, dt=F32: es2.enter_context(nc.psum_tensor(n, list(sh), dt))
            P = Phase(nc, "B2")
            tmp = Ring(P, [sb1(f"tmp{i}", [64, 512], F32) for i in range(4)])
            KR = sb1("KR", [64, S], BF16)
            KRb = Buf()
            for tb in range(4):
                sl = slice(tb * 512, (tb + 1) * 512)
                t1, b1_, _ = tmp.next()
                t2, b2_, _ = tmp.next()
                P.op("dve", lambda e, t1=t1, sl=sl: e.tensor_tensor(out=t1[:], in0=KPE[:, sl], in1=COS[:, sl], op=ALU.mult), w=[b1_])
                P.op("pool", lambda e, t2=t2, sl=sl: e.tensor_tensor(out=t2[:], in0=KPEP[:, sl], in1=SIN[:, sl], op=ALU.mult), w=[b2_])
                P.op("dve", lambda e, t1=t1, t2=t2, sl=sl: e.tensor_tensor(out=KR[:, sl], in0=t1[:], in1=t2[:], op=ALU.add),
                     r=[b1_, b2_], w=[KRb])
            P.dma(krot, KR[:], r=[KRb], dsem=P.dsem())
            wqs = Ring(P, [sb1(f"wqs{i}", [128, 2304], F32) for i in range(3)], True)
            wqb = Ring(P, [sb1(f"wqb{i}", [128, 2304], BF16) for i in range(3)])
            qn = Ring(P, [sb1(f"qn{i}", [128, S], BF16) for i in range(2)], True)
            qr = Ring(P, [sb1(f"qr{i}", [64, S], BF16) for i in range(2)], True)
            pqn = Ring(P, [ps1(f"pqn{i}", [128, 512]) for i in range(2)])
            pqa = Ring(P, [ps1(f"pqa{i}", [128, 512]) for i in range(2)])
            pqb = Ring(P, [ps1(f"pqb{i}", [128, 512]) for i in range(2)])
            ev = 0

            def castQ(j, t, b, t2, b2):
                for kc in range(6):
                    P.op("pool" if kc % 3 == 0 else "dve", lambda e, t=t, t2=t2, kc=kc: e.tensor_scalar(
                        out=t2[:, kc * 384:(kc + 1) * 384], in0=t[:, kc * 384:(kc + 1) * 384],
                        scalar1=GQ[:, kc:kc + 1], scalar2=None, op0=ALU.mult), r=[b], w=[b2])

            wsQ = WStream(P, [wUQ[h] for h in range(nhB2)], wqs, wqb, castQ)
            for h in range(nhB2):
                wsQ.want(h)
                t2, b2 = wsQ.get(h)
                w3 = t2[:].rearrange("p (k c) -> p k c", k=6)
                qnt, qnb, qnd = qn.next()
                qrt, qrb, qrd = qr.next()
                for tb in range(4):
                    sl = slice(tb * 512, (tb + 1) * 512)
                    p1, pb1, _ = pqn.next()
                    p2, pb2, _ = pqa.next()
                    p3, pb3, _ = pqb.next()
                    for kc in range(6):
                        P.mm(p1[:], w3[:, kc, 0:128], LAT[:, kc, sl], kc == 0, kc == 5, r=[b2, LATb[kc]], w=[pb1])
                    for kc in range(6):
                        P.mm(p2[:], w3[:, kc, 128:256], LAT[:, kc, sl], kc == 0, kc == 5, r=[b2, LATb[kc]], w=[pb2])
                    for kc in range(6):
                        P.mm(p3[:], w3[:, kc, 256:384], LAT[:, kc, sl], kc == 0, kc == 5, r=[b2, LATb[kc]], w=[pb3])
                    P.op("act", lambda e, qnt=qnt, p1=p1, sl=sl: e.activation(out=qnt[:, sl], in_=p1[:], func=AF.Copy),
                         r=[pb1], w=[qnb])
                    t1, b1_, _ = tmp.next()
                    t2_, b2_, _ = tmp.next()
                    P.op("dve", lambda e, t1=t1, p2=p2, sl=sl: e.tensor_tensor(out=t1[:], in0=p2[0:64, :], in1=COS[:, sl], op=ALU.mult),
                         r=[pb2], w=[b1_])
                    P.op("dve", lambda e, t2_=t2_, p3=p3, sl=sl: e.tensor_tensor(out=t2_[:], in0=p3[0:64, :], in1=SIN[:, sl], op=ALU.mult),
                         r=[pb3], w=[b2_])
                    P.op("pool", lambda e, t1=t1, t2_=t2_, qrt=qrt, sl=sl: e.tensor_tensor(out=qrt[:, sl], in0=t1[:], in1=t2_[:], op=ALU.add),
                         r=[b1_, b2_], w=[qrb])
                P.dma(qm[h, 0:128, :], qnt[:], r=[qnb], dsem=qnd, eng="act")
                P.dma(qm[h, 128:192, :], qrt[:], r=[qrb], dsem=qrd, eng="act")
            P.finalize()
    if stop_after == "B":
        return nc

    es = ExitStack()
    with es:
        P = Phase(nc, "C")
        VALL = sb("VALL", [128, 16, 2048], BF16)
        KROT = sb("KROT", [64, S], BF16)
        MASKC = sb("MASKC", [128, 128], BF16)
        ONESB = sb("ONESB", [128, 128], BF16)
        cb_ = Buf()
        P.dma(VALL[:], vm.rearrange("(t p) c -> p t c", p=128), w=[cb_], dsem=P.dsem())
        P.dma(KROT[:], krot, w=[cb_], dsem=P.dsem())
        P.dma(MASKC[:], maskc, w=[cb_], dsem=P.dsem())
        P.op("pool", lambda e: e.memset(ONESB[:], 1.0), w=[cb_])
        qn = Ring(P, [sb(f"cqn{i}", [128, S], BF16) for i in range(2)], True)
        qr = Ring(P, [sb(f"cqr{i}", [64, S], BF16) for i in range(2)], True)
        kn = Ring(P, [sb(f"ckn{i}", [128, S], BF16) for i in range(2)], True)
        pr = Ring(P, [sb(f"cp{i}", [128, 512], BF16) for i in range(4)])
        oh = Ring(P, [sb(f"coh{i}", [128, S], BF16) for i in range(2)], True)
        rz = Ring(P, [sb(f"crz{i}", [128, 512], F32) for i in range(2)])
        pS = Ring(P, [ps(f"cS{i}", [128, 512]) for i in range(3)])
        pO = Ring(P, [ps(f"cO{i}", [128, 512]) for i in range(2)])
        pZ = Ring(P, [ps(f"cZ{i}", [128, 512]) for i in range(2)])
        sc_mla = 192.0 ** -0.5
        def loadC(h):
            qnt, qnb, d1 = qn.next()
            qrt, qrb, d2 = qr.next()
            knt, knb, d3 = kn.next()
            P.dma(qnt[:], qm[h, 0:128, :], w=[qnb], dsem=d1)
            P.dma(qrt[:], qm[h, 128:192, :], w=[qrb], dsem=d2)
            P.dma(knt[:], km[h], w=[knb], dsem=d3)
            return qnt, qnb, qrt, qrb, knt, knb

        headC = {}

        def need_head(h):
            if h < 16 and h not in headC:
                headC[h] = loadC(h)

        itemsC = [(h, Q, j) for h in range(16) for Q in range(4) for j in range(4 * Q + 4)]
        stC = {}

        def emitS(it):
            h, Q, j = it
            if Q == 0 and j == 0:
                need_head(h)
                need_head(h + 1)
            qnt, qnb, qrt, qrb, knt, knb = headC[h]
            q0 = max(512 * Q, 128 * j)
            wd = 512 * Q + 512 - q0
            pst, psb, _ = pS.next()
            P.mm(pst[:, 0:wd], knt[:, j * 128:(j + 1) * 128], qnt[:, q0:q0 + wd], True, False, r=[knb, qnb], w=[psb])
            P.mm(pst[:, 0:wd], KROT[:, j * 128:(j + 1) * 128], qrt[:, q0:q0 + wd], False, True, r=[cb_, qrb], w=[psb])
            pt, ptb, _ = pr.next()
            P.op("act", lambda e, pt=pt, pst=pst, wd=wd: e.activation(out=pt[:, 0:wd], in_=pst[:, 0:wd], func=AF.Exp, scale=sc_mla),
                 r=[psb], w=[ptb])
            if j >= 4 * Q:
                P.op("pool", lambda e, pt=pt: e.tensor_tensor(out=pt[:, 0:128], in0=pt[:, 0:128], in1=MASKC[:], op=ALU.mult),
                     r=[ptb, cb_], w=[ptb])
            stC[it] = (pt, ptb, wd, q0 - 512 * Q)

        accC = {}

        def emitPV(it):
            h, Q, j = it
            pt, ptb, wd, c0 = stC.pop(it)
            nj = 4 * Q + 4
            if j == 0:
                if Q == 0:
                    accC["oh"] = oh.next()
                accC["o"] = pO.next()
                accC["z"] = pZ.next()
            po, pob, _ = accC["o"]
            pz, pzb, _ = accC["z"]
            oht, ohb, ohd = accC["oh"]
            P.mm(po[:, c0:c0 + wd], VALL[:, j, h * 128:(h + 1) * 128], pt[:, 0:wd], j == 0, j == nj - 1, r=[cb_, ptb], w=[pob])
            P.mm(pz[:, c0:c0 + wd], ONESB[:], pt[:, 0:wd], j == 0, j == nj - 1, r=[cb_, ptb], w=[pzb])
            if j == nj - 1:
                rt, rb, _ = rz.next()
                P.op("dve", lambda e, rt=rt, pz=pz: e.reciprocal(out=rt[:], in_=pz[:]), r=[pzb], w=[rb])
                P.op("dve", lambda e, oht=oht, po=po, rt=rt, Q=Q: e.tensor_tensor(
                    out=oht[:, Q * 512:(Q + 1) * 512], in0=po[:], in1=rt[:], op=ALU.mult), r=[pob, rb], w=[ohb])
                if Q == 3:
                    P.dma(om[h * 128:(h + 1) * 128, :], oht[:], r=[ohb], dsem=ohd, eng="act")

        LA = 2
        for i_ in range(min(LA, len(itemsC))):
            emitS(itemsC[i_])
        for i_, it in enumerate(itemsC):
            if i_ + LA < len(itemsC):
                emitS(itemsC[i_ + LA])
            emitPV(it)
        P.finalize()
    if stop_after == "C":
        return nc

    dmask = din("dmask", [128, 20, 128])
    od = dscr("od", [1536, S], BF16)
    es = ExitStack()
    with es:
        P = Phase(nc, "D")
        VD = sb("VD", [128, 3, 16, 512], BF16)
        MASKS = sb("MASKS", [128, 20, 128], F32)
        ONESB = sb("ONESBd", [128, 128], BF16)
        cb_ = Buf()
        for g in range(3):
            P.dma(VD[:, g], vd[g].rearrange("(t p) c -> p t c", p=128), w=[cb_], dsem=P.dsem())
        P.dma(MASKS[:], dmask, w=[cb_], dsem=P.dsem())
        P.op("pool", lambda e: e.memset(ONESB[:], 1.0), w=[cb_])
        UN = [sb(f"UN{g}", [128, S], F32) for g in range(3)]
        ZN = [sb(f"ZN{g}", [128, S], F32) for g in range(3)]
        UNb = [Buf() for _ in range(3)]
        ZNb = [Buf() for _ in range(3)]
        RT = sb("RTd", [128, S], F32)
        RTb = Buf()
        qdr = Ring(P, [sb(f"dq{i}", [128, S], BF16) for i in range(2)], True)
        kdr = Ring(P, [sb(f"dk{i}", [128, S], BF16) for i in range(2)], True)
        ecr = Ring(P, [sb(f"dec{i}", [128, 512], F32) for i in range(2)])
        epr = Ring(P, [sb(f"dep{i}", [128, 512], F32) for i in range(2)])
        pcr = Ring(P, [sb(f"dpc{i}", [128, 512], BF16) for i in range(2)])
        ppr = Ring(P, [sb(f"dpp{i}", [128, 512], BF16) for i in range(2)])
        odr = Ring(P, [sb(f"dod{i}", [128, S], BF16) for i in range(2)], True)
        pSc = Ring(P, [ps(f"dSc{i}", [128, 512]) for i in range(2)])
        pSp = Ring(P, [ps(f"dSp{i}", [128, 512]) for i in range(2)])
        pU = Ring(P, [ps(f"dU{i}", [128, 512]) for i in range(2)])
        pZ = Ring(P, [ps(f"dZ{i}", [128, 512]) for i in range(2)])
        sc_d = 128.0 ** -0.5
        itemsD = [(hs, g, c) for hs in range(4) for g in range(3) for c in range(4)]
        tilesD = {}
        stD = {}

        def emitSD(it):
            hs, g, c = it
            hd = g * 4 + hs
            n, dil = DIL[g]
            nb = n // 128
            if c == 0:
                qt, qb, d1 = qdr.next()
                kt, kb, d2 = kdr.next()
                P.dma(qt[:], qd[hd * 128:(hd + 1) * 128, :], w=[qb], dsem=d1)
                P.dma(kt[:], kd[hd * 128:(hd + 1) * 128, :], w=[kb], dsem=d2)
                tilesD[(hs, g)] = (qt, qb, kt, kb)
            qt, qb, kt, kb = tilesD[(hs, g)]
            us = [4 * c + s_ for s_ in range(4)]
            hp = [u % nb != 0 for u in us]
            s0 = hp.index(True) if any(hp) else 4
            assert all(hp[s0:])
            sc, scb, _ = pSc.next()
            for s_, u in enumerate(us):
                P.mm(sc[:, s_ * 128:(s_ + 1) * 128], kt[:, u * 128:(u + 1) * 128], qt[:, u * 128:(u + 1) * 128],
                     True, True, r=[kb, qb], w=[scb])
            ec, ecb, _ = ecr.next()
            P.op("act", lambda e, ec=ec, sc=sc: e.activation(out=ec[:], in_=sc[:], func=AF.Exp, scale=sc_d), r=[scb], w=[ecb])
            pc, pcb, _ = pcr.next()
            P.op("dve", lambda e, pc=pc, ec=ec, hd=hd: e.tensor_tensor(
                out=pc[:].rearrange("p (s i) -> p s i", s=4), in0=ec[:].rearrange("p (s i) -> p s i", s=4),
                in1=MASKS[:, hd:hd + 1, :].to_broadcast([128, 4, 128]), op=ALU.mult), r=[ecb, cb_], w=[pcb])
            pp, ppb = None, None
            if s0 < 4:
                sp, spb, _ = pSp.next()
                for s_ in range(s0, 4):
                    u = us[s_]
                    P.mm(sp[:, s_ * 128:(s_ + 1) * 128], kt[:, (u - 1) * 128:u * 128], qt[:, u * 128:(u + 1) * 128],
                         True, True, r=[kb, qb], w=[spb])
                ep, epb, _ = epr.next()
                P.op("act", lambda e, ep=ep, sp=sp, s0=s0: e.activation(
                    out=ep[:, s0 * 128:512], in_=sp[:, s0 * 128:512], func=AF.Exp, scale=sc_d), r=[spb], w=[epb])
                pp, ppb, _ = ppr.next()
                ns = 4 - s0
                P.op("dve", lambda e, pp=pp, ep=ep, hd=hd, s0=s0, ns=ns: e.tensor_tensor(
                    out=pp[:, s0 * 128:512].rearrange("p (s i) -> p s i", s=ns),
                    in0=ep[:, s0 * 128:512].rearrange("p (s i) -> p s i", s=ns),
                    in1=MASKS[:, 12 + hd:13 + hd, :].to_broadcast([128, ns, 128]), op=ALU.mult), r=[epb, cb_], w=[ppb])
            stD[it] = (us, hp, pc, pcb, pp, ppb)

        def emitUZ(it):
            hs, g, c = it
            us, hp, pc, pcb, pp, ppb = stD.pop(it)
            pu, pub, _ = pU.next()
            pz, pzb, _ = pZ.next()
            for s_, u in enumerate(us):
                sl = slice(s_ * 128, (s_ + 1) * 128)
                P.mm(pu[:, sl], VD[:, g, u, hs * 128:(hs + 1) * 128], pc[:, sl], True, not hp[s_], r=[cb_, pcb], w=[pub])
                if hp[s_]:
                    P.mm(pu[:, sl], VD[:, g, u - 1, hs * 128:(hs + 1) * 128], pp[:, sl], False, True, r=[cb_, ppb], w=[pub])
            for s_, u in enumerate(us):
                sl = slice(s_ * 128, (s_ + 1) * 128)
                P.mm(pz[:, sl], ONESB[:], pc[:, sl], True, not hp[s_], r=[cb_, pcb], w=[pzb])
                if hp[s_]:
                    P.mm(pz[:, sl], ONESB[:], pp[:, sl], False, True, r=[cb_, ppb], w=[pzb])

            def nat(t):
                if g == 0:
                    return t[:, c * 512:(c + 1) * 512], None
                if g == 1:
                    return t[:].rearrange("p (i r) -> p r i", r=4)[:, c, :], None
                return t[:].rearrange("p (i r) -> p r i", r=16)[:, 4 * c:4 * c + 4, :], 4

            uo, rs_ = nat(UN[g])
            zo, _ = nat(ZN[g])
            ui = pu[:] if rs_ is None else pu[:].rearrange("p (r i) -> p r i", r=4)
            zi = pz[:] if rs_ is None else pz[:].rearrange("p (r i) -> p r i", r=4)
            P.op("act", lambda e, uo=uo, ui=ui: e.activation(out=uo, in_=ui, func=AF.Copy), r=[pub], w=[UNb[g]])
            P.op("dve", lambda e, zo=zo, zi=zi: e.tensor_copy(out=zo, in_=zi), r=[pzb], w=[ZNb[g]])
            if g == 2 and c == 3:
                P.op("pool", lambda e: e.tensor_tensor(out=RT[:], in0=ZN[0][:], in1=ZN[1][:], op=ALU.add), r=[ZNb[0], ZNb[1]], w=[RTb])
                P.op("pool", lambda e: e.tensor_tensor(out=RT[:], in0=RT[:], in1=ZN[2][:], op=ALU.add), r=[RTb, ZNb[2]], w=[RTb])
                P.op("dve", lambda e: e.reciprocal(out=RT[:], in_=RT[:]), r=[RTb], w=[RTb])
                for g2 in range(3):
                    ot, ob, ods = odr.next()
                    P.op("pool" if g2 != 1 else "dve", lambda e, ot=ot, g2=g2: e.tensor_tensor(out=ot[:], in0=UN[g2][:], in1=RT[:], op=ALU.mult),
                         r=[UNb[g2], RTb], w=[ob])
                    P.dma(od[(g2 * 4 + hs) * 128:(g2 * 4 + hs + 1) * 128, :], ot[:], r=[ob], dsem=ods, eng="act")

        emitSD(itemsD[0])
        for i_, it in enumerate(itemsD):
            if i_ + 1 < len(itemsD):
                emitSD(itemsD[i_ + 1])
            emitUZ(it)
        P.finalize()
    if stop_after == "D":
        return nc

    wOM = din("wOM", [16, 128, 2048])
    wOD = din("wOD", [16, 128, 1536])
    p1s = dscr("p1s", [2048, S], F32)
    mgd = dscr("mgd", [2048, S], BF16) if "mgd" in debug else None
    es = ExitStack()
    with es:
        P = Phase(nc, "E1a")
        OM = sb("OM", [128, 16, S], BF16)
        OMb = Buf()
        P.dma(OM[:], om.rearrange("(k p) s -> p k s", p=128), w=[OMb], dsem=P.dsem())
        wst = Ring(P, [sb(f"e1ws{i}", [128, 2048], F32) for i in range(3)], True)
        wbf = Ring(P, [sb(f"e1wb{i}", [128, 2048], BF16) for i in range(3)])
        sgr = Ring(P, [sb(f"e1sg{i}", [128, S], F32) for i in range(2)], True)
        otr = Ring(P, [sb(f"e1o{i}", [128, S], F32) for i in range(2)], True)
        pY = Ring(P, [ps(f"e1p{i}", [128, 512]) for i in range(4)])
        def castE(j, t, b, wt, wb):
            P.op("pool", lambda e, t=t, wt=wt: e.tensor_copy(out=wt[:, 0:768], in_=t[:, 0:768]), r=[b], w=[wb])
            P.op("act", lambda e, t=t, wt=wt: e.activation(out=wt[:, 768:], in_=t[:, 768:], func=AF.Copy), r=[b], w=[wb])

        wsE = WStream(P, [wOM[m] for m in range(16)], wst, wbf, castE)
        for m in range(16):
            wsE.want(m)
            wt, wb = wsE.get(m)
            st, sbf, sds = sgr.next()
            P.dma(st[:], sg[m * 128:(m + 1) * 128, :], w=[sbf], dsem=sds)
            ot, ob, ods = otr.next()
            for tb in range(4):
                sl = slice(tb * 512, (tb + 1) * 512)
                pt, pb, _ = pY.next()
                for kc in range(16):
                    P.mm(pt[:], wt[:, kc * 128:(kc + 1) * 128], OM[:, kc, sl], kc == 0, kc == 15, r=[wb, OMb], w=[pb])
                P.op("dve", lambda e, ot=ot, pt=pt, st=st, sl=sl: e.tensor_tensor(out=ot[:, sl], in0=pt[:], in1=st[:, sl], op=ALU.mult),
                     r=[pb, sbf], w=[ob])
            P.dma(p1s[m * 128:(m + 1) * 128, :], ot[:], r=[ob], dsem=ods, eng="act")
        P.finalize()
    esMG = ExitStack()
    MG = esMG.enter_context(nc.sbuf_tensor("MG", [128, 16, S], BF16))
    es = ExitStack()
    with es:
        P = Phase(nc, "E1b")
        ODs = sb("ODs", [128, 12, S], BF16)
        ODb = Buf()
        MGb = Buf()
        P.dma(ODs[:], od.rearrange("(k p) s -> p k s", p=128), w=[ODb], dsem=P.dsem())
        wst = Ring(P, [sb(f"e2ws{i}", [128, 1536], F32) for i in range(3)], True)
        wbf = Ring(P, [sb(f"e2wb{i}", [128, 1536], BF16) for i in range(3)])
        sgr = Ring(P, [sb(f"e2sg{i}", [128, S], F32) for i in range(2)], True)
        p1r = Ring(P, [sb(f"e2p1{i}", [128, S], F32) for i in range(2)], True)
        tmr = Ring(P, [sb(f"e2t{i}", [128, 512], F32) for i in range(3)])
        pY = Ring(P, [ps(f"e2p{i}", [128, 512]) for i in range(4)])
        def castE(j, t, b, wt, wb):
            P.op("pool", lambda e, t=t, wt=wt: e.tensor_copy(out=wt[:, 0:512], in_=t[:, 0:512]), r=[b], w=[wb])
            P.op("act", lambda e, t=t, wt=wt: e.activation(out=wt[:, 512:], in_=t[:, 512:], func=AF.Copy), r=[b], w=[wb])

        wsE = WStream(P, [wOD[m] for m in range(16)], wst, wbf, castE)
        for m in range(16):
            wsE.want(m)
            wt, wb = wsE.get(m)
            st, sbf, sds = sgr.next()
            P.dma(st[:], sg[2048 + m * 128:2048 + (m + 1) * 128, :], w=[sbf], dsem=sds)
            p1t, p1b, p1d = p1r.next()
            P.dma(p1t[:], p1s[m * 128:(m + 1) * 128, :], w=[p1b], dsem=p1d)
            for tb in range(4):
                sl = slice(tb * 512, (tb + 1) * 512)
                pt, pb, _ = pY.next()
                for kc in range(12):
                    P.mm(pt[:], wt[:, kc * 128:(kc + 1) * 128], ODs[:, kc, sl], kc == 0, kc == 11, r=[wb, ODb], w=[pb])
                tt, tbf, _ = tmr.next()
                P.op("dve", lambda e, tt=tt, pt=pt, st=st, sl=sl: e.tensor_tensor(out=tt[:], in0=pt[:], in1=st[:, sl], op=ALU.mult),
                     r=[pb, sbf], w=[tbf])
                P.op("pool", lambda e, tt=tt, p1t=p1t, sl=sl, m=m: e.tensor_tensor(out=MG[:, m, sl], in0=tt[:], in1=p1t[:, sl], op=ALU.add),
                     r=[tbf, p1b], w=[MGb])
        if mgd is not None:
            P.dma(mgd.rearrange("(k p) s -> p k s", p=128), MG[:], r=[MGb], dsem=P.dsem())
        P.finalize()
    if stop_after == "E1":
        esMG.close()
        return nc

    wOUT = din("wOUT", [16, 128, 2048])
    hs_ = dscr("hs", [S, D], F32)
    es = ExitStack()
    with es:
        P = Phase(nc, "E2a")
        MGb = Buf()
        WOh = sb("WOh", [128, 16, 1024], BF16)
        WOb = [Buf() for _ in range(16)]
        wst = Ring(P, [sb(f"e3ws{i}", [128, 1024], F32) for i in range(3)], True)
        xr = Ring(P, [sb(f"e3x{i}", [128, 1024], F32) for i in range(2)], True)
        hr = Ring(P, [sb(f"e3h{i}", [128, 1024], F32) for i in range(2)], True)
        pH = Ring(P, [ps(f"e3p{i}", [128, 512]) for i in range(4)])
        ci = 0
        for half in range(2):
            hsl = slice(half * 1024, (half + 1) * 1024)
            for kc in range(16):
                t, b, ds = wst.next()
                P.dma(t[:], wOUT[kc][:, hsl], w=[b], dsem=ds)
                eng = ("dve", "pool")[ci % 2]
                ci += 1
                P.op(eng, lambda e, t=t, kc=kc: e.tensor_copy(out=WOh[:, kc, :], in_=t[:]), r=[b], w=[WOb[kc]])
            for t_ in range(16):
                xt, xb, xd = xr.next()
                P.dma(xt[:], x_tm[t_ * 128:(t_ + 1) * 128, hsl], w=[xb], dsem=xd)
                ht, hb, hd_ = hr.next()
                for cb in range(2):
                    csl = slice(cb * 512, (cb + 1) * 512)
                    pt, pb, _ = pH.next()
                    for kc in range(16):
                        P.mm(pt[:], MG[:, kc, t_ * 128:(t_ + 1) * 128], WOh[:, kc, csl], kc == 0, kc == 15,
                             r=[MGb, WOb[kc]], w=[pb])
                    P.op("dve", lambda e, ht=ht, xt=xt, pt=pt, csl=csl: e.scalar_tensor_tensor(
                        out=ht[:, csl], in0=xt[:, csl], scalar=DN_ALPHA, in1=pt[:], op0=ALU.mult, op1=ALU.add),
                        r=[xb, pb], w=[hb])
                P.dma(hs_[t_ * 128:(t_ + 1) * 128, hsl], ht[:], r=[hb], dsem=hd_, eng="act")
        P.finalize()
    esMG.close()
    if stop_after == "E2a":
        return nc

    ln1g = din("ln1_g", [1, D])
    ln1b = din("ln1_b", [1, D])
    ln2g = din("ln2_g", [1, D])
    ln2b = din("ln2_b", [1, D])
    wR = din("wR", [128, 16 * 32])
    bR = din("bR", [1, 32])
    identF = din("identF", [128, 128])
    identB = din("identB", [128, 128], BF16)
    tri = din("tri", [128, 128], BF16)
    eb1 = din("eb1", [1, 32])
    x1s = dscr("x1s", [S, D], F32)
    NROW = NE * CAP + 128
    DUMMY = NE * CAP
    xg = dscr("xg", [NROW, D], BF16)
    ysc = dscr("ysc", [NROW, D], F32)
    esR = ExitStack()
    DEST = esR.enter_context(nc.sbuf_tensor("DEST", [128, 16, 4], I32))
    GATE = esR.enter_context(nc.sbuf_tensor("GATE", [128, 16, 4], F32))

    def layer_norm(P, src, srcb, dst, dstb, G_, B_, gb_, small, t_):
        st, stb, mv, mvb, rs, rsb = small
        for c4 in range(4):
            P.op("dve", lambda e, c4=c4: e.bn_stats(out=st[:, c4, :], in_=src[:, c4 * 512:(c4 + 1) * 512]), r=[srcb], w=[stb])
        P.op("dve", lambda e: e.bn_aggr(out=mv[:], in_=st[:].rearrange("p a b -> p (a b)")), r=[stb], w=[mvb])
        P.op("dve", lambda e: e.tensor_scalar(out=rs[:], in0=mv[:, 1:2], scalar1=LN_EPS, scalar2=None, op0=ALU.add), r=[mvb], w=[rsb])
        P.op("act", lambda e: e.activation(out=rs[:], in_=rs[:], func=AF.Sqrt), r=[rsb], w=[rsb])
        P.op("dve", lambda e: e.reciprocal(out=rs[:], in_=rs[:]), r=[rsb], w=[rsb])
        P.op("dve", lambda e: e.tensor_scalar(out=dst[:], in0=src[:], scalar1=mv[:, 0:1], scalar2=rs[:, 0:1],
                                              op0=ALU.subtract, op1=ALU.mult), r=[srcb, mvb, rsb], w=[dstb])
        hA, hB = slice(0, 1280), slice(1280, D)
        for op_, T_ in ((ALU.mult, G_), (ALU.add, B_)):
            P.op("dve", lambda e, op_=op_, T_=T_: e.tensor_tensor(out=dst[:, hA], in0=dst[:, hA], in1=T_[:, hA], op=op_),
                 r=[dstb, gb_], w=[dstb])
            P.op("pool", lambda e, op_=op_, T_=T_: e.tensor_tensor(out=dst[:, hB], in0=dst[:, hB], in1=T_[:, hB], op=op_),
                 r=[dstb, gb_], w=[dstb])

    es = ExitStack()
    with es:
        P = Phase(nc, "E2b")
        cb_ = Buf()
        G1 = sb("G1", [128, D], F32)
        B1 = sb("B1", [128, D], F32)
        WR = sb("WR", [128, 16 * 32], F32)
        BR = sb("BR", [128, 32], F32)
        IDF = sb("IDF", [128, 128], F32)
        TRI = sb("TRI", [128, 128], BF16)
        ONESB = sb("ONESBe", [128, 128], BF16)
        EB1 = sb("EB1", [128, 32], F32)
        CNT = sb("CNT", [128, 32], F32)
        MASKB = sb("MASKB", [128, 16, 32], BF16)
        ZR = sb("ZR", [128, D], F32)
        P.dma(G1[:], ln1g.to_broadcast([128, D]), w=[cb_], dsem=P.dsem())
        P.dma(B1[:], ln1b.to_broadcast([128, D]), w=[cb_], dsem=P.dsem())
        P.dma(WR[:], wR, w=[cb_], dsem=P.dsem())
        P.dma(BR[:], bR.to_broadcast([128, 32]), w=[cb_], dsem=P.dsem())
        P.dma(IDF[:], identF, w=[cb_], dsem=P.dsem())
        P.dma(TRI[:], tri, w=[cb_], dsem=P.dsem())
        P.dma(EB1[:], eb1.to_broadcast([128, 32]), w=[cb_], dsem=P.dsem())
        P.op("pool", lambda e: e.memset(ONESB[:], 1.0), w=[cb_])
        cntb = Buf()
        P.op("pool", lambda e: e.memset(CNT[:], 0.0), w=[cntb])
        zrb = Buf()
        P.op("pool", lambda e: e.memset(ZR[:], 0.0), w=[zrb])
        P.dma(ysc[DUMMY:DUMMY + 128, :], ZR[:], r=[zrb], dsem=P.dsem())
        hr = Ring(P, [sb(f"e4h{i}", [128, D], F32) for i in range(2)], True)
        x1r = Ring(P, [sb(f"e4x1{i}", [128, D], F32) for i in range(2)], True)
        xbr = Ring(P, [sb(f"e4xb{i}", [128, D], BF16) for i in range(2)])
        xTr = Ring(P, [sb(f"e4xT{i}", [128, 16 * 128], F32) for i in range(2)])
        pT = Ring(P, [ps(f"e4pT{i}", [128, 512]) for i in range(2)])
        pL = Ring(P, [ps(f"e4pL{i}", [128, 512]) for i in range(2)])
        pPos = Ring(P, [ps(f"e4pP{i}", [128, 512]) for i in range(2)])
        pTot = Ring(P, [ps(f"e4pQ{i}", [128, 512]) for i in range(2)])
        scat_ds = [P.dsem() for _ in range(8)]
        sm = lambda n, sh, dt=F32: (sb(n, sh, dt), Buf())
        ST, STb = sm("e4st", [128, 4, 6])
        MV, MVb = sm("e4mv", [128, 2])
        RS, RSb = sm("e4rs", [128, 1])
        LG, LGb = sm("e4lg", [128, 32])
        T8, T8b = sm("e4t8", [128, 8])
        MK, MKb = sm("e4mk", [128, 32])
        NM, NMb = sm("e4nm", [128, 1])
        EX, EXb = sm("e4ex", [128, 32])
        DEN, DENb = sm("e4den", [128, 1])
        GG, GGb = sm("e4gg", [128, 32])
        PS_, PSb = sm("e4pos", [128, 32])
        VL, VLb = sm("e4vl", [128, 32])
        DV, DVb = sm("e4dv", [128, 32])
        D8, D8b = sm("e4d8", [128, 8])
        DF, DFb = sm("e4df", [128, 4])
        OH, OHb = sm("e4oh", [128, 32])
        destb = Buf()
        gateb = Buf()
        mbb = Buf()
        si = 0
        def stage1(t_):
            ht, hb, hd_ = hr.next()
            P.dma(ht[:], hs_[t_ * 128:(t_ + 1) * 128, :], w=[hb], dsem=hd_)
            x1t, x1b_, x1d = x1r.next()
            layer_norm(P, ht, hb, x1t, x1b_, G1, B1, cb_, (ST, STb, MV, MVb, RS, RSb), t_)
            P.dma(x1s[t_ * 128:(t_ + 1) * 128, :], x1t[:], r=[x1b_], dsem=x1d, eng="act")
            xbt, xbb, _ = xbr.next()
            P.op("act", lambda e, xbt=xbt, x1t=x1t: e.activation(out=xbt[:], in_=x1t[:], func=AF.Copy), r=[x1b_], w=[xbb])
            xT, xTb, _ = xTr.next()
            for q4 in range(4):
                pt, pb, _ = pT.next()
                for j in range(4):
                    kc = q4 * 4 + j
                    P.op("pe", lambda e, pt=pt, j=j, kc=kc, x1t=x1t: e.transpose(
                        out=pt[:, j * 128:(j + 1) * 128], in_=x1t[:, kc * 128:(kc + 1) * 128], identity=IDF[:]),
                        r=[x1b_, cb_], w=[pb])
                P.op("act" if q4 % 2 else "dve", (lambda e, xT=xT, pt=pt, q4=q4: e.activation(out=xT[:, q4 * 512:(q4 + 1) * 512], in_=pt[:], func=AF.Copy))
                     if q4 % 2 else (lambda e, xT=xT, pt=pt, q4=q4: e.tensor_copy(out=xT[:, q4 * 512:(q4 + 1) * 512], in_=pt[:])),
                     r=[pb], w=[xTb])
            return xbt, xbb, xT, xTb

        def stage2(t_, xbt, xbb, xT, xTb):
            nonlocal si
            pl, plb, _ = pL.next()
            for kc in range(16):
                P.mm(pl[:, 0:32], xT[:, kc * 128:(kc + 1) * 128], WR[:, kc * 32:(kc + 1) * 32], kc == 0, kc == 15, r=[xTb, cb_], w=[plb])
            P.op("dve", lambda e, pl=pl: e.tensor_tensor(out=LG[:], in0=pl[:, 0:32], in1=BR[:], op=ALU.add), r=[plb, cb_], w=[LGb])
            P.op("dve", lambda e: e.max(out=T8[:], in_=LG[:]), r=[LGb], w=[T8b])
            P.op("dve", lambda e: e.tensor_scalar(out=MK[:], in0=LG[:], scalar1=T8[:, 3:4], scalar2=None, op0=ALU.is_ge), r=[LGb, T8b], w=[MKb])
            P.op("dve", lambda e: e.tensor_scalar(out=NM[:], in0=T8[:, 0:1], scalar1=-1.0, scalar2=None, op0=ALU.mult), r=[T8b], w=[NMb])
            P.op("act", lambda e: e.activation(out=EX[:], in_=LG[:], func=AF.Exp, bias=NM[:, 0:1], scale=1.0), r=[LGb, NMb], w=[EXb])
            P.op("dve", lambda e: e.tensor_tensor(out=EX[:], in0=EX[:], in1=MK[:], op=ALU.mult), r=[EXb, MKb], w=[EXb])
            P.op("dve", lambda e: e.reduce_sum(out=DEN[:], in_=EX[:], axis=AX.X), r=[EXb], w=[DENb])
            P.op("dve", lambda e: e.reciprocal(out=DEN[:], in_=DEN[:]), r=[DENb], w=[DENb])
            P.op("pool", lambda e, t_=t_: e.tensor_copy(out=MASKB[:, t_, :], in_=MK[:]), r=[MKb], w=[mbb])
            pp_, ppb, _ = pPos.next()
            pq_, pqb, _ = pTot.next()
            P.mm(pp_[:, 0:32], TRI[:], MASKB[:, t_, :], True, True, r=[mbb, cb_], w=[ppb])
            P.mm(pq_[:, 0:32], ONESB[:], MASKB[:, t_, :], True, True, r=[mbb, cb_], w=[pqb])
            P.op("dve", lambda e, pp_=pp_: e.tensor_tensor(out=PS_[:], in0=pp_[:, 0:32], in1=CNT[:], op=ALU.add), r=[ppb, cntb], w=[PSb])
            P.op("dve", lambda e, pq_=pq_: e.tensor_tensor(out=CNT[:], in0=pq_[:, 0:32], in1=CNT[:], op=ALU.add), r=[pqb, cntb], w=[cntb])
            P.op("dve", lambda e: e.tensor_scalar(out=VL[:], in0=PS_[:], scalar1=float(CAP), scalar2=None, op0=ALU.is_lt), r=[PSb], w=[VLb])
            P.op("dve", lambda e: e.tensor_tensor(out=VL[:], in0=VL[:], in1=MK[:], op=ALU.mult), r=[VLb, MKb], w=[VLb])
            P.op("dve", lambda e: e.scalar_tensor_tensor(out=GG[:], in0=EX[:], scalar=DEN[:, 0:1], in1=VL[:], op0=ALU.mult, op1=ALU.mult),
                 r=[EXb, DENb, VLb], w=[GGb])
            P.op("dve", lambda e: e.tensor_tensor(out=DV[:], in0=PS_[:], in1=EB1[:], op=ALU.add), r=[PSb, cb_], w=[DVb])
            P.op("dve", lambda e: e.tensor_tensor(out=DV[:], in0=DV[:], in1=VL[:], op=ALU.mult), r=[DVb, VLb], w=[DVb])
            P.op("dve", lambda e: e.max(out=D8[:], in_=DV[:]), r=[DVb], w=[D8b])
            P.op("dve", lambda e: e.tensor_scalar(out=DF[:], in0=D8[:, 0:4], scalar1=0.0, scalar2=float(DUMMY + 1),
                                                  op0=ALU.is_equal, op1=ALU.mult), r=[D8b], w=[DFb])
            P.op("dve", lambda e: e.scalar_tensor_tensor(out=DF[:], in0=D8[:, 0:4], scalar=-1.0, in1=DF[:], op0=ALU.add, op1=ALU.add),
                 r=[D8b, DFb], w=[DFb])
            P.op("dve", lambda e, t_=t_: e.tensor_copy(out=DEST[:, t_, :], in_=DF[:]), r=[DFb], w=[destb])
            for k in range(4):
                P.op("dve", lambda e, k=k: e.tensor_scalar(out=OH[:], in0=DV[:], scalar1=D8[:, k:k + 1], scalar2=None, op0=ALU.is_equal),
                     r=[DVb, D8b], w=[OHb])
                P.op("dve", lambda e: e.tensor_tensor(out=OH[:], in0=OH[:], in1=GG[:], op=ALU.mult), r=[OHb, GGb], w=[OHb])
                P.op("dve", lambda e, k=k, t_=t_: e.reduce_sum(out=GATE[:, t_, k:k + 1], in_=OH[:], axis=AX.X), r=[OHb], w=[gateb])
            for k in range(4):
                ds = scat_ds[si % 8]
                si += 1
                P.op("pool", lambda e, xbt=xbt, t_=t_, k=k: e.indirect_dma_start(
                    out=xg, out_offset=bass.IndirectOffsetOnAxis(ap=DEST[:, t_, k:k + 1], axis=0),
                    in_=xbt[:], in_offset=None), r=[xbb, destb], w=[], dsem=ds)
        nx1 = stage1(0)
        for t_ in range(16):
            cur1 = nx1
            if t_ + 1 < 16:
                nx1 = stage1(t_ + 1)
            stage2(t_, *cur1)
        P.finalize()
    if stop_after == "E2":
        esR.close()
        return nc

    w1r = din("w1r", [NE, 16, 128, 4096])
    w2r = din("w2r", [NE, 8, 128, 4096])
    b1r = din("b1r", [128, NE * 32])
    b2 = din("b2", [NE, D])
    es = ExitStack()
    with es:
        P = Phase(nc, "G")
        cb_ = Buf()
        IDB = sb("IDB", [128, 128], BF16)
        B1A = sb("B1A", [128, NE * 32], F32)
        B1L = sb("B1L", [128, NE * 16], F32)
        P.dma(IDB[:], identB, w=[cb_], dsem=P.dsem())
        P.dma(B1A[:], b1r, w=[cb_], dsem=P.dsem())
        P.op("dve", lambda e: e.tensor_scalar(
            out=B1L[:].rearrange("p (e m) -> p e m", e=NE), in0=B1A[:].rearrange("p (e m) -> p e m", e=NE)[:, :, 16:32],
            scalar1=1.0, scalar2=None, op0=ALU.add), r=[cb_], w=[cb_])
        xgr = Ring(P, [sb(f"gx{i}", [128, D], BF16) for i in range(3)], True)
        xgT = Ring(P, [sb(f"gxT{i}", [128, 16, CAP], BF16) for i in range(2)])
        wst = Ring(P, [sb(f"gws{i}", [128, 4096], F32) for i in range(3)], True)
        wbf = Ring(P, [sb(f"gwb{i}", [128, 4096], BF16) for i in range(3)])
        atr = Ring(P, [sb(f"gat{i}", [128, 16, CAP], BF16) for i in range(2)])
        g1r = Ring(P, [sb(f"gg1{i}", [128, CAP], F32) for i in range(2)])
        sgr = Ring(P, [sb(f"gsg{i}", [128, CAP], F32) for i in range(2)])
        l1r = Ring(P, [sb(f"gl1{i}", [128, CAP], F32) for i in range(2)])
        ytr = Ring(P, [sb(f"gy{i}", [128, 256], F32) for i in range(4)], True)
        b2r = Ring(P, [sb(f"gb2{i}", [128, D], F32) for i in range(2)], True)
        pTr = Ring(P, [ps(f"gpT{i}", [128, 1024], BF16) for i in range(2)])
        pG = Ring(P, [ps(f"gpG{i}", [128, 512]) for i in range(2)])
        pLn = Ring(P, [ps(f"gpL{i}", [128, 512]) for i in range(2)])
        pYr = Ring(P, [ps(f"gpY{i}", [128, 512]) for i in range(2)])
        cast_pat = ("act", "dve", "act", "dve", "act", "pool")
        cig = [0]

        def castG(j, t, b, wt, wb):
            for hf in range(2):
                eng = cast_pat[cig[0] % 6]
                cig[0] += 1
                hsl = slice(hf * 2048, (hf + 1) * 2048)
                if eng == "act":
                    P.op("act", lambda e, wt=wt, t=t, hsl=hsl: e.activation(out=wt[:, hsl], in_=t[:, hsl], func=AF.Copy), r=[b], w=[wb])
                else:
                    P.op(eng, lambda e, wt=wt, t=t, hsl=hsl: e.tensor_copy(out=wt[:, hsl], in_=t[:, hsl]), r=[b], w=[wb])

        srcsG = []
        for ex in range(NE):
            srcsG += [w1r[ex, m] for m in range(16)] + [w2r[ex, cb] for cb in range(8)]
        wsG = WStream(P, srcsG, wst, wbf, castG)

        def load_xg(ex):
            res_ = []
            for st_ in range(NST):
                xt, xb, xd = xgr.next()
                r0 = ex * CAP + st_ * 128
                P.dma(xt[:], xg[r0:r0 + 128, :], w=[xb], dsem=xd)
                res_.append((xt, xb))
            return res_

        nxt_xg = load_xg(0)
        for ex in range(NE):
            b2t, b2b, b2d = b2r.next()
            P.dma(b2t[:], b2[ex:ex + 1, :].to_broadcast([128, D]), w=[b2b], dsem=b2d)
            XT_, XTb_, _ = xgT.next()
            cur_xg = nxt_xg
            for st_ in range(NST):
                xt, xb = cur_xg[st_]
                for h8 in range(2):
                    pt, pb, _ = pTr.next()
                    for j in range(8):
                        kc = h8 * 8 + j
                        P.op("pe", lambda e, pt=pt, j=j, kc=kc, xt=xt: e.transpose(
                            out=pt[:, j * 128:(j + 1) * 128], in_=xt[:, kc * 128:(kc + 1) * 128], identity=IDB[:]),
                            r=[xb, cb_], w=[pb])
                    o_ap = XT_[:, h8 * 8:(h8 + 1) * 8, st_ * 128:(st_ + 1) * 128]
                    i_ap = pt[:].rearrange("p (j c) -> p j c", j=8)
                    if h8 == 0:
                        P.op("dve", lambda e, o_ap=o_ap, i_ap=i_ap: e.tensor_copy(out=o_ap, in_=i_ap), r=[pb], w=[XTb_])
                    else:
                        P.op("act", lambda e, o_ap=o_ap, i_ap=i_ap: e.activation(out=o_ap, in_=i_ap, func=AF.Copy), r=[pb], w=[XTb_])
            AT, ATb, _ = atr.next()
            for m in range(16):
                wsG.want(ex * 24 + m)
                wt, wb = wsG.get(ex * 24 + m)
                w3 = wt[:].rearrange("p (k c) -> p k c", k=16)
                pg, pgb, _ = pG.next()
                pl, plb, _ = pLn.next()
                for kc in range(16):
                    P.mm(pg[:, 0:CAP], w3[:, kc, 0:128], XT_[:, kc, :], kc == 0, kc == 15, r=[wb, XTb_], w=[pgb])
                for kc in range(16):
                    P.mm(pl[:, 0:CAP], w3[:, kc, 128:256], XT_[:, kc, :], kc == 0, kc == 15, r=[wb, XTb_], w=[plb])
                g1, g1b, _ = g1r.next()
                sgt, sgb, _ = sgr.next()
                l1, l1b, _ = l1r.next()
                bg = B1A[:, ex * 32 + m:ex * 32 + m + 1]
                bl = B1L[:, ex * 16 + m:ex * 16 + m + 1]
                P.op("dve", lambda e, g1=g1, pg=pg, bg=bg: e.tensor_scalar(out=g1[:], in0=pg[:, 0:CAP], scalar1=bg, scalar2=7.0,
                                                                            op0=ALU.add, op1=ALU.min), r=[pgb, cb_], w=[g1b])
                P.op("act", lambda e, sgt=sgt, g1=g1: e.activation(out=sgt[:], in_=g1[:], func=AF.Sigmoid, scale=1.702), r=[g1b], w=[sgb])
                P.op("dve", lambda e, l1=l1, pl=pl, bl=bl: e.tensor_scalar(out=l1[:], in0=pl[:, 0:CAP], scalar1=bl, scalar2=-6.0,
                                                                            op0=ALU.add, op1=ALU.max), r=[plb, cb_], w=[l1b])
                P.op("pool", lambda e, g1=g1, sgt=sgt: e.tensor_tensor(out=g1[:], in0=g1[:], in1=sgt[:], op=ALU.mult), r=[g1b, sgb], w=[g1b])
                P.op("dve", lambda e, AT=AT, m=m, l1=l1, g1=g1: e.scalar_tensor_tensor(
                    out=AT[:, m, :], in0=l1[:], scalar=8.0, in1=g1[:], op0=ALU.min, op1=ALU.mult), r=[l1b, g1b], w=[ATb])
            if ex + 1 < NE:
                nxt_xg = load_xg(ex + 1)
            for cb in range(8):
                wsG.want(ex * 24 + 16 + cb)
                wt, wb = wsG.get(ex * 24 + 16 + cb)
                w3 = wt[:].rearrange("p (k c) -> p k c", k=16)
                for st_ in range(NST):
                    py, pyb, _ = pYr.next()
                    for kc in range(16):
                        P.mm(py[:, 0:256], AT[:, kc, st_ * 128:(st_ + 1) * 128], w3[:, kc, :], kc == 0, kc == 15, r=[ATb, wb], w=[pyb])
                    yt, yb, yd = ytr.next()
                    P.op("dve", lambda e, yt=yt, py=py, b2t=b2t, cb=cb: e.tensor_tensor(
                        out=yt[:], in0=py[:, 0:256], in1=b2t[:, cb * 256:(cb + 1) * 256], op=ALU.add), r=[pyb, b2b], w=[yb])
                    r0 = ex * CAP + st_ * 128
                    P.dma(ysc[r0:r0 + 128, cb * 256:(cb + 1) * 256], yt[:], r=[yb], dsem=yd, eng="act")
        P.finalize()
    if stop_after == "G":
        esR.close()
        return nc

    es = ExitStack()
    with es:
        P = Phase(nc, "H")
        cb_ = Buf()
        rb_ = Buf()
        G2 = sb("G2", [128, D], F32)
        B2_ = sb("B2_", [128, D], F32)
        P.dma(G2[:], ln2g.to_broadcast([128, D]), w=[cb_], dsem=P.dsem())
        P.dma(B2_[:], ln2b.to_broadcast([128, D]), w=[cb_], dsem=P.dsem())
        ygr = Ring(P, [sb(f"hy{i}", [128, D], F32) for i in range(8)], True)
        x1r = Ring(P, [sb(f"hx{i}", [128, D], F32) for i in range(2)], True)
        acr = Ring(P, [sb(f"ha{i}", [128, D], F32) for i in range(2)])
        otr = Ring(P, [sb(f"ho{i}", [128, D], F32) for i in range(2)], True)
        ST = sb("hst", [128, 4, 6], F32)
        MV = sb("hmv", [128, 2], F32)
        RS = sb("hrs", [128, 1], F32)
        STb, MVb, RSb = Buf(), Buf(), Buf()
        def issueH(t_):
            x1t, x1b_, x1d = x1r.next()
            P.dma(x1t[:], x1s[t_ * 128:(t_ + 1) * 128, :], w=[x1b_], dsem=x1d)
            ys = []
            for k in range(4):
                yt, yb, yd = ygr.next()
                P.op("pool", lambda e, yt=yt, t_=t_, k=k: e.indirect_dma_start(
                    out=yt[:], out_offset=None, in_=ysc,
                    in_offset=bass.IndirectOffsetOnAxis(ap=DEST[:, t_, k:k + 1], axis=0)), r=[rb_], w=[yb], dsem=yd)
                ys.append((yt, yb))
            return x1t, x1b_, ys

        nxtH = issueH(0)
        for t_ in range(16):
            x1t, x1b_, ys = nxtH
            if t_ + 1 < 16:
                nxtH = issueH(t_ + 1)
            ac, acb, _ = acr.next()
            P.op("act", lambda e, ac=ac, x1t=x1t: e.activation(out=ac[:], in_=x1t[:], func=AF.Copy, scale=DN_ALPHA), r=[x1b_], w=[acb])
            for k in range(4):
                yt, yb = ys[k]
                P.op("dve", lambda e, ac=ac, yt=yt, t_=t_, k=k: e.scalar_tensor_tensor(
                    out=ac[:], in0=yt[:], scalar=GATE[:, t_, k:k + 1], in1=ac[:], op0=ALU.mult, op1=ALU.add), r=[yb, acb], w=[acb])
            ot, ob, od_ = otr.next()
            layer_norm(P, ac, acb, ot, ob, G2, B2_, cb_, (ST, STb, MV, MVb, RS, RSb), t_)
            P.dma(out[t_ * 128:(t_ + 1) * 128, :], ot[:], r=[ob], dsem=od_, eng="act")
        P.finalize()
    esR.close()
    return nc


OFF_CQ, OFF_CKV, OFF_KPE, OFF_QD, OFF_KD, OFF_VD, OFF_GM, OFF_GD = 0, 768, 1280, 1344, 2880, 4416, 5952, 8000


def lhs_blocks(w, cols):
    K = w.shape[0]
    ws = w[:, cols]
    nb = ws.shape[1] // 128
    return np.ascontiguousarray(ws.reshape(K // 128, 128, nb, 128).transpose(2, 1, 0, 3))


def prep_shared(inp):
    w_in = np.asarray(inp["w_in"])[0]
    half = ROPE // 2
    kpe = np.arange(OFF_KPE, OFF_KPE + ROPE)
    kpe_perm = np.concatenate([kpe[half:], kpe[:half]])
    colsA = np.concatenate([
        np.arange(0, OFF_KPE), kpe, kpe_perm,
        np.arange(OFF_QD, OFF_QD + 1536), np.arange(OFF_KD, OFF_KD + 1536),
        np.arange(OFF_GM, OFF_GM + 4096)])
    sh = {}
    sh["wA"] = lhs_blocks(w_in, colsA)
    wv = w_in[:, OFF_VD:OFF_VD + 1536].reshape(4, 4, 128, 3, 512)
    sh["wV"] = np.ascontiguousarray(wv.transpose(3, 0, 2, 1, 4)).reshape(12, 128, 4, 512)
    w_uq = np.asarray(inp["w_uq"])[0]
    cols = []
    for h in range(MH):
        b = h * 192
        rope = np.arange(b + 128, b + 192)
        perm = np.concatenate([rope[half:], rope[:half]])
        cols.append(np.concatenate([np.arange(b, b + 128), rope, perm, perm, rope]))
    cols = np.concatenate(cols)
    wq = w_uq[:, cols].reshape(6, 128, MH, 384)
    sh["wUQ"] = np.ascontiguousarray(wq.transpose(2, 1, 0, 3)).reshape(MH, 128, 6 * 384)
    sh["gq"] = np.ascontiguousarray(np.asarray(inp["q_norm_g"])[0].reshape(6, 128).T)
    w_ukv = np.asarray(inp["w_ukv"])[0].reshape(4, 128, MH, 256)
    sh["wUK"] = np.ascontiguousarray(w_ukv[:, :, :, 0:128].transpose(2, 1, 0, 3)).reshape(MH, 128, 4 * 128)
    sh["wUV"] = np.ascontiguousarray(w_ukv[:, :, :, 128:256]).reshape(4, 128, MH * 128)
    sh["gkv"] = np.ascontiguousarray(np.asarray(inp["kv_norm_g"])[0].reshape(4, 128).T)
    inv = (np.float32(10000.0) ** (-np.arange(half, dtype=np.float32) / np.float32(half))).astype(np.float32)
    ang = (np.arange(S, dtype=np.float32)[:, None] * inv[None, :]).astype(np.float32)
    cs, sn = np.cos(ang).astype(np.float32).T, np.sin(ang).astype(np.float32).T
    sh["cosT"] = np.ascontiguousarray(np.concatenate([cs, cs], 0))
    sh["sinT"] = np.ascontiguousarray(np.concatenate([-sn, sn], 0))
    jj, ii = np.arange(128)[:, None], np.arange(128)[None, :]
    sh["maskc"] = (jj <= ii).astype(np.float32).astype(ml_dtypes.bfloat16)
    slopes = 2.0 ** (-8.0 * np.arange(1, DH + 1, dtype=np.float64) / DH)
    dm = np.zeros((20, 128, 128), np.float64)
    for hd in range(DH):
        dil = DIL[hd // 4][1]
        st = (ii - jj).astype(np.float64)
        dm[hd] = np.where(ii >= jj, np.exp(-slopes[hd] * dil * st), 0.0)
        if hd < 8:
            dm[12 + hd] = np.where(ii <= jj, np.exp(-slopes[hd] * dil * (st + 128.0)), 0.0)
    sh["dmask"] = np.ascontiguousarray(dm.transpose(1, 0, 2)).astype(np.float32)
    sh["wOUT"] = np.ascontiguousarray(np.asarray(inp["w_out"])[0].reshape(16, 128, 2048))
    for k in ("ln1_g", "ln1_b", "ln2_g", "ln2_b"):
        sh[k] = np.ascontiguousarray(np.asarray(inp[k]).reshape(1, D))
    sh["wR"] = np.ascontiguousarray(np.asarray(inp["w_router"])[0].reshape(16, 128, NE).transpose(1, 0, 2)).reshape(128, 16 * NE)
    sh["bR"] = np.ascontiguousarray(np.asarray(inp["b_router"]).reshape(1, NE))
    sh["identF"] = np.eye(128, dtype=np.float32)
    sh["identB"] = np.eye(128, dtype=np.float32).astype(ml_dtypes.bfloat16)
    sh["tri"] = (jj < ii).astype(np.float32).astype(ml_dtypes.bfloat16)
    sh["eb1"] = (np.arange(NE, dtype=np.float32) * CAP + 1.0).reshape(1, NE)
    w1 = np.asarray(inp["w1"])[0]
    w1 = w1.reshape(NE, 16, 128, 16, 128, 2)
    sh["w1r"] = np.ascontiguousarray(w1.transpose(0, 3, 2, 1, 5, 4)).reshape(NE, 16, 128, 4096)
    w2 = np.asarray(inp["w2"])[0].reshape(NE, 16, 128, 8, 256)
    sh["w2r"] = np.ascontiguousarray(w2.transpose(0, 3, 2, 1, 4)).reshape(NE, 8, 128, 4096)
    b1 = np.asarray(inp["b1"])[0].reshape(NE, 16, 128, 2)
    sh["b1r"] = np.ascontiguousarray(b1.transpose(2, 0, 3, 1)).reshape(128, NE * 32)
    sh["b2"] = np.ascontiguousarray(np.asarray(inp["b2"])[0])
    sh["wOM"] = lhs_blocks(np.asarray(inp["w_o_mla"])[0], np.arange(2048)).reshape(16, 128, 2048)
    sh["wOD"] = lhs_blocks(np.asarray(inp["w_o_dil"])[0], np.arange(2048)).reshape(16, 128, 1536)
    return sh


def make_in_maps(inp):
    sh = prep_shared(inp)
    x = np.asarray(inp["x"])
    maps = []
    for c in range(NCORES):
        m = dict(sh)
        m["x"] = np.ascontiguousarray(x[c])
        m["xT"] = np.ascontiguousarray(x[c].T)
        maps.append(m)
    return maps


def kernel(**inputs):
    nc = build()
    in_maps = make_in_maps(inputs)
    res = run_bass_kernel_spmd(nc, in_maps, core_ids=list(range(NCORES)))
    return np.stack([np.asarray(r["out"]) for r in res.results], axis=0).astype(np.float32)
```
